# Optimizing a Trainium2 kernel written in Bass

```python
import math
import jax
import jax.numpy as jnp
from jax import lax
import numpy as np


D_MODEL = 1024
BATCH = 8
SEQ = 8192
DEPTH = 2

CHUNK = 64
Q_BLOCK = 128
EPS = 1e-6
ML_HEADS = 4
ML_HD = 256
ML_W = ML_HEADS * ML_HD
ML_CONV = 4
GLA_HEADS = 4
GLA_DK = 64
GLA_DV = 128
GLA_WK = GLA_HEADS * GLA_DK
GLA_W = GLA_HEADS * GLA_DV
GLA_RANK = 16
GLA_TAU = 16.0
DSA_HEADS = 4
DSA_HD = 128
DSA_W = DSA_HEADS * DSA_HD
IDX_HEADS = 8
IDX_DIM = 64
TOPK_MAX = 256
REL_BUCKETS = 32
REL_MAX_DIST = 128
D_MIX = ML_W + GLA_W + DSA_W

COLUMN_LAYOUT = (
    ('ml_q', ML_W), ('ml_k', ML_W), ('ml_v', ML_W), ('ml_o', ML_W), ('ml_z', ML_W),
    ('ml_i', ML_HEADS), ('ml_f', ML_HEADS),
    ('gla_q', GLA_WK), ('gla_k', GLA_WK), ('gla_v', GLA_W), ('gla_a', GLA_RANK), ('gla_r', GLA_W),
    ('dsa_q', DSA_W), ('dsa_k', DSA_W), ('dsa_v', DSA_W), ('dsa_z', DSA_W),
    ('idx_q', IDX_HEADS * IDX_DIM), ('idx_k', IDX_DIM), ('idx_w', IDX_HEADS),
)
N_IN = sum(width for _, width in COLUMN_LAYOUT)

kernel_name = 'hybrid_mlstm_gla_dsa_block'


def rmsnorm(x, g):
    xf = x.astype(jnp.float32)
    y = xf * lax.rsqrt(jnp.mean(xf * xf, axis=-1, keepdims=True) + EPS)
    return (y * g.astype(jnp.float32)).astype(x.dtype)


def split_columns(u):
    out, off = {}, 0
    for name, width in COLUMN_LAYOUT:
        out[name] = u[..., off:off + width]
        off += width
    return out


def heads(a, h):
    return a.reshape(a.shape[:-1] + (h, a.shape[-1] // h))


def causal_dwconv(x, w, b):
    c = x.shape[-1]
    y = lax.conv_general_dilated(x, w.astype(x.dtype)[:, None, :], window_strides=(1,),
                                 padding=[(w.shape[0] - 1, 0)],
                                 dimension_numbers=('NWC', 'WIO', 'NWC'), feature_group_count=c)
    return y + b.astype(x.dtype)


def to_chunks(a):
    b, s, h, d = a.shape
    return a.reshape(b, s // CHUNK, CHUNK, h, d).transpose(1, 0, 3, 2, 4)


def gate_chunks(g):
    b, s, h = g.shape
    return g.reshape(b, s // CHUNK, CHUNK, h).transpose(1, 0, 3, 2)


def from_chunks(y):
    nc, b, h, l, d = y.shape
    return y.transpose(1, 0, 3, 2, 4).reshape(b, nc * l, h, d)


def mlstm_chunkwise(q, k, v, li, lf):
    b, s, h, d = q.shape
    dv = v.shape[-1]
    f32 = jnp.float32
    qc = to_chunks(q.astype(f32))
    kc = to_chunks(k.astype(f32) * (d ** -0.5))
    vc = to_chunks(v.astype(f32))
    lic, lfc = gate_chunks(li.astype(f32)), gate_chunks(lf.astype(f32))
    causal = jnp.tril(jnp.ones((CHUNK, CHUNK), dtype=bool))

    def step(carry, inp):
        C, n, m = carry
        qj, kj, vj, lij, lfj = inp
        bcum = jnp.cumsum(lfj, axis=-1)
        dmat = jnp.where(causal, bcum[..., :, None] - bcum[..., None, :] + lij[..., None, :], -jnp.inf)
        inter = bcum + m[..., None]
        m_row = jnp.maximum(inter, jnp.max(dmat, axis=-1))
        w_intra = jnp.exp(dmat - m_row[..., None])
        w_inter = jnp.exp(inter - m_row)
        s_qk = jnp.einsum('bhld,bhsd->bhls', qj, kj) * w_intra
        num = jnp.einsum('bhls,bhse->bhle', s_qk, vj) + w_inter[..., None] * jnp.einsum('bhld,bhde->bhle', qj, C)
        den = jnp.sum(s_qk, axis=-1) + w_inter * jnp.einsum('bhld,bhd->bhl', qj, n)
        hj = num / jnp.maximum(jnp.abs(den), jnp.exp(-m_row))[..., None]
        g = bcum[..., -1]
        a = g[..., None] - bcum + lij
        m_new = jnp.maximum(g + m, jnp.max(a, axis=-1))
        decay = jnp.exp(g + m - m_new)
        wa = jnp.exp(a - m_new[..., None])
        C_new = decay[..., None, None] * C + jnp.einsum('bhl,bhld,bhle->bhde', wa, kj, vj)
        n_new = decay[..., None] * n + jnp.einsum('bhl,bhld->bhd', wa, kj)
        return (C_new, n_new, m_new), hj

    init = (jnp.zeros((b, h, d, dv), f32), jnp.zeros((b, h, d), f32), jnp.zeros((b, h), f32))
    _, hc = lax.scan(step, init, (qc, kc, vc, lic, lfc))
    return from_chunks(hc)


def gla_chunked(q, k, v, la):
    b, s, h, dk = q.shape
    dv = v.shape[-1]
    f32 = jnp.float32
    qc = to_chunks(q.astype(f32) * (dk ** -0.5))
    kc, vc, lac = to_chunks(k.astype(f32)), to_chunks(v.astype(f32)), to_chunks(la.astype(f32))
    causal = jnp.tril(jnp.ones((CHUNK, CHUNK), dtype=bool))

    def step(S, inp):
        qj, kj, vj, laj = inp
        bcum = jnp.cumsum(laj, axis=2)
        diff = bcum[:, :, :, None, :] - bcum[:, :, None, :, :]
        decay = jnp.exp(jnp.where(causal[..., None], diff, -jnp.inf))
        attn = jnp.einsum('bhld,bhsd,bhlsd->bhls', qj, kj, decay)
        o = jnp.einsum('bhls,bhse->bhle', attn, vj) + jnp.einsum('bhld,bhde->bhle', qj * jnp.exp(bcum), S)
        btot = bcum[:, :, -1:, :]
        S_new = jnp.exp(btot[:, :, 0, :])[..., None] * S + jnp.einsum('bhld,bhle->bhde', kj * jnp.exp(btot - bcum), vj)
        return S_new, o

    _, oc = lax.scan(step, jnp.zeros((b, h, dk, dv), f32), (qc, kc, vc, lac))
    return from_chunks(oc)


def t5_bucket(rel):
    half = REL_BUCKETS // 2
    max_exact = half // 2
    ret = jnp.where(rel > 0, half, 0)
    n = jnp.abs(rel)
    nf = jnp.maximum(n, 1).astype(jnp.float32)
    large = max_exact + (jnp.log(nf / max_exact) / math.log(REL_MAX_DIST / max_exact)
                         * (half - max_exact)).astype(jnp.int32)
    large = jnp.minimum(large, half - 1)
    return ret + jnp.where(n < max_exact, n, large)


def dsa_sparse_attention(q, k, v, iq, ik, iw, rel_bias, topk):
    b, s, h, dh = q.shape
    nqb = s // Q_BLOCK
    key_pos = jnp.arange(s, dtype=jnp.int32)
    bidx = jnp.arange(b)[:, None, None]

    def blocks(a):
        return jnp.moveaxis(a.reshape((b, nqb, Q_BLOCK) + a.shape[2:]), 1, 0)

    qpos = key_pos.reshape(nqb, Q_BLOCK)

    def one_block(args):
        qb, iqb, iwb, pos = args
        limit = (pos // CHUNK + 1) * CHUNK
        admissible = key_pos[None, :] < limit[:, None]
        idx_logits = jnp.einsum('bthd,bsd->bths', iqb, ik)
        score = jnp.einsum('bths,bth->bts', jax.nn.relu(idx_logits), iwb).astype(jnp.float32)
        score = jnp.where(admissible[None], score, -jnp.inf)
        _, sel = lax.top_k(score, topk)
        ks = k[bidx, sel]
        vs = v[bidx, sel]
        valid = sel < limit[None, :, None]
        bias = rel_bias[t5_bucket(sel - pos[None, :, None])].astype(jnp.float32)
        logits = jnp.einsum('bthd,btkhd->bthk', qb, ks).astype(jnp.float32) * (dh ** -0.5) \
            + jnp.moveaxis(bias, -1, -2)
        logits = jnp.where(valid[:, :, None, :], logits, -jnp.inf)
        p = jax.nn.softmax(logits, axis=-1).astype(v.dtype)
        return jnp.einsum('bthk,btkhd->bthd', p, vs)

    out = lax.map(one_block, (blocks(q), blocks(iq), blocks(iw), qpos))
    return jnp.moveaxis(out, 0, 1).reshape(b, s, h, dh)


def hybrid_layer(x, norm_g, w_in, ml_conv_w, ml_conv_b, ml_b_i, ml_b_f, ml_norm_g,
                 gla_w_a, gla_b_a, gla_norm_g, dsa_q_g, dsa_k_g, w_out, rel_bias):
    b, s, _ = x.shape
    dt = x.dtype
    p = split_columns(rmsnorm(x, norm_g) @ w_in)
    qk = jax.nn.silu(causal_dwconv(jnp.concatenate([p['ml_q'], p['ml_k']], axis=-1), ml_conv_w, ml_conv_b))
    mq, mk = jnp.split(qk, 2, axis=-1)
    li = (p['ml_i'] + ml_b_i).astype(jnp.float32)
    lf = jax.nn.log_sigmoid((p['ml_f'] + ml_b_f).astype(jnp.float32))
    hm = mlstm_chunkwise(heads(mq, ML_HEADS), heads(mk, ML_HEADS), heads(p['ml_v'], ML_HEADS), li, lf).astype(dt)
    hm = hm * jax.nn.sigmoid(heads(p['ml_o'], ML_HEADS))
    y_ml = rmsnorm(hm, ml_norm_g.reshape(ML_HEADS, ML_HD)).reshape(b, s, ML_W) * jax.nn.silu(p['ml_z'])
    la = jax.nn.log_sigmoid((p['gla_a'] @ gla_w_a + gla_b_a).astype(jnp.float32)) / GLA_TAU
    hg = gla_chunked(heads(p['gla_q'], GLA_HEADS), heads(p['gla_k'], GLA_HEADS),
                     heads(p['gla_v'], GLA_HEADS), heads(la, GLA_HEADS)).astype(dt)
    y_gla = rmsnorm(hg, gla_norm_g.reshape(GLA_HEADS, GLA_DV)).reshape(b, s, GLA_W) * jax.nn.silu(p['gla_r'])
    dq = rmsnorm(heads(p['dsa_q'], DSA_HEADS), dsa_q_g)
    dk = rmsnorm(heads(p['dsa_k'], DSA_HEADS), dsa_k_g)
    iq = heads(p['idx_q'], IDX_HEADS) * (IDX_DIM ** -0.5)
    iw = p['idx_w'] * (IDX_HEADS ** -0.5)
    topk = min(TOPK_MAX, s // 4)
    hd = dsa_sparse_attention(dq, dk, heads(p['dsa_v'], DSA_HEADS), iq, p['idx_k'], iw, rel_bias, topk)
    y_dsa = hd.reshape(b, s, DSA_W) * jax.nn.silu(p['dsa_z'])
    y = jnp.concatenate([y_ml, y_gla, y_dsa], axis=-1) @ w_out
    return x + y


def setup_inputs(seed: int = 0) -> dict:
    key = jax.random.key(seed)
    ks = jax.random.split(key, 16)
    f32 = jnp.float32

    def nrm(k, shape, scale):
        return jax.random.normal(k, shape, f32) * scale

    x = nrm(ks[0], (BATCH, SEQ, D_MODEL), 1.0)
    norm_g = 1.0 + nrm(ks[1], (DEPTH, D_MODEL), 0.02)
    w_in = nrm(ks[2], (DEPTH, D_MODEL, N_IN), D_MODEL ** -0.5)
    ml_conv_w = nrm(ks[3], (DEPTH, ML_CONV, 2 * ML_W), ML_CONV ** -0.5)
    ml_conv_b = nrm(ks[4], (DEPTH, 2 * ML_W), 0.02)
    ml_b_i = nrm(ks[5], (DEPTH, ML_HEADS), 0.1)
    ml_b_f = jnp.linspace(3.0, 6.0, ML_HEADS, dtype=f32)[None, :] + nrm(ks[6], (DEPTH, ML_HEADS), 0.1)
    ml_norm_g = 1.0 + nrm(ks[7], (DEPTH, ML_W), 0.02)
    gla_w_a = nrm(ks[8], (DEPTH, GLA_RANK, GLA_WK), GLA_RANK ** -0.5)
    gla_b_a = nrm(ks[9], (DEPTH, GLA_WK), 0.02)
    gla_norm_g = 1.0 + nrm(ks[10], (DEPTH, GLA_W), 0.02)
    dsa_q_g = 1.0 + nrm(ks[11], (DEPTH, DSA_HD), 0.02)
    dsa_k_g = 1.0 + nrm(ks[12], (DEPTH, DSA_HD), 0.02)
    w_out = nrm(ks[13], (DEPTH, D_MIX, D_MODEL), D_MIX ** -0.5)
    rel_bias = nrm(ks[14], (REL_BUCKETS, DSA_HEADS), 0.2)
    return {'x': x, 'norm_g': norm_g, 'w_in': w_in, 'ml_conv_w': ml_conv_w, 'ml_conv_b': ml_conv_b,
            'ml_b_i': ml_b_i, 'ml_b_f': ml_b_f, 'ml_norm_g': ml_norm_g, 'gla_w_a': gla_w_a,
            'gla_b_a': gla_b_a, 'gla_norm_g': gla_norm_g, 'dsa_q_g': dsa_q_g, 'dsa_k_g': dsa_k_g,
            'w_out': w_out, 'rel_bias': rel_bias}


def reference(x, norm_g, w_in, ml_conv_w, ml_conv_b, ml_b_i, ml_b_f, ml_norm_g,
              gla_w_a, gla_b_a, gla_norm_g, dsa_q_g, dsa_k_g, w_out, rel_bias):
    for layer in range(DEPTH):
        x = hybrid_layer(x, norm_g[layer], w_in[layer], ml_conv_w[layer], ml_conv_b[layer],
                         ml_b_i[layer], ml_b_f[layer], ml_norm_g[layer], gla_w_a[layer],
                         gla_b_a[layer], gla_norm_g[layer], dsa_q_g[layer], dsa_k_g[layer],
                         w_out[layer], rel_bias)
    return x
```

```python
import numpy as np
import ml_dtypes
from contextlib import ExitStack
import concourse.bass as bass
import concourse.mybir as mybir
from concourse.bass_utils import run_bass_kernel_spmd

F32 = mybir.dt.float32
BF16 = mybir.dt.bfloat16
AF = mybir.ActivationFunctionType
ALU = mybir.AluOpType
AX = mybir.AxisListType


class Prog:
    ENG = ['pe', 'act', 'dve', 'pool', 'sp']

    def __init__(self, nc, es, n_dma_sems=40):
        self.nc = nc
        self.es = es
        self.es0 = es
        self.sem = {e: es.enter_context(nc.semaphore('s_' + e)) for e in self.ENG}
        self.dsem = [es.enter_context(nc.semaphore('d%d' % i)) for i in range(n_dma_sems)]
        self.cnt = {e: 0 for e in self.ENG}
        self.dcnt = 0
        self.dval = [0] * n_dma_sems
        self.waited = {e: {} for e in self.ENG}
        self.ops = {e: [] for e in self.ENG}
        self.lastw = {}
        self.readers = {}
        self.nops = 0
        self._e_pe = nc.tensor
        self._e_act = nc.scalar
        self._e_dve = nc.vector
        self._e_pool = nc.gpsimd
        self._e_sp = nc.sync

    def sbuf(self, name, shape, dtype):
        self.uid = getattr(self, 'uid', 0) + 1
        return self.es.enter_context(self.nc.sbuf_tensor("sb%d_%s" % (self.uid, name), shape, dtype))

    def psum(self, name, shape, dtype):
        self.uid = getattr(self, 'uid', 0) + 1
        return self.es.enter_context(self.nc.psum_tensor("ps%d_%s" % (self.uid, name), shape, dtype))

    def _wait(self, eng, tok):
        if tok is None:
            return
        kind, who, val = tok
        if kind == 'e' and who == 'pe' and eng == 'pe':
            return
        key = (kind, who)
        if self.waited[eng].get(key, 0) >= val:
            return
        self.waited[eng][key] = val
        sem = self.sem[who] if kind == 'e' else self.dsem[who]
        getattr(self, '_e_' + eng).wait_ge(sem, val)

    def _deps(self, eng, reads, writes):
        for k in reads:
            self._wait(eng, self.lastw.get(k))
        for k in writes:
            self._wait(eng, self.lastw.get(k))
            for t in self.readers.get(k, ()):
                self._wait(eng, t)

    def _commit(self, tok, reads, writes):
        for k in writes:
            self.lastw[k] = tok
            self.readers[k] = []
        for k in reads:
            if k in writes:
                continue
            self.readers.setdefault(k, []).append(tok)

    def op(self, eng, fn, reads=(), writes=()):
        self._deps(eng, reads, writes)
        self.cnt[eng] += 1
        tok = ('e', eng, self.cnt[eng])
        fn(getattr(self, '_e_' + eng)).then_inc(self.sem[eng], 1)
        self._commit(tok, reads, writes)
        self.nops += 1

    def dma(self, q, out, in_, reads=(), writes=(), **kw):
        n = len(self.dsem)
        idx = self.dcnt % n
        self.dcnt += 1
        if self.dval[idx] > 0:
            self._wait(q, ('d', idx, self.dval[idx]))
        self._deps(q, reads, writes)
        self.dval[idx] += 16
        tok = ('d', idx, self.dval[idx])
        getattr(self, '_e_' + q).dma_start(out=out, in_=in_, **kw).then_inc(self.dsem[idx], 16)
        self._commit(tok, reads, writes)
        self.nops += 1

    def finish(self):
        for idx, v in enumerate(self.dval):
            if v > 0:
                self._wait('sp', ('d', idx, v))
        for e in ['pe', 'act', 'dve', 'pool']:
            if self.cnt[e] > 0:
                self._wait('sp', ('e', e, self.cnt[e]))

    def barrier(self):
        for e in self.ENG:
            for o in ['pe', 'act', 'dve', 'pool']:
                if self.cnt[o] > 0 and not (e == o):
                    self._wait(e, ('e', o, self.cnt[o]))
            for idx, v in enumerate(self.dval):
                if v > 0:
                    self._wait(e, ('d', idx, v))
        if not hasattr(self, 'bar'):
            self.bar = self.es0.enter_context(self.nc.semaphore('s_bar'))
            self.go = self.es0.enter_context(self.nc.semaphore('s_go'))
            self.epoch = 0
        self.epoch += 1
        comp = ['pe', 'act', 'dve', 'pool']
        for e in comp:
            getattr(self, '_e_' + e).sem_inc(self.bar, 1)
        sp = self._e_sp
        sp.wait_ge(self.bar, 4 * self.epoch)
        for e in comp:
            sp.sem_clear(self.sem[e])
        for d in self.dsem:
            sp.sem_clear(d)
        sp.sem_inc(self.go, 1)
        for e in comp:
            getattr(self, '_e_' + e).wait_ge(self.go, self.epoch)
        for e in comp:
            self.cnt[e] = 0
        self.dval = [0] * len(self.dsem)
        self.waited = {e: {} for e in self.ENG}
        self.lastw = {}
        self.readers = {}


C_MLQ, C_MLK, C_MLV, C_MLO, C_MLZ = 0, 1024, 2048, 3072, 4096
C_MLI, C_MLF = 5120, 5124
C_GQ, C_GK, C_GV, C_GA, C_GR = 5128, 5384, 5640, 6152, 6168
C_DQ, C_DK, C_DV, C_DZ = 6680, 7192, 7704, 8216
C_IQ, C_IK, C_IW = 8728, 9240, 9304
N_IN = 9312
EPS = 1e-6
NBIS = 24
NEG = -30000.0

CF_ID, CF_ONES, CF_MASK, CF_PW, CF_MISC = 0, 128, 256, 384, 416
NCF = 432
CB_ID, CB_MASK4 = 0, 128
NCB = 640


def t5_bucket_np(rel):
    half, max_exact = 16, 8
    ret = np.where(rel > 0, half, 0)
    n = np.abs(rel)
    nf = np.maximum(n, 1).astype(np.float32)
    large = max_exact + (np.log(nf / max_exact) / np.float32(np.log(128 / max_exact)) * (half - max_exact)).astype(np.int32)
    large = np.minimum(large, half - 1)
    return ret + np.where(n < max_exact, n, large)


def host_consts():
    cf = np.zeros((128, NCF), np.float32)
    cf[:, CF_ID:CF_ID + 128] = np.eye(128, dtype=np.float32)
    cf[:, CF_ONES:CF_ONES + 128] = 1.0
    s = np.arange(128)
    maskT = (s[:, None] <= s[None, :]).astype(np.float32)
    cf[:, CF_MASK:CF_MASK + 128] = maskT
    cf[:, CF_PW:CF_PW + 32] = (0.5 ** (np.arange(32) + 1))[None, :]
    cf[:, CF_MISC + 0] = EPS
    cf[:, CF_MISC + 1] = 1.0
    cf[:, CF_MISC + 2] = 0.0
    cb = np.zeros((128, NCB), np.float32)
    cb[:, CB_ID:CB_ID + 128] = np.eye(128)
    cb[:, CB_MASK4:CB_MASK4 + 512] = np.tile(maskT, (1, 4))
    k = np.arange(128)[:, None]
    q = np.arange(128)[None, :]
    oh = []
    ohidx = []
    for r in (0, 1):
        b = t5_bucket_np((k - q - 128 * r).astype(np.int32))
        for bb in np.unique(b):
            oh.append((b == bb).astype(np.float32))
            ohidx.append((r, int(bb)))
    oh = np.stack(oh, 1)
    return cf, cb.astype(ml_dtypes.bfloat16), oh.astype(ml_dtypes.bfloat16), ohidx


_OHIDX = host_consts()[3]


class K:
    pass


def build(S, depth=2, debug=False, stop_after=None):
    NT = S // 128
    NB = S // 512
    TOPK = min(256, S // 4)
    nc = bass.Bass("TRN2", target_bir_lowering=False)

    def din(name, shape, dt=F32):
        return nc.dram_tensor(name, list(shape), dt, kind="ExternalInput").ap()

    def dscr(name, shape, dt=F32):
        return nc.dram_tensor(name, list(shape), dt, kind=("ExternalOutput" if debug else "Internal")).ap()

    x_in = din("x", [S, 1024])
    w_in = din("w_in", [2, 1024, N_IN])
    w_out = din("w_out", [2, 2048, 1024])
    normB = din("normB", [2, 128, 1024])
    convwT = din("convwT", [2, 2048, 5])
    ml_b_i = din("ml_b_i", [2, 4, 1])
    ml_b_f = din("ml_b_f", [2, 4, 1])
    mlnB = din("mlnB", [2, 128, 1024])
    gla_w_a = din("gla_w_a", [2, 16, 256])
    gla_b_aT = din("gla_b_aT", [2, 64, 4])
    glnB = din("glnB", [2, 128, 512])
    dsa_q_g = din("dsa_q_g", [2, 128, 1])
    dsa_k_g = din("dsa_k_g", [2, 128, 1])
    relB = din("relB", [128, 128])
    cf_in = din("cf", [128, NCF])
    cb_in = din("cb", [128, NCB], BF16)
    oh_in = din("oh", [128, len(_OHIDX), 128], BF16)
    out = nc.dram_tensor("out", [S, 1024], F32, kind="ExternalOutput").ap()

    h1 = dscr("h1", [S, 1024])
    qkT = dscr("qkT", [2048, S], BF16)
    vml = dscr("vml", [S, 1024], BF16)
    sigo = dscr("sigo", [S, 1024], BF16)
    zml = dscr("zml", [S, 1024], BF16)
    liT = dscr("liT", [4, S])
    lfT = dscr("lfT", [4, S])
    gqT = dscr("gqT", [4, 64, S])
    gkT = dscr("gkT", [4, 64, S])
    laT = dscr("laT", [4, 64, S])
    vg = dscr("vg", [S, 512], BF16)
    rg = dscr("rg", [S, 512], BF16)
    QhT = dscr("QhT", [4, 128, S], BF16)
    KhT = dscr("KhT", [4, 128, S], BF16)
    vd = dscr("vd", [S, 512], BF16)
    zd = dscr("zd", [S, 512], BF16)
    iqT = dscr("iqT", [512, S], BF16)
    ikT2 = dscr("ikT2", [128, S], BF16)
    iwS = dscr("iw", [S, 8])
    MB = dscr("MB", [NT, 128, S], BF16)
    ycat = dscr("ycat", [S, 2048], BF16)

    es = ExitStack()
    P = Prog(nc, es)
    cf = P.sbuf("cf", [128, NCF], F32)
    cb = P.sbuf("cb", [128, NCB], BF16)
    P.dma('sp', cf[:], cf_in, writes=['cf'])
    P.dma('sp', cb[:], cb_in, writes=['cb'])
    ident_f = cf[:, CF_ID:CF_ID + 128]
    ones_f = cf[:, CF_ONES:CF_ONES + 128]
    maskT_f = cf[:, CF_MASK:CF_MASK + 128]
    epsc = cf[:, CF_MISC:CF_MISC + 1]
    onec = cf[:, CF_MISC + 1:CF_MISC + 2]
    ident_b = cb[:, CB_ID:CB_ID + 128]
    mask4_b = cb[:, CB_MASK4:CB_MASK4 + 512]

    def act(outp, inp, func, r, w, **kw):
        P.op('act', lambda e: e.activation(outp, inp, func, **kw), reads=r, writes=w)

    def mm(outp, lhsT, rhs, start, stop, r, w):
        P.op('pe', lambda e: e.matmul(outp, lhsT, rhs, start=start, stop=stop), reads=r, writes=w)

    def phaseA(l, src):
        with ExitStack() as ph:
            P.es = ph
            xnT = P.sbuf("xnT", [128, 8, S], BF16)
            gB = P.sbuf("gB", [128, 1024], F32)
            P.dma('sp', gB[:], normB[l], writes=['gB'])
            with ExitStack() as ph1:
                P.es = ph1
                xts = [P.sbuf("xt%d" % i, [128, 1024], F32) for i in range(2)]
                xns = [P.sbuf("xn%d" % i, [128, 1024], BF16) for i in range(2)]
                junk = P.sbuf("junkA", [128, 1024], BF16)
                ss = [P.sbuf("ssA%d" % i, [128, 1], F32) for i in range(2)]
                tps = [P.psum("tpA%d" % i, [128, 8, 128], BF16) for i in range(2)]
                for t in range(NT):
                    j = t % 2
                    xt, xn, tp, s1 = xts[j], xns[j], tps[j], ss[j]
                    P.dma('sp', xt[:], src[t * 128:(t + 1) * 128, :], writes=[('xt', j)])
                    P.op('pool', lambda e: e.memset(s1[:], 0.0), writes=[('ss', j)])
                    act(junk[:], xt[:], AF.Square, [('xt', j)], ['junkA', ('ss', j)], accum_out=s1[:, 0:1])
                    act(s1[:], s1[:], AF.Ln, [('ss', j), 'cf'], [('ss', j)], scale=1.0 / 1024, bias=epsc)
                    act(s1[:], s1[:], AF.Exp, [('ss', j)], [('ss', j)], scale=-0.5)
                    P.op('dve', lambda e: e.scalar_tensor_tensor(xn[:], xt[:], s1[:, 0:1], gB[:], ALU.mult, ALU.mult),
                         reads=[('xt', j), ('ss', j), 'gB'], writes=[('xn', j)])
                    for c in range(8):
                        P.op('pe', lambda e: e.transpose(tp[:, c, :], xn[:, c * 128:(c + 1) * 128], ident_b),
                             reads=[('xn', j), 'cb'], writes=[('tp', j)])
                    P.op('dve' if j else 'act', (lambda e: e.tensor_copy(xnT[:, :, t * 128:(t + 1) * 128], tp[:])) if j else
                         (lambda e: e.copy(xnT[:, :, t * 128:(t + 1) * 128], tp[:])),
                         reads=[('tp', j)], writes=[('xnT', t // 4)])
            P.barrier()
            P.es = ph
            wf = P.sbuf("wf", [128, 8, 512], F32)
            wbs = [P.sbuf("wb%d" % i, [128, 8, 512], BF16) for i in range(2)]
            sts = [P.sbuf("st%d" % i, [128, 515], F32) for i in range(2)]
            accs = [P.sbuf("acc%d" % i, [128, 512], F32) for i in range(2)]
            obf = [P.sbuf("obf%d" % i, [128, 512], F32) for i in range(3)]
            obb = [P.sbuf("obb%d" % i, [128, 512], BF16) for i in range(3)]
            cws = [P.sbuf("cw%d" % i, [128, 5], F32) for i in range(2)]
            smallp = P.sbuf("smallp", [128, 16], F32)
            wab_f = P.sbuf("wab_f", [16, 256], F32)
            wab = P.sbuf("wab", [16, 256], BF16)
            aTb = [P.sbuf("aTb%d" % i, [16, 512], BF16) for i in range(2)]
            pas = [P.psum("paA%d" % i, [128, 512], F32) for i in range(4)]
            pzs = [P.psum("pzA%d" % i, [128, 512], F32) for i in range(2)]
            wl = w_in[l].rearrange("(c p) n -> p c n", p=128)
            P.dma('sp', smallp[:, 0:1], dsa_q_g[l], writes=['smallp'])
            P.dma('sp', smallp[:, 1:2], dsa_k_g[l], writes=['smallp'])
            P.dma('sp', smallp[0:4, 2:3], ml_b_f[l], writes=['smallp'])
            P.dma('sp', smallp[0:4, 3:4], ml_b_i[l], writes=['smallp'])
            P.dma('sp', smallp[0:64, 4:8], gla_b_aT[l], writes=['smallp'])
            P.dma('sp', wab_f[:], gla_w_a[l], writes=['wab_f'])
            P.op('dve', lambda e: e.tensor_scalar(smallp[:, 0:1], smallp[:, 0:1], 128.0 ** -0.5, None, ALU.mult), reads=['smallp'], writes=['smallp'])
            P.op('dve', lambda e: e.tensor_scalar(smallp[0:4, 2:3], smallp[0:4, 2:3], -1.0, None, ALU.mult), reads=['smallp'], writes=['smallp'])
            P.op('dve', lambda e: e.tensor_scalar(smallp[0:64, 4:8], smallp[0:64, 4:8], -1.0, None, ALU.mult), reads=['smallp'], writes=['smallp'])
            P.op('dve', lambda e: e.tensor_copy(wab[:], wab_f[:]), reads=['wab_f'], writes=['wab'])

            fm = []
            for g in range(8):
                fm.append(('mlq', C_MLQ + g * 128, 128, g))
            for g in range(8):
                fm.append(('mlk', C_MLK + g * 128, 128, 8 + g))
            for h in range(4):
                fm.append(('glaq', C_GQ + h * 64, 64, h))
            for h in range(4):
                fm.append(('glak', C_GK + h * 64, 64, h))
            fm.append(('glaa', C_GA, 16, 0))
            for h in range(4):
                fm.append(('dsaq', C_DQ + h * 128, 128, h))
            for h in range(4):
                fm.append(('dsak', C_DK + h * 128, 128, h))
            for g in range(4):
                fm.append(('idxq', C_IQ + g * 128, 128, g))
            fm.append(('idxk', C_IK, 64, 0))
            fm.append(('mli', C_MLI, 4, 0))
            fm.append(('mlf', C_MLF, 4, 0))
            tm = [('mlv', C_MLV, 512, vml, 0), ('mlv', C_MLV + 512, 512, vml, 512),
                  ('mlo', C_MLO, 512, sigo, 0), ('mlo', C_MLO + 512, 512, sigo, 512),
                  ('mlz', C_MLZ, 512, zml, 0), ('mlz', C_MLZ + 512, 512, zml, 512),
                  ('glav', C_GV, 512, vg, 0), ('glar', C_GR, 512, rg, 0),
                  ('dsav', C_DV, 512, vd, 0), ('dsaz', C_DZ, 512, zd, 0), ('idxw', C_IW, 8, iwS, 0)]
            groups = [('fm',) + g for g in fm] + [('tm',) + g for g in tm]
            cnt = {'ev': 0, 'ob': 0, 'pa': 0, 'pz': 0, 'cw': 0, 'at': 0}

            def load_w(gi):
                g = groups[gi]
                c0, n = g[2], g[3]
                if g[1] == 'idxk':
                    P.dma('sp', wf[:, :, 0:64], wl[:, :, c0:c0 + 64], writes=['wf'])
                    P.dma('sp', wf[:, :, 64:128], wl[:, :, c0:c0 + 64], writes=['wf'])
                    n = 128
                else:
                    P.dma('sp', wf[:, :, 0:n], wl[:, :, c0:c0 + n], writes=['wf'])
                wb = wbs[gi % 2]
                P.op('pool', lambda e: e.tensor_copy(wb[:, :, 0:n], wf[:, :, 0:n]), reads=['wf'], writes=[('wb', gi % 2)])

            def next_ob(kind):
                i = cnt['ob'] % 3
                cnt['ob'] += 1
                return (obf if kind == 'f' else obb)[i], ('obf' if kind == 'f' else 'obb', i)

            def plain_evac(dst, srcp, r, w, scale=None):
                i = cnt['ev']
                cnt['ev'] += 1
                if scale is not None:
                    if i % 2:
                        P.op('act', lambda e: e.mul(dst, srcp, scale), reads=r, writes=w)
                    else:
                        P.op('dve', lambda e: e.tensor_scalar(dst, srcp, scale, None, ALU.mult), reads=r, writes=w)
                else:
                    if i % 2:
                        P.op('act', lambda e: e.copy(dst, srcp), reads=r, writes=w)
                    else:
                        P.op('dve', lambda e: e.tensor_copy(dst, srcp), reads=r, writes=w)

            def do_fm(gi):
                _, kind, c0, n, idx = groups[gi]
                wb = wbs[gi % 2]
                wk = ('wb', gi % 2)
                M = 128 if kind == 'idxk' else n
                if kind in ('mlq', 'mlk'):
                    cw = cws[cnt['cw'] % 2]
                    cwk = ('cw', cnt['cw'] % 2)
                    cnt['cw'] += 1
                    P.dma('sp', cw[:], convwT[l][idx * 128:(idx + 1) * 128, :], writes=[cwk])
                for tb in range(NB):
                    pi = cnt['pa'] % 4
                    cnt['pa'] += 1
                    pa = pas[pi]
                    pk = ('pa', pi)
                    for c in range(8):
                        mm(pa[0:M, :], wb[:, c, 0:M], xnT[:, c, tb * 512:(tb + 1) * 512], c == 0, c == 7,
                           [wk, ('xnT', tb)], [pk])
                    tsl = slice(tb * 512, (tb + 1) * 512)
                    if kind in ('mlq', 'mlk'):
                        st, stk = sts[tb % 2], ('st', tb % 2)
                        pst, pstk = sts[(tb + 1) % 2], ('st', (tb + 1) % 2)
                        acc, acck = accs[tb % 2], ('acc', tb % 2)
                        P.op('act', lambda e: e.copy(st[:, 3:515], pa[:, :]), reads=[pk], writes=[stk])
                        if tb == 0:
                            P.op('pool', lambda e: e.memset(st[:, 0:3], 0.0), writes=[stk])
                        else:
                            P.op('pool', lambda e: e.tensor_copy(st[:, 0:3], pst[:, 512:515]), reads=[pstk], writes=[stk])
                        P.op('dve', lambda e: e.tensor_scalar(acc[:], st[:, 0:512], cw[:, 0:1], cw[:, 4:5], ALU.mult, ALU.add),
                             reads=[stk, cwk], writes=[acck])
                        for j in (1, 2, 3):
                            P.op('dve', lambda e: e.scalar_tensor_tensor(acc[:], st[:, j:j + 512], cw[:, j:j + 1], acc[:], ALU.mult, ALU.add),
                                 reads=[stk, cwk, acck], writes=[acck])
                        ob, obk = next_ob('b')
                        act(ob[:], acc[:], AF.Silu, [acck], [obk])
                        P.dma('sp', qkT[idx * 128:(idx + 1) * 128, tsl], ob[:], reads=[obk], writes=[('qkT', idx, tb)])
                    elif kind == 'glaq':
                        ob, obk = next_ob('f')
                        plain_evac(ob[0:64, :], pa[0:64, :], [pk], [obk], scale=0.125)
                        P.dma('sp', gqT[idx, :, tsl], ob[0:64, :], reads=[obk], writes=[('gqT', idx, tb)])
                    elif kind == 'glak':
                        ob, obk = next_ob('f')
                        plain_evac(ob[0:64, :], pa[0:64, :], [pk], [obk])
                        P.dma('sp', gkT[idx, :, tsl], ob[0:64, :], reads=[obk], writes=[('gkT', idx, tb)])
                    elif kind == 'glaa':
                        at = aTb[cnt['at'] % 2]
                        atk = ('aTb', cnt['at'] % 2)
                        cnt['at'] += 1
                        P.op('act', lambda e: e.copy(at[:, :], pa[0:16, :]), reads=[pk], writes=[atk])
                        for h in range(4):
                            zi = cnt['pz'] % 2
                            cnt['pz'] += 1
                            pz, pzk = pzs[zi], ('pz', zi)
                            mm(pz[0:64, :], wab[:, h * 64:(h + 1) * 64], at[:, :], True, True, ['wab', atk], [pzk])
                            ob, obk = next_ob('f')
                            act(ob[0:64, :], pz[0:64, :], AF.Exp, [pzk, 'smallp'], [obk], scale=-1.0, bias=smallp[0:64, 4 + h:5 + h])
                            act(ob[0:64, :], ob[0:64, :], AF.Ln, [obk, 'cf'], [obk], bias=onec[0:64, :])
                            P.op('dve', lambda e: e.tensor_scalar(ob[0:64, :], ob[0:64, :], -1.0 / 16.0, None, ALU.mult), reads=[obk], writes=[obk])
                            P.dma('sp', laT[h, :, tsl], ob[0:64, :], reads=[obk], writes=[('laT', h, tb)])
                    elif kind in ('dsaq', 'dsak'):
                        sq, sqk = next_ob('f')
                        act(sq[:], pa[:], AF.Square, [pk], [sqk])
                        zi = cnt['pz'] % 2
                        cnt['pz'] += 1
                        pz, pzk = pzs[zi], ('pz', zi)
                        mm(pz[:, :], ones_f, sq[:], True, True, ['cf', sqk], [pzk])
                        act(sq[:], pz[:], AF.Ln, [pzk, 'cf'], [sqk], scale=1.0 / 128, bias=epsc)
                        act(sq[:], sq[:], AF.Exp, [sqk], [sqk], scale=-0.5)
                        ob, obk = next_ob('b')
                        gcol = 0 if kind == 'dsaq' else 1
                        P.op('dve', lambda e: e.scalar_tensor_tensor(ob[:], pa[:], smallp[:, gcol:gcol + 1], sq[:], ALU.mult, ALU.mult),
                             reads=[pk, 'smallp', sqk], writes=[obk])
                        dstT = QhT if kind == 'dsaq' else KhT
                        P.dma('sp', dstT[idx, :, tsl], ob[:], reads=[obk], writes=[(kind, idx, tb)])
                    elif kind == 'idxq':
                        ob, obk = next_ob('b')
                        plain_evac(ob[:], pa[:], [pk], [obk], scale=0.125)
                        P.dma('sp', iqT[idx * 128:(idx + 1) * 128, tsl], ob[:], reads=[obk], writes=[('iqT', idx, tb)])
                    elif kind == 'idxk':
                        ob, obk = next_ob('b')
                        plain_evac(ob[:], pa[:], [pk], [obk])
                        P.dma('sp', ikT2[:, tsl], ob[:], reads=[obk], writes=[('ikT2', tb)])
                    elif kind == 'mli':
                        ob, obk = next_ob('f')
                        P.op('dve', lambda e: e.tensor_scalar(ob[0:4, :], pa[0:4, :], smallp[0:4, 3:4], None, ALU.add), reads=[pk, 'smallp'], writes=[obk])
                        P.dma('sp', liT[:, tsl], ob[0:4, :], reads=[obk], writes=[('liT', tb)])
                    elif kind == 'mlf':
                        ob, obk = next_ob('f')
                        act(ob[0:4, :], pa[0:4, :], AF.Exp, [pk, 'smallp'], [obk], scale=-1.0, bias=smallp[0:4, 2:3])
                        act(ob[0:4, :], ob[0:4, :], AF.Ln, [obk, 'cf'], [obk], bias=onec[0:4, :])
                        P.op('dve', lambda e: e.tensor_scalar(ob[0:4, :], ob[0:4, :], -1.0, None, ALU.mult), reads=[obk], writes=[obk])
                        P.dma('sp', lfT[:, tsl], ob[0:4, :], reads=[obk], writes=[('lfT', tb)])

            def do_tm(gi):
                _, kind, c0, n, dst, off = groups[gi]
                wb = wbs[gi % 2]
                wk = ('wb', gi % 2)
                for t in range(NT):
                    pi = cnt['pa'] % 4
                    cnt['pa'] += 1
                    pa, pk = pas[pi], ('pa', pi)
                    for c in range(8):
                        mm(pa[:, 0:n], xnT[:, c, t * 128:(t + 1) * 128], wb[:, c, 0:n], c == 0, c == 7,
                           [wk, ('xnT', t // 4)], [pk])
                    rows = slice(t * 128, (t + 1) * 128)
                    if kind == 'idxw':
                        ob, obk = next_ob('f')
                        plain_evac(ob[:, 0:8], pa[:, 0:8], [pk], [obk], scale=8.0 ** -0.5)
                        P.dma('sp', dst[rows, :], ob[:, 0:8], reads=[obk], writes=[(kind, t)])
                        continue
                    ob, obk = next_ob('b')
                    if kind in ('mlv', 'glav', 'dsav'):
                        plain_evac(ob[:], pa[:], [pk], [obk])
                    elif kind == 'mlo':
                        act(ob[:], pa[:], AF.Sigmoid, [pk], [obk])
                    else:
                        act(ob[:], pa[:], AF.Silu, [pk], [obk])
                    P.dma('sp', dst[rows, off:off + 512], ob[:], reads=[obk], writes=[(kind, off, t)])

            load_w(0)
            for gi in range(len(groups)):
                if gi + 1 < len(groups):
                    load_w(gi + 1)
                if groups[gi][0] == 'fm':
                    do_fm(gi)
                else:
                    do_tm(gi)
            P.barrier()
        P.es = es

    def phaseB(l):
        with ExitStack() as ph:
            P.es = ph
            Cn = P.sbuf("Cn", [128, 4, 2, 257], F32)
            CnS = P.sbuf("CnS", [128, 4, 2, 257], BF16)
            mst = [P.sbuf("mst%d" % i, [4, 1], F32) for i in range(2)]
            gmB = P.sbuf("gmB", [128, 1024], F32)
            q4s = [P.sbuf("q4_%d" % i, [128, 8, 128], BF16) for i in range(2)]
            k4s = [P.sbuf("k4_%d" % i, [128, 8, 128], BF16) for i in range(2)]
            v1s = [P.sbuf("v1_%d" % i, [128, 4, 257], BF16) for i in range(2)]
            sgs = [P.sbuf("sg%d" % i, [128, 1024], BF16) for i in range(2)]
            zms = [P.sbuf("zm%d" % i, [128, 1024], BF16) for i in range(2)]
            glis = [P.sbuf("gli%d" % i, [4, 128], F32) for i in range(2)]
            glfs = [P.sbuf("glf%d" % i, [4, 128], F32) for i in range(2)]
            bc = P.sbuf("bcB", [4, 128], F32)
            aa = P.sbuf("aaB", [4, 128], F32)
            waT = P.sbuf("waT", [4, 128], F32)
            clT = P.sbuf("clT", [4, 128], F32)
            sm = P.sbuf("smB", [4, 8], F32)
            diagE = P.sbuf("diagE", [4, 4], F32)
            gt = P.sbuf("gtB", [128, 12], F32)
            sw = P.sbuf("swB", [128, 4, 128], BF16)
            kw = P.sbuf("kwB", [128, 4, 2, 128], BF16)
            hmo = P.sbuf("hmo", [128, 4, 256], F32)
            junk = P.sbuf("junkB", [128, 256], BF16)
            sml = P.sbuf("smlB", [128, 16], F32)
            Gz = P.sbuf("Gz", [128, 1024], F32)
            yts = [P.sbuf("ytB%d" % i, [128, 1024], BF16) for i in range(2)]
            g_ps = P.psum("g_ps", [128, 12], F32)
            st_ps = P.psum("st_ps", [128, 4, 128], F32)
            kt_ps = P.psum("kt_ps", [128, 8, 128], BF16)
            nds = [P.psum("nd%d" % i, [128, 257], F32) for i in range(2)]
            cus = [P.psum("cu%d" % i, [128, 257], F32) for i in range(2)]
            P.dma('sp', gmB[:], mlnB[l], writes=['gmB'])
            P.op('pool', lambda e: e.memset(Cn[:], 0.0), writes=['Cn'])
            P.op('pool', lambda e: e.memset(mst[0][:], 0.0), writes=[('mst', 0)])
            for i in range(2):
                P.op('pool', lambda e: e.memset(v1s[i][:, :, 256:257], 1.0), writes=[('v1', i)])
            qv = qkT[0:1024, :].rearrange("(g p) s -> p g s", p=128)
            kv = qkT[1024:2048, :].rearrange("(g p) s -> p g s", p=128)

            def loads(t):
                j = t % 2
                cols = slice(t * 128, (t + 1) * 128)
                P.dma('sp', q4s[j][:], qv[:, :, cols], writes=[('q4', j)])
                P.dma('sp', k4s[j][:], kv[:, :, cols], writes=[('k4', j)])
                P.dma('sp', v1s[j][:, :, 0:256], vml[cols, :].rearrange("p (h e) -> p h e", h=4), writes=[('v1', j)])
                P.dma('sp', sgs[j][:], sigo[cols, :], writes=[('sg', j)])
                P.dma('sp', zms[j][:], zml[cols, :], writes=[('zm', j)])
                P.dma('sp', glis[j][:], liT[:, cols], writes=[('gli', j)])
                P.dma('sp', glfs[j][:], lfT[:, cols], writes=[('glf', j)])

            loads(0)
            LN16 = float(np.log(16.0))
            for t in range(NT):
                j = t % 2
                if t + 1 < NT:
                    loads(t + 1)
                q4, k4, v1, sg, zm, gli, glf, yt = q4s[j], k4s[j], v1s[j], sgs[j], zms[j], glis[j], glfs[j], yts[j]
                mprev, mnext = mst[j], mst[1 - j]
                mk_, mnk = ('mst', j), ('mst', 1 - j)
                P.op('dve', lambda e: e.tensor_tensor_scan(bc[:], ones_f[0:4, 0:128], glf[:], 0.0, ALU.mult, ALU.add),
                     reads=['cf', ('glf', j)], writes=['bcB'])
                P.op('dve', lambda e: e.tensor_tensor(aa[:], gli[:], bc[:], ALU.subtract), reads=[('gli', j), 'bcB'], writes=['aaB'])
                P.op('dve', lambda e: e.tensor_reduce(sm[:, 0:1], aa[:], AX.X, ALU.max), reads=['aaB'], writes=['sm0'])
                P.op('dve', lambda e: e.tensor_tensor(sm[:, 1:2], sm[:, 0:1], mprev[:], ALU.max), reads=['sm0', mk_], writes=['sm1'])
                P.op('dve', lambda e: e.tensor_scalar(sm[:, 2:3], sm[:, 1:2], -1.0, None, ALU.mult), reads=['sm1'], writes=['sm2'])
                P.op('dve', lambda e: e.tensor_scalar(sm[:, 3:4], sm[:, 1:2], -1.0, -LN16, ALU.mult, ALU.add), reads=['sm1'], writes=['sm3'])
                act(sm[:, 4:5], mprev[:], AF.Exp, [mk_, 'sm2'], ['sm4'], bias=sm[:, 2:3])
                P.op('dve', lambda e: e.tensor_tensor(mnext[:], bc[:, 127:128], sm[:, 1:2], ALU.add), reads=['bcB', 'sm1'], writes=[mnk])
                act(waT[:], aa[:], AF.Exp, ['aaB', 'sm3'], ['waT'], bias=sm[:, 3:4])
                act(clT[:], bc[:], AF.Exp, ['bcB', 'sm2'], ['clT'], scale=-1.0, bias=sm[:, 2:3])
                P.op('dve', lambda e: e.tensor_scalar(diagE[:], ident_f[0:4, 0:4], sm[:, 4:5], None, ALU.mult), reads=['cf', 'sm4'], writes=['diagE'])
                mm(g_ps[:, 0:4], waT[:], ident_f[0:4, 0:4], True, True, ['waT', 'cf'], ['g_ps'])
                mm(g_ps[:, 4:8], clT[:], ident_f[0:4, 0:4], True, True, ['clT', 'cf'], ['g_ps'])
                mm(g_ps[:, 8:12], ones_f[0:4, 0:128], diagE[:], True, True, ['diagE', 'cf'], ['g_ps'])
                P.op('dve', lambda e: e.tensor_copy(gt[:], g_ps[:]), reads=['g_ps'], writes=['gt'])
                P.op('pool', lambda e: e.memset(sml[:, 0:4], 0.0), writes=['ssq'])
                for h in range(4):
                    P.op('act', lambda e: e.mul(CnS[:, h, :, :], Cn[:, h, :, :], gt[:, 8 + h:9 + h]), reads=['Cn', 'gt'], writes=[('CnS', h)])
                for h in range(4):
                    for dc in range(2):
                        mm(st_ps[:, h, :], k4[:, 2 * h + dc, :], q4[:, 2 * h + dc, :], dc == 0, dc == 1, [('k4', j), ('q4', j)], [('st_ps', h)])
                    P.op('dve', lambda e: e.scalar_tensor_tensor(sw[:, h, :], st_ps[:, h, :], gt[:, h:h + 1], maskT_f, ALU.mult, ALU.mult),
                         reads=[('st_ps', h), 'gt', 'cf'], writes=[('sw', h)])
                    for dc in range(2):
                        P.op('pe', lambda e: e.transpose(kt_ps[:, 2 * h + dc, :], k4[:, 2 * h + dc, :], ident_b), reads=[('k4', j), 'cb'], writes=[('kt_ps', h)])
                    P.op('act', lambda e: e.mul(kw[:, h, :, :], kt_ps[:, 2 * h:2 * h + 2, :], gt[:, h:h + 1]), reads=[('kt_ps', h), 'gt'], writes=[('kw', h)])
                    nd, ndk = nds[h % 2], ('nd', h % 2)
                    mm(nd[:, :], sw[:, h, :], v1[:, h, :], True, False, [('sw', h), ('v1', j)], [ndk])
                    for dc in range(2):
                        mm(nd[:, :], q4[:, 2 * h + dc, :], CnS[:, h, dc, :], False, dc == 1, [('q4', j), ('CnS', h)], [ndk])
                    for dc in range(2):
                        cu, cuk = cus[dc], ('cu', dc)
                        mm(cu[:, :], kw[:, h, dc, :], v1[:, h, :], True, True, [('kw', h), ('v1', j)], [cuk])
                        P.op('dve', lambda e: e.scalar_tensor_tensor(Cn[:, h, dc, :], Cn[:, h, dc, :], gt[:, 8 + h:9 + h], cu[:, :], ALU.mult, ALU.add),
                             reads=['Cn', 'gt', cuk, ('CnS', h)], writes=['Cn'])
                    act(sml[:, 4 + h:5 + h], nd[:, 256:257], AF.Abs, [ndk], [('dn', h)])
                    P.op('dve', lambda e: e.tensor_tensor(sml[:, 4 + h:5 + h], sml[:, 4 + h:5 + h], gt[:, 4 + h:5 + h], ALU.max),
                         reads=[('dn', h), 'gt'], writes=[('dn', h)])
                    P.op('dve', lambda e: e.reciprocal(sml[:, 8 + h:9 + h], sml[:, 4 + h:5 + h]), reads=[('dn', h)], writes=[('rd', h)])
                    P.op('dve', lambda e: e.scalar_tensor_tensor(hmo[:, h, :], nd[:, 0:256], sml[:, 8 + h:9 + h], sg[:, h * 256:(h + 1) * 256], ALU.mult, ALU.mult),
                         reads=[ndk, ('rd', h), ('sg', j)], writes=[('hmo', h)])
                    act(junk[:], hmo[:, h, :], AF.Square, [('hmo', h), 'ssq'], ['junkB', 'ssq'], accum_out=sml[:, h:h + 1])
                act(sml[:, 12:16], sml[:, 0:4], AF.Ln, ['ssq', 'cf'], ['rstdB'], scale=1.0 / 256, bias=epsc)
                act(sml[:, 12:16], sml[:, 12:16], AF.Exp, ['rstdB'], ['rstdB'], scale=-0.5)
                P.op('pool', lambda e: e.tensor_tensor(Gz[:], zm[:], gmB[:], ALU.mult), reads=[('zm', j), 'gmB'], writes=['Gz'])
                for h in range(4):
                    P.op('dve', lambda e: e.scalar_tensor_tensor(yt[:, h * 256:(h + 1) * 256], hmo[:, h, :], sml[:, 12 + h:13 + h], Gz[:, h * 256:(h + 1) * 256], ALU.mult, ALU.mult),
                         reads=[('hmo', h), 'rstdB', 'Gz'], writes=[('ytB', j)])
                P.dma('sp', ycat[t * 128:(t + 1) * 128, 0:1024], yt[:], reads=[('ytB', j)], writes=[('ycat', 0, t)])
            P.barrier()
        P.es = es

    def phaseC(l):
        with ExitStack() as ph:
            P.es = ph
            Sst = P.sbuf("Sst", [64, 4, 128], F32)
            Sbf = P.sbuf("Sbf", [64, 4, 128], BF16)
            tmpS = P.sbuf("tmpS", [64, 4, 128], F32)
            ggB = P.sbuf("ggB", [128, 512], F32)
            gqs = [P.sbuf("gq%d" % i, [64, 4, 128], F32) for i in range(2)]
            gks = [P.sbuf("gk%d" % i, [64, 4, 128], F32) for i in range(2)]
            las = [P.sbuf("la%d" % i, [64, 4, 128], F32) for i in range(2)]
            vgs = [P.sbuf("vgt%d" % i, [128, 512], BF16) for i in range(2)]
            rgs = [P.sbuf("rgt%d" % i, [128, 512], BF16) for i in range(2)]
            bc = P.sbuf("bcC", [64, 4, 128], F32)
            eb = P.sbuf("ebC", [64, 4, 128], F32)
            enb = P.sbuf("enbC", [64, 4, 128], F32)
            qt = P.sbuf("qtC", [64, 4, 128], BF16)
            kt = P.sbuf("ktC", [64, 4, 128], BF16)
            am = P.sbuf("amC", [128, 4, 128], BF16)
            ktk = P.sbuf("ktkC", [128, 4, 64], BF16)
            osq = P.sbuf("osqC", [128, 4, 128], F32)
            sml = P.sbuf("smlC", [128, 8], F32)
            Gr = P.sbuf("GrC", [128, 512], F32)
            ygs = [P.sbuf("yg%d" % i, [128, 512], BF16) for i in range(2)]
            at_ps = P.psum("at_ps", [128, 4, 128], F32)
            ktk_ps = P.psum("ktk_ps", [128, 4, 64], BF16)
            o_ps = P.psum("o_psC", [128, 4, 128], F32)
            su_ps = P.psum("su_ps", [64, 4, 128], F32)
            P.dma('sp', ggB[:], glnB[l], writes=['ggB'])
            P.op('pool', lambda e: e.memset(Sst[:], 0.0), writes=['Sst'])

            def loads(t):
                j = t % 2
                cols = slice(t * 128, (t + 1) * 128)
                P.dma('sp', gqs[j][:], gqT[:, :, cols].rearrange("h p s -> p h s"), writes=[('gq', j)])
                P.dma('sp', gks[j][:], gkT[:, :, cols].rearrange("h p s -> p h s"), writes=[('gk', j)])
                P.dma('sp', las[j][:], laT[:, :, cols].rearrange("h p s -> p h s"), writes=[('la', j)])
                P.dma('sp', vgs[j][:], vg[cols, :], writes=[('vgt', j)])
                P.dma('sp', rgs[j][:], rg[cols, :], writes=[('rgt', j)])

            loads(0)
            for t in range(NT):
                j = t % 2
                if t + 1 < NT:
                    loads(t + 1)
                gq, gk, la, vgt, rgt, yg = gqs[j], gks[j], las[j], vgs[j], rgs[j], ygs[j]
                for h in range(4):
                    P.op('dve', lambda e: e.tensor_tensor_scan(bc[:, h, :], ones_f[0:64, 0:128], la[:, h, :], 0.0, ALU.mult, ALU.add),
                         reads=['cf', ('la', j)], writes=['bcC'])
                act(eb[:], bc[:], AF.Exp, ['bcC'], ['ebC'])
                act(enb[:], bc[:], AF.Exp, ['bcC'], ['enbC'], scale=-1.0)
                P.op('pool', lambda e: e.tensor_tensor(qt[:], gq[:], eb[:], ALU.mult), reads=[('gq', j), 'ebC'], writes=['qtC'])
                P.op('dve', lambda e: e.tensor_tensor(kt[:], gk[:], enb[:], ALU.mult), reads=[('gk', j), 'enbC'], writes=['ktC'])
                for h in range(4):
                    mm(at_ps[:, h, :], kt[:, h, :], qt[:, h, :], True, True, ['ktC', 'qtC'], ['at_ps'])
                P.op('dve', lambda e: e.tensor_tensor(am[:], at_ps[:], mask4_b.rearrange("p (h s) -> p h s", h=4), ALU.mult),
                     reads=['at_ps', 'cb'], writes=['amC'])
                for h in range(4):
                    P.op('pe', lambda e: e.transpose(ktk_ps[:, h, :], kt[:, h, :], ident_b[0:64, 0:64]), reads=['ktC', 'cb'], writes=['ktk_ps'])
                P.op('act', lambda e: e.copy(ktk[:], ktk_ps[:]), reads=['ktk_ps'], writes=['ktkC'])
                P.op('pool', lambda e: e.tensor_copy(Sbf[:], Sst[:]), reads=['Sst'], writes=['Sbf'])
                for h in range(4):
                    mm(o_ps[:, h, :], am[:, h, :], vgt[:, h * 128:(h + 1) * 128], True, False, ['amC', ('vgt', j)], ['o_psC'])
                    mm(o_ps[:, h, :], qt[:, h, :], Sbf[:, h, :], False, True, ['qtC', 'Sbf'], ['o_psC'])
                for h in range(4):
                    mm(su_ps[:, h, :], ktk[:, h, :], vgt[:, h * 128:(h + 1) * 128], True, True, ['ktkC', ('vgt', j)], ['su_ps'])
                P.op('dve', lambda e: e.tensor_tensor(tmpS[:], Sst[:], su_ps[:], ALU.add), reads=['Sst', 'su_ps', 'Sbf'], writes=['tmpS'])
                for h in range(4):
                    P.op('dve', lambda e: e.tensor_scalar(Sst[:, h, :], tmpS[:, h, :], eb[:, h, 127:128], None, ALU.mult),
                         reads=['tmpS', 'ebC', 'Sbf'], writes=['Sst'])
                act(osq[:], o_ps[:], AF.Square, ['o_psC'], ['osqC'])
                P.op('dve', lambda e: e.tensor_reduce(sml[:, 0:4], osq[:], AX.X, ALU.add), reads=['osqC'], writes=['ssqC'])
                act(sml[:, 4:8], sml[:, 0:4], AF.Ln, ['ssqC', 'cf'], ['rstdC'], scale=1.0 / 128, bias=epsc)
                act(sml[:, 4:8], sml[:, 4:8], AF.Exp, ['rstdC'], ['rstdC'], scale=-0.5)
                P.op('pool', lambda e: e.tensor_tensor(Gr[:], rgt[:], ggB[:], ALU.mult), reads=[('rgt', j), 'ggB'], writes=['GrC'])
                for h in range(4):
                    P.op('dve', lambda e: e.scalar_tensor_tensor(yg[:, h * 128:(h + 1) * 128], o_ps[:, h, :], sml[:, 4 + h:5 + h], Gr[:, h * 128:(h + 1) * 128], ALU.mult, ALU.mult),
                         reads=['o_psC', 'rstdC', 'GrC'], writes=[('yg', j)])
                P.dma('sp', ycat[t * 128:(t + 1) * 128, 1024:1536], yg[:], reads=[('yg', j)], writes=[('ycat', 1, t)])
            P.barrier()
        P.es = es

    def phaseD(l):
        with ExitStack() as ph:
            P.es = ph
            ikt = P.sbuf("ikt", [128, S], BF16)
            score = P.sbuf("score", [128, S], F32)
            junkb = P.sbuf("junkD", [128, S], BF16)
            mbs = [P.sbuf("mb%d" % i, [128, S], BF16) for i in range(2)]
            iqts = [P.sbuf("iqt%d" % i, [128, 4, 128], BF16) for i in range(2)]
            iwts = [P.sbuf("iwt%d" % i, [128, 8], F32) for i in range(2)]
            dWs = [P.sbuf("dW%d" % i, [128, 8, 128], BF16) for i in range(2)]
            rbufs = [P.sbuf("rbuf%d" % i, [128, 512], BF16) for i in range(4)]
            sm = P.sbuf("smD", [128, 16], F32)
            wtab = P.sbuf("wtab", [128, 32], F32)
            lgs = [P.psum("lg%d" % i, [128, 512], F32) for i in range(4)]
            scs = [P.psum("sc%d" % i, [128, 512], F32) for i in range(2)]
            for c in range(0, S, 1024):
                P.dma('sp', ikt[:, c:c + 1024], ikT2[:, c:c + 1024], writes=['ikt'])
            iqv = iqT.rearrange("(g p) s -> p g s", p=128)

            def loads(t):
                j = t % 2
                cols = slice(t * 128, (t + 1) * 128)
                P.dma('sp', iqts[j][:], iqv[:, :, cols], writes=[('iqt', j)])
                P.dma('sp', iwts[j][:], iwS[cols, :], writes=[('iwt', j)])

            loads(0)
            ci = {'lg': 0, 'sc': 0, 'rb': 0}
            for t in range(NT):
                j = t % 2
                if t + 1 < NT:
                    loads(t + 1)
                iqt, iwt, dW, mb = iqts[j], iwts[j], dWs[j], mbs[j]
                nk = 128 * (t + 1)
                nkb = (nk + 511) // 512
                for h in range(8):
                    P.op('pool', lambda e: e.tensor_scalar(dW[:, h, :], ident_b, iwt[:, h:h + 1], None, ALU.mult),
                         reads=['cb', ('iwt', j)], writes=[('dW', j)])
                for kb in range(nkb):
                    sc, sck = scs[ci['sc'] % 2], ('sc', ci['sc'] % 2)
                    ci['sc'] += 1
                    for h in range(8):
                        lg, lgk = lgs[ci['lg'] % 4], ('lg', ci['lg'] % 4)
                        ci['lg'] += 1
                        rb, rbk = rbufs[ci['rb'] % 4], ('rb', ci['rb'] % 4)
                        ci['rb'] += 1
                        po = (h % 2) * 64
                        mm(lg[:, :], iqt[po:po + 64, h // 2, :], ikt[po:po + 64, kb * 512:(kb + 1) * 512], True, True,
                           [('iqt', j), 'ikt'], [lgk])
                        if h % 2:
                            act(rb[:], lg[:], AF.Relu, [lgk], [rbk])
                        else:
                            P.op('dve', lambda e: e.tensor_scalar(rb[:], lg[:], 0.0, None, ALU.max), reads=[lgk], writes=[rbk])
                        mm(sc[:, :], dW[:, h, :], rb[:], h == 0, h == 7, [('dW', j), rbk], [sck])
                    P.op('act', lambda e: e.copy(score[:, kb * 512:(kb + 1) * 512], sc[:, :]), reads=[sck], writes=['score'])
                P.op('dve', lambda e: e.tensor_reduce(sm[:, 0:1], score[:, 0:nk], AX.X, ALU.max), reads=['score'], writes=['smD'])
                P.op('dve', lambda e: e.tensor_reduce(sm[:, 1:2], score[:, 0:nk], AX.X, ALU.min), reads=['score'], writes=['smD'])
                P.op('pool', lambda e: e.memset(score[0:64, nk - 64:nk], -1.0e30), reads=['smD'], writes=['score'])
                P.op('dve', lambda e: e.tensor_tensor(sm[:, 2:3], sm[:, 0:1], sm[:, 1:2], ALU.subtract), reads=['smD'], writes=['smD'])
                P.op('dve', lambda e: e.tensor_scalar(sm[:, 3:4], sm[:, 2:3], 1.02, 2.0e-6, ALU.mult, ALU.add), reads=['smD'], writes=['smD'])
                P.op('dve', lambda e: e.tensor_scalar(wtab[:], cf[:, CF_PW:CF_PW + 32], sm[:, 3:4], None, ALU.mult), reads=['smD', 'cf'], writes=['wtab'])
                P.op('dve', lambda e: e.scalar_tensor_tensor(sm[:, 4:5], sm[:, 2:3], -0.01, sm[:, 1:2], ALU.mult, ALU.add), reads=['smD'], writes=['smD'])
                P.op('dve', lambda e: e.tensor_scalar(sm[:, 4:5], sm[:, 4:5], -1.0e-6, None, ALU.add), reads=['smD'], writes=['smD'])
                P.op('dve', lambda e: e.tensor_tensor(sm[:, 4:5], sm[:, 4:5], wtab[:, 0:1], ALU.add), reads=['smD', 'wtab'], writes=['smD'])
                for it in range(NBIS):
                    P.op('dve', lambda e: e.tensor_scalar(junkb[:, 0:nk], score[:, 0:nk], sm[:, 4:5], None, ALU.is_ge, ALU.add, accum_out=sm[:, 5:6]),
                         reads=['score', 'smD'], writes=['junkD', 'smD'])
                    P.op('dve', lambda e: e.tensor_scalar(sm[:, 6:7], sm[:, 5:6], TOPK - 0.5, 0.5, ALU.is_ge, ALU.subtract), reads=['smD'], writes=['smD'])
                    P.op('dve', lambda e: e.scalar_tensor_tensor(sm[:, 4:5], sm[:, 6:7], wtab[:, it:it + 1], sm[:, 4:5], ALU.mult, ALU.add),
                         reads=['smD', 'wtab'], writes=['smD'])
                P.op('dve', lambda e: e.tensor_tensor(sm[:, 7:8], sm[:, 4:5], wtab[:, NBIS:NBIS + 1], ALU.subtract), reads=['smD', 'wtab'], writes=['smD'])
                P.op('dve', lambda e: e.tensor_scalar(mb[:, 0:nk], score[:, 0:nk], sm[:, 7:8], NEG, ALU.is_lt, ALU.mult), reads=['score', 'smD'], writes=[('mb', j)])
                P.dma('sp', MB[t, :, 0:nk], mb[:, 0:nk], reads=[('mb', j)], writes=[('MB', t)])
            P.barrier()
        with ExitStack() as ph:
            P.es = ph
            KT = P.sbuf("KT", [128, 4, S], BF16)
            V1 = P.sbuf("V1", [128, NT, 4, 129], BF16)
            ohs = P.sbuf("ohs", [128, len(_OHIDX), 128], BF16)
            relb = P.sbuf("relb", [128, 128], F32)
            biasT = P.sbuf("biasT", [128, 2, 4, 128], F32)
            ec = P.sbuf("ecD", [128, 4], F32)
            qhs = [P.sbuf("qh%d" % i, [128, 4, 128], BF16) for i in range(2)]
            mbts = [P.sbuf("mbt%d" % i, [128, S], BF16) for i in range(2)]
            zdts = [P.sbuf("zdt%d" % i, [128, 512], BF16) for i in range(2)]
            pTs = [P.sbuf("pT%d" % i, [128, 4, 128], BF16) for i in range(3)]
            tmps = [P.sbuf("tmpD%d" % i, [128, 4, 128], F32) for i in range(2)]
            on_sb = P.sbuf("on_sb", [128, 4, 129], F32)
            O = P.sbuf("O_D", [128, 4, 129], F32)
            rden = P.sbuf("rdenD", [128, 4], F32)
            yds = [P.sbuf("yd%d" % i, [128, 512], BF16) for i in range(2)]
            s_pss = [P.psum("s_ps%d" % i, [128, 4, 128], F32) for i in range(2)]
            ofs = [P.psum("of%d" % i, [128, 2, 129], F32) for i in range(2)]
            ons = [P.psum("on%d" % i, [128, 2, 129], F32) for i in range(2)]
            for h in range(4):
                P.dma('sp', KT[:, h, :], KhT[h], writes=['KT'])
            P.op('pool', lambda e: e.memset(V1[:, :, :, 128:129], 1.0), writes=['V1'])
            for u0 in range(NT):
                P.dma('sp', V1[:, u0, :, 0:128], vd[u0 * 128:(u0 + 1) * 128, :].rearrange("p (h e) -> p h e", h=4), writes=['V1'])
            P.dma('sp', ohs[:], oh_in, writes=['ohs'])
            P.dma('sp', relb[:], relB, writes=['relb'])
            P.op('pool', lambda e: e.memset(biasT[:], 0.0), writes=['biasT'])
            for i, (r, b) in enumerate(_OHIDX):
                for h in range(4):
                    P.op('dve', lambda e: e.scalar_tensor_tensor(biasT[:, r, h, :], ohs[:, i, :], relb[:, b * 4 + h:b * 4 + h + 1], biasT[:, r, h, :], ALU.mult, ALU.add),
                         reads=['ohs', 'relb', 'biasT'], writes=['biasT'])
            act(ec[:], relb[:, 60:64], AF.Exp, ['relb'], ['ecD'])

            def loads(t):
                j = t % 2
                cols = slice(t * 128, (t + 1) * 128)
                nk = 128 * (t + 1)
                P.dma('sp', qhs[j][:], QhT[:, :, cols].rearrange("h p s -> p h s"), writes=[('qh', j)])
                P.dma('sp', mbts[j][:, 0:nk], MB[t, :, 0:nk], writes=[('mbt', j)])
                P.dma('sp', zdts[j][:], zd[cols, :], writes=[('zdt', j)])

            loads(0)
            ci = {'s': 0, 'p': 0, 'tmp': 0}
            for t in range(NT):
                j = t % 2
                if t + 1 < NT:
                    loads(t + 1)
                qh, mbt, zdt, yd = qhs[j], mbts[j], zdts[j], yds[j]
                far_last = t - 2
                near_first = max(0, t - 1)
                for u in range(t + 1):
                    near = u >= t - 1
                    s_ps, sk = s_pss[ci['s'] % 2], ('s_ps', ci['s'] % 2)
                    ci['s'] += 1
                    pT, pk = pTs[ci['p'] % 3], ('pT', ci['p'] % 3)
                    ci['p'] += 1
                    for h in range(4):
                        mm(s_ps[:, h, :], KT[:, h, u * 128:(u + 1) * 128], qh[:, h, :], True, False, ['KT', ('qh', j)], [sk])
                        mm(s_ps[:, h, :], mbt[:, u * 128:(u + 1) * 128], ident_b, False, True, [('mbt', j), 'cb'], [sk])
                    if near:
                        tmp, tk = tmps[ci['tmp'] % 2], ('tmpD', ci['tmp'] % 2)
                        ci['tmp'] += 1
                        P.op('dve', lambda e: e.tensor_tensor(tmp[:], s_ps[:], biasT[:, t - u, :, :], ALU.add), reads=[sk, 'biasT'], writes=[tk])
                        act(pT[:], tmp[:], AF.Exp, [tk], [pk])
                    else:
                        act(pT[:], s_ps[:], AF.Exp, [sk], [pk])
                    for h in range(4):
                        if near:
                            o_, ok_ = ons[h // 2], ('on', h // 2)
                            first, last = (u == near_first), (u == t)
                        else:
                            o_, ok_ = ofs[h // 2], ('of', h // 2)
                            first, last = (u == 0), (u == far_last)
                        mm(o_[:, h % 2, :], pT[:, h, :], V1[:, u, h, :], first and (h % 2 == 0), last, [pk, 'V1'], [ok_])
                for hh in range(2):
                    P.op('act', lambda e: e.copy(on_sb[:, 2 * hh:2 * hh + 2, :], ons[hh][:]), reads=[('on', hh)], writes=[('on_sb', hh)])
                for h in range(4):
                    if t >= 2:
                        P.op('dve', lambda e: e.scalar_tensor_tensor(O[:, h, :], ofs[h // 2][:, h % 2, :], ec[:, h:h + 1], on_sb[:, h, :], ALU.mult, ALU.add),
                             reads=[('of', h // 2), 'ecD', ('on_sb', h // 2)], writes=[('O', h)])
                        Oh = O
                        Ok = ('O', h)
                    else:
                        Oh = on_sb
                        Ok = ('on_sb', h // 2)
                    P.op('dve', lambda e: e.reciprocal(rden[:, h:h + 1], Oh[:, h, 128:129]), reads=[Ok], writes=[('rden', h)])
                    P.op('dve', lambda e: e.scalar_tensor_tensor(yd[:, h * 128:(h + 1) * 128], Oh[:, h, 0:128], rden[:, h:h + 1], zdt[:, h * 128:(h + 1) * 128], ALU.mult, ALU.mult),
                         reads=[Ok, ('rden', h), ('zdt', j)], writes=[('yd', j)])
                P.dma('sp', ycat[t * 128:(t + 1) * 128, 1536:2048], yd[:], reads=[('yd', j)], writes=[('ycat', 2, t)])
            P.barrier()
        P.es = es

    def phaseE(l, src, dst):
        with ExitStack() as ph:
            P.es = ph
            wo = P.sbuf("wo", [128, 16, 1024], BF16)
            wf2 = P.sbuf("wf2", [128, 4, 1024], F32)
            yts = [P.sbuf("ytE%d" % i, [128, 2048], BF16) for i in range(2)]
            yTs = [P.sbuf("yTE%d" % i, [128, 16, 128], BF16) for i in range(2)]
            xts = [P.sbuf("xtE%d" % i, [128, 1024], F32) for i in range(2)]
            ots = [P.sbuf("otE%d" % i, [128, 1024], F32) for i in range(2)]
            tpe = [P.psum("tpe%d" % i, [128, 8, 128], BF16) for i in range(2)]
            oe = [P.psum("oe%d" % i, [128, 512], F32) for i in range(2)]
            wv = w_out[l].rearrange("(c p) n -> p c n", p=128)
            for q in range(4):
                P.dma('sp', wf2[:], wv[:, 4 * q:4 * q + 4, :], writes=['wf2'])
                P.op('pool', lambda e: e.tensor_copy(wo[:, 4 * q:4 * q + 4, :], wf2[:]), reads=['wf2'], writes=['wo'])

            def loads(t):
                j = t % 2
                rows = slice(t * 128, (t + 1) * 128)
                P.dma('sp', yts[j][:], ycat[rows, :], writes=[('ytE', j)])
                P.dma('sp', xts[j][:], src[rows, :], writes=[('xtE', j)])

            loads(0)
            for t in range(NT):
                j = t % 2
                if t + 1 < NT:
                    loads(t + 1)
                yt, yT, xt, ot = yts[j], yTs[j], xts[j], ots[j]
                for c in range(16):
                    P.op('pe', lambda e: e.transpose(tpe[c // 8][:, c % 8, :], yt[:, c * 128:(c + 1) * 128], ident_b),
                         reads=[('ytE', j), 'cb'], writes=[('tpe', c // 8)])
                P.op('act', lambda e: e.copy(yT[:, 0:8, :], tpe[0][:]), reads=[('tpe', 0)], writes=[('yTE', j)])
                P.op('dve', lambda e: e.tensor_copy(yT[:, 8:16, :], tpe[1][:]), reads=[('tpe', 1)], writes=[('yTE', j)])
                for half in range(2):
                    for c in range(16):
                        mm(oe[half][:, :], yT[:, c, :], wo[:, c, half * 512:(half + 1) * 512], c == 0, c == 15, [('yTE', j), 'wo'], [('oe', half)])
                    P.op('dve', lambda e: e.tensor_tensor(ot[:, half * 512:(half + 1) * 512], oe[half][:, :], xt[:, half * 512:(half + 1) * 512], ALU.add),
                         reads=[('oe', half), ('xtE', j)], writes=[('otE', j)])
                P.dma('sp', dst[t * 128:(t + 1) * 128, :], ot[:], reads=[('otE', j)], writes=[('dst', t)])
            P.barrier()
        P.es = es

    k = K()
    k.__dict__.update(locals())
    return k


def prep_shared(inputs):
    f = lambda a: np.ascontiguousarray(np.asarray(a, dtype=np.float32))
    cf, cb, oh, _ = host_consts()
    d = {}
    d["w_in"] = f(inputs["w_in"])
    d["w_out"] = f(inputs["w_out"])
    d["normB"] = f(np.broadcast_to(np.asarray(inputs["norm_g"])[:, None, :], (2, 128, 1024)))
    d["convwT"] = f(np.concatenate([np.transpose(np.asarray(inputs["ml_conv_w"]), (0, 2, 1)),
                                    np.asarray(inputs["ml_conv_b"])[:, :, None]], axis=2))
    d["ml_b_i"] = f(np.asarray(inputs["ml_b_i"])[:, :, None])
    d["ml_b_f"] = f(np.asarray(inputs["ml_b_f"])[:, :, None])
    d["mlnB"] = f(np.broadcast_to(np.asarray(inputs["ml_norm_g"])[:, None, :], (2, 128, 1024)))
    d["gla_w_a"] = f(inputs["gla_w_a"])
    d["gla_b_aT"] = f(np.transpose(np.asarray(inputs["gla_b_a"]).reshape(2, 4, 64), (0, 2, 1)))
    d["glnB"] = f(np.broadcast_to(np.asarray(inputs["gla_norm_g"])[:, None, :], (2, 128, 512)))
    d["dsa_q_g"] = f(np.asarray(inputs["dsa_q_g"])[:, :, None])
    d["dsa_k_g"] = f(np.asarray(inputs["dsa_k_g"])[:, :, None])
    d["relB"] = f(np.broadcast_to(np.asarray(inputs["rel_bias"]).reshape(1, 128), (128, 128)))
    d["cf"] = cf
    d["cb"] = cb
    d["oh"] = oh
    return d


def emit(k, phases="ABCDE", layers=(0,)):
    for l in layers:
        src = k.x_in if l == 0 else k.h1
        if "A" in phases:
            k.phaseA(l, src)
        if "B" in phases:
            k.phaseB(l)
        if "C" in phases:
            k.phaseC(l)
        if "D" in phases:
            k.phaseD(l)
        if "E" in phases:
            k.phaseE(l, src, k.h1 if l == 0 else k.out)


_CACHE = {}


def kernel(**inputs):
    x = np.asarray(inputs["x"], dtype=np.float32)
    B, S, D = x.shape
    key = (S,)
    if key not in _CACHE:
        k = build(S, depth=2, debug=False)
        emit(k, "ABCDE", layers=(0, 1))
        k.P.finish()
        _CACHE[key] = k
    k = _CACHE[key]
    shared = prep_shared(inputs)
    in_maps = []
    for b in range(B):
        d = dict(shared)
        d["x"] = np.ascontiguousarray(x[b])
        in_maps.append(d)
    res = run_bass_kernel_spmd(k.nc, in_maps, core_ids=list(range(B)))
    return np.stack([np.asarray(r["out"], dtype=np.float32) for r in res.results], axis=0)
```

```python
import numpy as np
import ml_dtypes
from contextlib import ExitStack
import concourse.bass as bass
import concourse.mybir as mybir
from concourse.bass_utils import run_bass_kernel_spmd

F32 = mybir.dt.float32
BF16 = mybir.dt.bfloat16
AF = mybir.ActivationFunctionType
ALU = mybir.AluOpType
AX = mybir.AxisListType


class Prog:
    ENG = ['pe', 'act', 'dve', 'pool', 'sp']

    def __init__(self, nc, es, n_dma_sems=40):
        self.nc = nc
        self.es = es
        self.es0 = es
        self.sem = {e: es.enter_context(nc.semaphore('s_' + e)) for e in self.ENG}
        self.dsem = [es.enter_context(nc.semaphore('d%d' % i)) for i in range(n_dma_sems)]
        self.cnt = {e: 0 for e in self.ENG}
        self.dcnt = 0
        self.dval = [0] * n_dma_sems
        self.waited = {e: {} for e in self.ENG}
        self.ops = {e: [] for e in self.ENG}
        self.lastw = {}
        self.readers = {}
        self.nops = 0
        self._e_pe = nc.tensor
        self._e_act = nc.scalar
        self._e_dve = nc.vector
        self._e_pool = nc.gpsimd
        self._e_sp = nc.sync

    def sbuf(self, name, shape, dtype):
        self.uid = getattr(self, 'uid', 0) + 1
        return self.es.enter_context(self.nc.sbuf_tensor("sb%d_%s" % (self.uid, name), shape, dtype))

    def psum(self, name, shape, dtype):
        self.uid = getattr(self, 'uid', 0) + 1
        return self.es.enter_context(self.nc.psum_tensor("ps%d_%s" % (self.uid, name), shape, dtype))

    def _wait(self, eng, tok):
        if tok is None:
            return
        kind, who, val = tok
        if kind == 'e' and who == 'pe' and eng == 'pe':
            return
        key = (kind, who)
        if self.waited[eng].get(key, 0) >= val:
            return
        self.waited[eng][key] = val
        sem = self.sem[who] if kind == 'e' else self.dsem[who]
        getattr(self, '_e_' + eng).wait_ge(sem, val)

    def _deps(self, eng, reads, writes):
        for k in reads:
            self._wait(eng, self.lastw.get(k))
        for k in writes:
            self._wait(eng, self.lastw.get(k))
            for t in self.readers.get(k, ()):
                self._wait(eng, t)

    def _commit(self, tok, reads, writes):
        for k in writes:
            self.lastw[k] = tok
            self.readers[k] = []
        for k in reads:
            if k in writes:
                continue
            self.readers.setdefault(k, []).append(tok)

    def op(self, eng, fn, reads=(), writes=()):
        self._deps(eng, reads, writes)
        self.cnt[eng] += 1
        tok = ('e', eng, self.cnt[eng])
        fn(getattr(self, '_e_' + eng)).then_inc(self.sem[eng], 1)
        self._commit(tok, reads, writes)
        self.nops += 1

    def dma(self, q, out, in_, reads=(), writes=(), **kw):
        n = len(self.dsem)
        idx = self.dcnt % n
        self.dcnt += 1
        if self.dval[idx] > 0:
            self._wait(q, ('d', idx, self.dval[idx]))
        self._deps(q, reads, writes)
        self.dval[idx] += 16
        tok = ('d', idx, self.dval[idx])
        getattr(self, '_e_' + q).dma_start(out=out, in_=in_, **kw).then_inc(self.dsem[idx], 16)
        self._commit(tok, reads, writes)
        self.nops += 1

    def finish(self):
        for idx, v in enumerate(self.dval):
            if v > 0:
                self._wait('sp', ('d', idx, v))
        for e in ['pe', 'act', 'dve', 'pool']:
            if self.cnt[e] > 0:
                self._wait('sp', ('e', e, self.cnt[e]))

    def barrier(self):
        for e in self.ENG:
            for o in ['pe', 'act', 'dve', 'pool']:
                if self.cnt[o] > 0 and not (e == o):
                    self._wait(e, ('e', o, self.cnt[o]))
            for idx, v in enumerate(self.dval):
                if v > 0:
                    self._wait(e, ('d', idx, v))
        if not hasattr(self, 'bar'):
            self.bar = self.es0.enter_context(self.nc.semaphore('s_bar'))
            self.go = self.es0.enter_context(self.nc.semaphore('s_go'))
            self.epoch = 0
        self.epoch += 1
        comp = ['pe', 'act', 'dve', 'pool']
        for e in comp:
            getattr(self, '_e_' + e).sem_inc(self.bar, 1)
        sp = self._e_sp
        sp.wait_ge(self.bar, 4 * self.epoch)
        for e in comp:
            sp.sem_clear(self.sem[e])
        for d in self.dsem:
            sp.sem_clear(d)
        sp.sem_inc(self.go, 1)
        for e in comp:
            getattr(self, '_e_' + e).wait_ge(self.go, self.epoch)
        for e in comp:
            self.cnt[e] = 0
        self.dval = [0] * len(self.dsem)
        self.waited = {e: {} for e in self.ENG}
        self.lastw = {}
        self.readers = {}


C_MLQ, C_MLK, C_MLV, C_MLO, C_MLZ = 0, 1024, 2048, 3072, 4096
C_MLI, C_MLF = 5120, 5124
C_GQ, C_GK, C_GV, C_GA, C_GR = 5128, 5384, 5640, 6152, 6168
C_DQ, C_DK, C_DV, C_DZ = 6680, 7192, 7704, 8216
C_IQ, C_IK, C_IW = 8728, 9240, 9304
N_IN = 9312
EPS = 1e-6
NBIS = 24
NEG = -30000.0

CF_ID, CF_ONES, CF_MASK, CF_PW, CF_MISC = 0, 128, 256, 384, 416
NCF = 432
CB_ID, CB_MASK4 = 0, 128
NCB = 640


def t5_bucket_np(rel):
    half, max_exact = 16, 8
    ret = np.where(rel > 0, half, 0)
    n = np.abs(rel)
    nf = np.maximum(n, 1).astype(np.float32)
    large = max_exact + (np.log(nf / max_exact) / np.float32(np.log(128 / max_exact)) * (half - max_exact)).astype(np.int32)
    large = np.minimum(large, half - 1)
    return ret + np.where(n < max_exact, n, large)


def host_consts():
    cf = np.zeros((128, NCF), np.float32)
    cf[:, CF_ID:CF_ID + 128] = np.eye(128, dtype=np.float32)
    cf[:, CF_ONES:CF_ONES + 128] = 1.0
    s = np.arange(128)
    maskT = (s[:, None] <= s[None, :]).astype(np.float32)
    cf[:, CF_MASK:CF_MASK + 128] = maskT
    cf[:, CF_PW:CF_PW + 32] = (0.5 ** (np.arange(32) + 1))[None, :]
    cf[:, CF_MISC + 0] = EPS
    cf[:, CF_MISC + 1] = 1.0
    cf[:, CF_MISC + 2] = 0.0
    cb = np.zeros((128, NCB), np.float32)
    cb[:, CB_ID:CB_ID + 128] = np.eye(128)
    cb[:, CB_MASK4:CB_MASK4 + 512] = np.tile(maskT, (1, 4))
    k = np.arange(128)[:, None]
    q = np.arange(128)[None, :]
    oh = []
    ohidx = []
    for r in (0, 1):
        b = t5_bucket_np((k - q - 128 * r).astype(np.int32))
        for bb in np.unique(b):
            oh.append((b == bb).astype(np.float32))
            ohidx.append((r, int(bb)))
    oh = np.stack(oh, 1)
    return cf, cb.astype(ml_dtypes.bfloat16), oh.astype(ml_dtypes.bfloat16), ohidx


_OHIDX = host_consts()[3]


class K:
    pass


def build(S, depth=2, debug=False, stop_after=None):
    NT = S // 128
    NB = S // 512
    TOPK = min(256, S // 4)
    nc = bass.Bass("TRN2", target_bir_lowering=False)

    def din(name, shape, dt=F32):
        return nc.dram_tensor(name, list(shape), dt, kind="ExternalInput").ap()

    def dscr(name, shape, dt=F32):
        return nc.dram_tensor(name, list(shape), dt, kind=("ExternalOutput" if debug else "Internal")).ap()

    x_in = din("x", [S, 1024])
    w_in = din("w_in", [2, 1024, N_IN])
    w_out = din("w_out", [2, 2048, 1024])
    normB = din("normB", [2, 128, 1024])
    convwT = din("convwT", [2, 2048, 5])
    ml_b_i = din("ml_b_i", [2, 4, 1])
    ml_b_f = din("ml_b_f", [2, 4, 1])
    mlnB = din("mlnB", [2, 128, 1024])
    gla_w_a = din("gla_w_a", [2, 16, 256])
    gla_b_aT = din("gla_b_aT", [2, 64, 4])
    glnB = din("glnB", [2, 128, 512])
    dsa_q_g = din("dsa_q_g", [2, 128, 1])
    dsa_k_g = din("dsa_k_g", [2, 128, 1])
    relB = din("relB", [128, 128])
    cf_in = din("cf", [128, NCF])
    cb_in = din("cb", [128, NCB], BF16)
    oh_in = din("oh", [128, len(_OHIDX), 128], BF16)
    out = nc.dram_tensor("out", [S, 1024], F32, kind="ExternalOutput").ap()

    h1 = dscr("h1", [S, 1024])
    qkT = dscr("qkT", [2048, S], BF16)
    vml = dscr("vml", [S, 1024], BF16)
    sigo = dscr("sigo", [S, 1024], BF16)
    zml = dscr("zml", [S, 1024], BF16)
    liT = dscr("liT", [4, S])
    lfT = dscr("lfT", [4, S])
    gqT = dscr("gqT", [4, 64, S])
    gkT = dscr("gkT", [4, 64, S])
    laT = dscr("laT", [4, 64, S])
    vg = dscr("vg", [S, 512], BF16)
    rg = dscr("rg", [S, 512], BF16)
    QhT = dscr("QhT", [4, 128, S], BF16)
    KhT = dscr("KhT", [4, 128, S], BF16)
    vd = dscr("vd", [S, 512], BF16)
    zd = dscr("zd", [S, 512], BF16)
    iqT = dscr("iqT", [512, S], BF16)
    ikT2 = dscr("ikT2", [128, S], BF16)
    iwS = dscr("iw", [S, 8])
    MB = dscr("MB", [NT, 128, S], BF16)
    ycat = dscr("ycat", [S, 2048], BF16)

    es = ExitStack()
    P = Prog(nc, es)
    cf = P.sbuf("cf", [128, NCF], F32)
    cb = P.sbuf("cb", [128, NCB], BF16)
    P.dma('sp', cf[:], cf_in, writes=['cf'])
    P.dma('sp', cb[:], cb_in, writes=['cb'])
    ident_f = cf[:, CF_ID:CF_ID + 128]
    ones_f = cf[:, CF_ONES:CF_ONES + 128]
    maskT_f = cf[:, CF_MASK:CF_MASK + 128]
    epsc = cf[:, CF_MISC:CF_MISC + 1]
    onec = cf[:, CF_MISC + 1:CF_MISC + 2]
    ident_b = cb[:, CB_ID:CB_ID + 128]
    mask4_b = cb[:, CB_MASK4:CB_MASK4 + 512]

    def act(outp, inp, func, r, w, **kw):
        P.op('act', lambda e: e.activation(outp, inp, func, **kw), reads=r, writes=w)

    def mm(outp, lhsT, rhs, start, stop, r, w):
        P.op('pe', lambda e: e.matmul(outp, lhsT, rhs, start=start, stop=stop), reads=r, writes=w)

    def phaseA(l, src):
        with ExitStack() as ph:
            P.es = ph
            xnT = P.sbuf("xnT", [128, 8, S], BF16)
            gB = P.sbuf("gB", [128, 1024], F32)
            P.dma('sp', gB[:], normB[l], writes=['gB'])
            with ExitStack() as ph1:
                P.es = ph1
                xts = [P.sbuf("xt%d" % i, [128, 1024], F32) for i in range(2)]
                xns = [P.sbuf("xn%d" % i, [128, 1024], BF16) for i in range(2)]
                junk = P.sbuf("junkA", [128, 1024], BF16)
                ss = [P.sbuf("ssA%d" % i, [128, 1], F32) for i in range(2)]
                tps = [P.psum("tpA%d" % i, [128, 8, 128], BF16) for i in range(2)]
                for t in range(NT):
                    j = t % 2
                    xt, xn, tp, s1 = xts[j], xns[j], tps[j], ss[j]
                    P.dma('sp', xt[:], src[t * 128:(t + 1) * 128, :], writes=[('xt', j)])
                    P.op('pool', lambda e: e.memset(s1[:], 0.0), writes=[('ss', j)])
                    act(junk[:], xt[:], AF.Square, [('xt', j)], ['junkA', ('ss', j)], accum_out=s1[:, 0:1])
                    act(s1[:], s1[:], AF.Ln, [('ss', j), 'cf'], [('ss', j)], scale=1.0 / 1024, bias=epsc)
                    act(s1[:], s1[:], AF.Exp, [('ss', j)], [('ss', j)], scale=-0.5)
                    P.op('dve', lambda e: e.scalar_tensor_tensor(xn[:], xt[:], s1[:, 0:1], gB[:], ALU.mult, ALU.mult),
                         reads=[('xt', j), ('ss', j), 'gB'], writes=[('xn', j)])
                    for c in range(8):
                        P.op('pe', lambda e: e.transpose(tp[:, c, :], xn[:, c * 128:(c + 1) * 128], ident_b),
                             reads=[('xn', j), 'cb'], writes=[('tp', j)])
                    P.op('dve' if j else 'act', (lambda e: e.tensor_copy(xnT[:, :, t * 128:(t + 1) * 128], tp[:])) if j else
                         (lambda e: e.copy(xnT[:, :, t * 128:(t + 1) * 128], tp[:])),
                         reads=[('tp', j)], writes=[('xnT', t // 4)])
            P.barrier()
            P.es = ph
            wf = P.sbuf("wf", [128, 8, 512], F32)
            wbs = [P.sbuf("wb%d" % i, [128, 8, 512], BF16) for i in range(2)]
            sts = [P.sbuf("st%d" % i, [128, 515], F32) for i in range(2)]
            accs = [P.sbuf("acc%d" % i, [128, 512], F32) for i in range(2)]
            obf = [P.sbuf("obf%d" % i, [128, 512], F32) for i in range(3)]
            obb = [P.sbuf("obb%d" % i, [128, 512], BF16) for i in range(3)]
            cws = [P.sbuf("cw%d" % i, [128, 5], F32) for i in range(2)]
            smallp = P.sbuf("smallp", [128, 16], F32)
            wab_f = P.sbuf("wab_f", [16, 256], F32)
            wab = P.sbuf("wab", [16, 256], BF16)
            aTb = [P.sbuf("aTb%d" % i, [16, 512], BF16) for i in range(2)]
            pas = [P.psum("paA%d" % i, [128, 512], F32) for i in range(4)]
            pzs = [P.psum("pzA%d" % i, [128, 512], F32) for i in range(2)]
            wl = w_in[l].rearrange("(c p) n -> p c n", p=128)
            P.dma('sp', smallp[:, 0:1], dsa_q_g[l], writes=['smallp'])
            P.dma('sp', smallp[:, 1:2], dsa_k_g[l], writes=['smallp'])
            P.dma('sp', smallp[0:4, 2:3], ml_b_f[l], writes=['smallp'])
            P.dma('sp', smallp[0:4, 3:4], ml_b_i[l], writes=['smallp'])
            P.dma('sp', smallp[0:64, 4:8], gla_b_aT[l], writes=['smallp'])
            P.dma('sp', wab_f[:], gla_w_a[l], writes=['wab_f'])
            P.op('dve', lambda e: e.tensor_scalar(smallp[:, 0:1], smallp[:, 0:1], 128.0 ** -0.5, None, ALU.mult), reads=['smallp'], writes=['smallp'])
            P.op('dve', lambda e: e.tensor_scalar(smallp[0:4, 2:3], smallp[0:4, 2:3], -1.0, None, ALU.mult), reads=['smallp'], writes=['smallp'])
            P.op('dve', lambda e: e.tensor_scalar(smallp[0:64, 4:8], smallp[0:64, 4:8], -1.0, None, ALU.mult), reads=['smallp'], writes=['smallp'])
            P.op('dve', lambda e: e.tensor_copy(wab[:], wab_f[:]), reads=['wab_f'], writes=['wab'])

            fm = []
            for g in range(8):
                fm.append(('mlq', C_MLQ + g * 128, 128, g))
            for g in range(8):
                fm.append(('mlk', C_MLK + g * 128, 128, 8 + g))
            for h in range(4):
                fm.append(('glaq', C_GQ + h * 64, 64, h))
            for h in range(4):
                fm.append(('glak', C_GK + h * 64, 64, h))
            fm.append(('glaa', C_GA, 16, 0))
            for h in range(4):
                fm.append(('dsaq', C_DQ + h * 128, 128, h))
            for h in range(4):
                fm.append(('dsak', C_DK + h * 128, 128, h))
            for g in range(4):
                fm.append(('idxq', C_IQ + g * 128, 128, g))
            fm.append(('idxk', C_IK, 64, 0))
            fm.append(('mli', C_MLI, 4, 0))
            fm.append(('mlf', C_MLF, 4, 0))
            tm = [('mlv', C_MLV, 512, vml, 0), ('mlv', C_MLV + 512, 512, vml, 512),
                  ('mlo', C_MLO, 512, sigo, 0), ('mlo', C_MLO + 512, 512, sigo, 512),
                  ('mlz', C_MLZ, 512, zml, 0), ('mlz', C_MLZ + 512, 512, zml, 512),
                  ('glav', C_GV, 512, vg, 0), ('glar', C_GR, 512, rg, 0),
                  ('dsav', C_DV, 512, vd, 0), ('dsaz', C_DZ, 512, zd, 0), ('idxw', C_IW, 8, iwS, 0)]
            groups = [('fm',) + g for g in fm] + [('tm',) + g for g in tm]
            cnt = {'ev': 0, 'ob': 0, 'pa': 0, 'pz': 0, 'cw': 0, 'at': 0}

            def load_w(gi):
                g = groups[gi]
                c0, n = g[2], g[3]
                if g[1] == 'idxk':
                    P.dma('sp', wf[:, :, 0:64], wl[:, :, c0:c0 + 64], writes=['wf'])
                    P.dma('sp', wf[:, :, 64:128], wl[:, :, c0:c0 + 64], writes=['wf'])
                    n = 128
                else:
                    P.dma('sp', wf[:, :, 0:n], wl[:, :, c0:c0 + n], writes=['wf'])
                wb = wbs[gi % 2]
                P.op('pool', lambda e: e.tensor_copy(wb[:, :, 0:n], wf[:, :, 0:n]), reads=['wf'], writes=[('wb', gi % 2)])

            def next_ob(kind):
                i = cnt['ob'] % 3
                cnt['ob'] += 1
                return (obf if kind == 'f' else obb)[i], ('obf' if kind == 'f' else 'obb', i)

            def plain_evac(dst, srcp, r, w, scale=None):
                i = cnt['ev']
                cnt['ev'] += 1
                if scale is not None:
                    if i % 2:
                        P.op('act', lambda e: e.mul(dst, srcp, scale), reads=r, writes=w)
                    else:
                        P.op('dve', lambda e: e.tensor_scalar(dst, srcp, scale, None, ALU.mult), reads=r, writes=w)
                else:
                    if i % 2:
                        P.op('act', lambda e: e.copy(dst, srcp), reads=r, writes=w)
                    else:
                        P.op('dve', lambda e: e.tensor_copy(dst, srcp), reads=r, writes=w)

            def do_fm(gi):
                _, kind, c0, n, idx = groups[gi]
                wb = wbs[gi % 2]
                wk = ('wb', gi % 2)
                M = 128 if kind == 'idxk' else n
                if kind in ('mlq', 'mlk'):
                    cw = cws[cnt['cw'] % 2]
                    cwk = ('cw', cnt['cw'] % 2)
                    cnt['cw'] += 1
                    P.dma('sp', cw[:], convwT[l][idx * 128:(idx + 1) * 128, :], writes=[cwk])
                for tb in range(NB):
                    pi = cnt['pa'] % 4
                    cnt['pa'] += 1
                    pa = pas[pi]
                    pk = ('pa', pi)
                    for c in range(8):
                        mm(pa[0:M, :], wb[:, c, 0:M], xnT[:, c, tb * 512:(tb + 1) * 512], c == 0, c == 7,
                           [wk, ('xnT', tb)], [pk])
                    tsl = slice(tb * 512, (tb + 1) * 512)
                    if kind in ('mlq', 'mlk'):
                        st, stk = sts[tb % 2], ('st', tb % 2)
                        pst, pstk = sts[(tb + 1) % 2], ('st', (tb + 1) % 2)
                        acc, acck = accs[tb % 2], ('acc', tb % 2)
                        P.op('act', lambda e: e.copy(st[:, 3:515], pa[:, :]), reads=[pk], writes=[stk])
                        if tb == 0:
                            P.op('pool', lambda e: e.memset(st[:, 0:3], 0.0), writes=[stk])
                        else:
                            P.op('pool', lambda e: e.tensor_copy(st[:, 0:3], pst[:, 512:515]), reads=[pstk], writes=[stk])
                        P.op('dve', lambda e: e.tensor_scalar(acc[:], st[:, 0:512], cw[:, 0:1], cw[:, 4:5], ALU.mult, ALU.add),
                             reads=[stk, cwk], writes=[acck])
                        for j in (1, 2, 3):
                            P.op('dve', lambda e: e.scalar_tensor_tensor(acc[:], st[:, j:j + 512], cw[:, j:j + 1], acc[:], ALU.mult, ALU.add),
                                 reads=[stk, cwk, acck], writes=[acck])
                        ob, obk = next_ob('b')
                        act(ob[:], acc[:], AF.Silu, [acck], [obk])
                        P.dma('sp', qkT[idx * 128:(idx + 1) * 128, tsl], ob[:], reads=[obk], writes=[('qkT', idx, tb)])
                    elif kind == 'glaq':
                        ob, obk = next_ob('f')
                        plain_evac(ob[0:64, :], pa[0:64, :], [pk], [obk], scale=0.125)
                        P.dma('sp', gqT[idx, :, tsl], ob[0:64, :], reads=[obk], writes=[('gqT', idx, tb)])
                    elif kind == 'glak':
                        ob, obk = next_ob('f')
                        plain_evac(ob[0:64, :], pa[0:64, :], [pk], [obk])
                        P.dma('sp', gkT[idx, :, tsl], ob[0:64, :], reads=[obk], writes=[('gkT', idx, tb)])
                    elif kind == 'glaa':
                        at = aTb[cnt['at'] % 2]
                        atk = ('aTb', cnt['at'] % 2)
                        cnt['at'] += 1
                        P.op('act', lambda e: e.copy(at[:, :], pa[0:16, :]), reads=[pk], writes=[atk])
                        for h in range(4):
                            zi = cnt['pz'] % 2
                            cnt['pz'] += 1
                            pz, pzk = pzs[zi], ('pz', zi)
                            mm(pz[0:64, :], wab[:, h * 64:(h + 1) * 64], at[:, :], True, True, ['wab', atk], [pzk])
                            ob, obk = next_ob('f')
                            act(ob[0:64, :], pz[0:64, :], AF.Exp, [pzk, 'smallp'], [obk], scale=-1.0, bias=smallp[0:64, 4 + h:5 + h])
                            act(ob[0:64, :], ob[0:64, :], AF.Ln, [obk, 'cf'], [obk], bias=onec[0:64, :])
                            P.op('dve', lambda e: e.tensor_scalar(ob[0:64, :], ob[0:64, :], -1.0 / 16.0, None, ALU.mult), reads=[obk], writes=[obk])
                            P.dma('sp', laT[h, :, tsl], ob[0:64, :], reads=[obk], writes=[('laT', h, tb)])
                    elif kind in ('dsaq', 'dsak'):
                        sq, sqk = next_ob('f')
                        act(sq[:], pa[:], AF.Square, [pk], [sqk])
                        zi = cnt['pz'] % 2
                        cnt['pz'] += 1
                        pz, pzk = pzs[zi], ('pz', zi)
                        mm(pz[:, :], ones_f, sq[:], True, True, ['cf', sqk], [pzk])
                        act(sq[:], pz[:], AF.Ln, [pzk, 'cf'], [sqk], scale=1.0 / 128, bias=epsc)
                        act(sq[:], sq[:], AF.Exp, [sqk], [sqk], scale=-0.5)
                        ob, obk = next_ob('b')
                        gcol = 0 if kind == 'dsaq' else 1
                        P.op('dve', lambda e: e.scalar_tensor_tensor(ob[:], pa[:], smallp[:, gcol:gcol + 1], sq[:], ALU.mult, ALU.mult),
                             reads=[pk, 'smallp', sqk], writes=[obk])
                        dstT = QhT if kind == 'dsaq' else KhT
                        P.dma('sp', dstT[idx, :, tsl], ob[:], reads=[obk], writes=[(kind, idx, tb)])
                    elif kind == 'idxq':
                        ob, obk = next_ob('b')
                        plain_evac(ob[:], pa[:], [pk], [obk], scale=0.125)
                        P.dma('sp', iqT[idx * 128:(idx + 1) * 128, tsl], ob[:], reads=[obk], writes=[('iqT', idx, tb)])
                    elif kind == 'idxk':
                        ob, obk = next_ob('b')
                        plain_evac(ob[:], pa[:], [pk], [obk])
                        P.dma('sp', ikT2[:, tsl], ob[:], reads=[obk], writes=[('ikT2', tb)])
                    elif kind == 'mli':
                        ob, obk = next_ob('f')
                        P.op('dve', lambda e: e.tensor_scalar(ob[0:4, :], pa[0:4, :], smallp[0:4, 3:4], None, ALU.add), reads=[pk, 'smallp'], writes=[obk])
                        P.dma('sp', liT[:, tsl], ob[0:4, :], reads=[obk], writes=[('liT', tb)])
                    elif kind == 'mlf':
                        ob, obk = next_ob('f')
                        act(ob[0:4, :], pa[0:4, :], AF.Exp, [pk, 'smallp'], [obk], scale=-1.0, bias=smallp[0:4, 2:3])
                        act(ob[0:4, :], ob[0:4, :], AF.Ln, [obk, 'cf'], [obk], bias=onec[0:4, :])
                        P.op('dve', lambda e: e.tensor_scalar(ob[0:4, :], ob[0:4, :], -1.0, None, ALU.mult), reads=[obk], writes=[obk])
                        P.dma('sp', lfT[:, tsl], ob[0:4, :], reads=[obk], writes=[('lfT', tb)])

            def do_tm(gi):
                _, kind, c0, n, dst, off = groups[gi]
                wb = wbs[gi % 2]
                wk = ('wb', gi % 2)
                for t in range(NT):
                    pi = cnt['pa'] % 4
                    cnt['pa'] += 1
                    pa, pk = pas[pi], ('pa', pi)
                    for c in range(8):
                        mm(pa[:, 0:n], xnT[:, c, t * 128:(t + 1) * 128], wb[:, c, 0:n], c == 0, c == 7,
                           [wk, ('xnT', t // 4)], [pk])
                    rows = slice(t * 128, (t + 1) * 128)
                    if kind == 'idxw':
                        ob, obk = next_ob('f')
                        plain_evac(ob[:, 0:8], pa[:, 0:8], [pk], [obk], scale=8.0 ** -0.5)
                        P.dma('sp', dst[rows, :], ob[:, 0:8], reads=[obk], writes=[(kind, t)])
                        continue
                    ob, obk = next_ob('b')
                    if kind in ('mlv', 'glav', 'dsav'):
                        plain_evac(ob[:], pa[:], [pk], [obk])
                    elif kind == 'mlo':
                        act(ob[:], pa[:], AF.Sigmoid, [pk], [obk])
                    else:
                        act(ob[:], pa[:], AF.Silu, [pk], [obk])
                    P.dma('sp', dst[rows, off:off + 512], ob[:], reads=[obk], writes=[(kind, off, t)])

            load_w(0)
            for gi in range(len(groups)):
                if gi + 1 < len(groups):
                    load_w(gi + 1)
                if groups[gi][0] == 'fm':
                    do_fm(gi)
                else:
                    do_tm(gi)
            P.barrier()
        P.es = es

    def phaseB(l):
        with ExitStack() as ph:
            P.es = ph
            Cn = P.sbuf("Cn", [128, 4, 2, 257], F32)
            CnS = P.sbuf("CnS", [128, 4, 2, 257], BF16)
            mst = [P.sbuf("mst%d" % i, [4, 1], F32) for i in range(2)]
            gmB = P.sbuf("gmB", [128, 1024], F32)
            q4s = [P.sbuf("q4_%d" % i, [128, 8, 128], BF16) for i in range(2)]
            k4s = [P.sbuf("k4_%d" % i, [128, 8, 128], BF16) for i in range(2)]
            v1s = [P.sbuf("v1_%d" % i, [128, 4, 257], BF16) for i in range(2)]
            sgs = [P.sbuf("sg%d" % i, [128, 1024], BF16) for i in range(2)]
            zms = [P.sbuf("zm%d" % i, [128, 1024], BF16) for i in range(2)]
            glis = [P.sbuf("gli%d" % i, [4, 128], F32) for i in range(2)]
            glfs = [P.sbuf("glf%d" % i, [4, 128], F32) for i in range(2)]
            bc = P.sbuf("bcB", [4, 128], F32)
            aa = P.sbuf("aaB", [4, 128], F32)
            waT = P.sbuf("waT", [4, 128], F32)
            clT = P.sbuf("clT", [4, 128], F32)
            sm = P.sbuf("smB", [4, 8], F32)
            diagE = P.sbuf("diagE", [4, 4], F32)
            gt = P.sbuf("gtB", [128, 12], F32)
            sw = P.sbuf("swB", [128, 4, 128], BF16)
            kw = P.sbuf("kwB", [128, 4, 2, 128], BF16)
            hmo = P.sbuf("hmo", [128, 4, 256], F32)
            junk = P.sbuf("junkB", [128, 256], BF16)
            sml = P.sbuf("smlB", [128, 16], F32)
            Gz = P.sbuf("Gz", [128, 1024], F32)
            yts = [P.sbuf("ytB%d" % i, [128, 1024], BF16) for i in range(2)]
            g_ps = P.psum("g_ps", [128, 12], F32)
            st_ps = P.psum("st_ps", [128, 4, 128], F32)
            kt_ps = P.psum("kt_ps", [128, 8, 128], BF16)
            nds = [P.psum("nd%d" % i, [128, 257], F32) for i in range(2)]
            cus = [P.psum("cu%d" % i, [128, 257], F32) for i in range(2)]
            P.dma('sp', gmB[:], mlnB[l], writes=['gmB'])
            P.op('pool', lambda e: e.memset(Cn[:], 0.0), writes=['Cn'])
            P.op('pool', lambda e: e.memset(mst[0][:], 0.0), writes=[('mst', 0)])
            for i in range(2):
                P.op('pool', lambda e: e.memset(v1s[i][:, :, 256:257], 1.0), writes=[('v1', i)])
            qv = qkT[0:1024, :].rearrange("(g p) s -> p g s", p=128)
            kv = qkT[1024:2048, :].rearrange("(g p) s -> p g s", p=128)

            def loads(t):
                j = t % 2
                cols = slice(t * 128, (t + 1) * 128)
                P.dma('sp', q4s[j][:], qv[:, :, cols], writes=[('q4', j)])
                P.dma('sp', k4s[j][:], kv[:, :, cols], writes=[('k4', j)])
                P.dma('sp', v1s[j][:, :, 0:256], vml[cols, :].rearrange("p (h e) -> p h e", h=4), writes=[('v1', j)])
                P.dma('sp', sgs[j][:], sigo[cols, :], writes=[('sg', j)])
                P.dma('sp', zms[j][:], zml[cols, :], writes=[('zm', j)])
                P.dma('sp', glis[j][:], liT[:, cols], writes=[('gli', j)])
                P.dma('sp', glfs[j][:], lfT[:, cols], writes=[('glf', j)])

            loads(0)
            LN16 = float(np.log(16.0))
            for t in range(NT):
                j = t % 2
                if t + 1 < NT:
                    loads(t + 1)
                q4, k4, v1, sg, zm, gli, glf, yt = q4s[j], k4s[j], v1s[j], sgs[j], zms[j], glis[j], glfs[j], yts[j]
                mprev, mnext = mst[j], mst[1 - j]
                mk_, mnk = ('mst', j), ('mst', 1 - j)
                P.op('dve', lambda e: e.tensor_tensor_scan(bc[:], ones_f[0:4, 0:128], glf[:], 0.0, ALU.mult, ALU.add),
                     reads=['cf', ('glf', j)], writes=['bcB'])
                P.op('dve', lambda e: e.tensor_tensor(aa[:], gli[:], bc[:], ALU.subtract), reads=[('gli', j), 'bcB'], writes=['aaB'])
                P.op('dve', lambda e: e.tensor_reduce(sm[:, 0:1], aa[:], AX.X, ALU.max), reads=['aaB'], writes=['sm0'])
                P.op('dve', lambda e: e.tensor_tensor(sm[:, 1:2], sm[:, 0:1], mprev[:], ALU.max), reads=['sm0', mk_], writes=['sm1'])
                P.op('dve', lambda e: e.tensor_scalar(sm[:, 2:3], sm[:, 1:2], -1.0, None, ALU.mult), reads=['sm1'], writes=['sm2'])
                P.op('dve', lambda e: e.tensor_scalar(sm[:, 3:4], sm[:, 1:2], -1.0, -LN16, ALU.mult, ALU.add), reads=['sm1'], writes=['sm3'])
                act(sm[:, 4:5], mprev[:], AF.Exp, [mk_, 'sm2'], ['sm4'], bias=sm[:, 2:3])
                P.op('dve', lambda e: e.tensor_tensor(mnext[:], bc[:, 127:128], sm[:, 1:2], ALU.add), reads=['bcB', 'sm1'], writes=[mnk])
                act(waT[:], aa[:], AF.Exp, ['aaB', 'sm3'], ['waT'], bias=sm[:, 3:4])
                act(clT[:], bc[:], AF.Exp, ['bcB', 'sm2'], ['clT'], scale=-1.0, bias=sm[:, 2:3])
                P.op('dve', lambda e: e.tensor_scalar(diagE[:], ident_f[0:4, 0:4], sm[:, 4:5], None, ALU.mult), reads=['cf', 'sm4'], writes=['diagE'])
                mm(g_ps[:, 0:4], waT[:], ident_f[0:4, 0:4], True, True, ['waT', 'cf'], ['g_ps'])
                mm(g_ps[:, 4:8], clT[:], ident_f[0:4, 0:4], True, True, ['clT', 'cf'], ['g_ps'])
                mm(g_ps[:, 8:12], ones_f[0:4, 0:128], diagE[:], True, True, ['diagE', 'cf'], ['g_ps'])
                P.op('dve', lambda e: e.tensor_copy(gt[:], g_ps[:]), reads=['g_ps'], writes=['gt'])
                P.op('pool', lambda e: e.memset(sml[:, 0:4], 0.0), writes=['ssq'])
                for h in range(4):
                    P.op('act', lambda e: e.mul(CnS[:, h, :, :], Cn[:, h, :, :], gt[:, 8 + h:9 + h]), reads=['Cn', 'gt'], writes=[('CnS', h)])
                for h in range(4):
                    for dc in range(2):
                        mm(st_ps[:, h, :], k4[:, 2 * h + dc, :], q4[:, 2 * h + dc, :], dc == 0, dc == 1, [('k4', j), ('q4', j)], [('st_ps', h)])
                    P.op('dve', lambda e: e.scalar_tensor_tensor(sw[:, h, :], st_ps[:, h, :], gt[:, h:h + 1], maskT_f, ALU.mult, ALU.mult),
                         reads=[('st_ps', h), 'gt', 'cf'], writes=[('sw', h)])
                    for dc in range(2):
                        P.op('pe', lambda e: e.transpose(kt_ps[:, 2 * h + dc, :], k4[:, 2 * h + dc, :], ident_b), reads=[('k4', j), 'cb'], writes=[('kt_ps', h)])
                    P.op('act', lambda e: e.mul(kw[:, h, :, :], kt_ps[:, 2 * h:2 * h + 2, :], gt[:, h:h + 1]), reads=[('kt_ps', h), 'gt'], writes=[('kw', h)])
                    nd, ndk = nds[h % 2], ('nd', h % 2)
                    mm(nd[:, :], sw[:, h, :], v1[:, h, :], True, False, [('sw', h), ('v1', j)], [ndk])
                    for dc in range(2):
                        mm(nd[:, :], q4[:, 2 * h + dc, :], CnS[:, h, dc, :], False, dc == 1, [('q4', j), ('CnS', h)], [ndk])
                    for dc in range(2):
                        cu, cuk = cus[dc], ('cu', dc)
                        mm(cu[:, :], kw[:, h, dc, :], v1[:, h, :], True, True, [('kw', h), ('v1', j)], [cuk])
                        P.op('dve', lambda e: e.scalar_tensor_tensor(Cn[:, h, dc, :], Cn[:, h, dc, :], gt[:, 8 + h:9 + h], cu[:, :], ALU.mult, ALU.add),
                             reads=['Cn', 'gt', cuk, ('CnS', h)], writes=['Cn'])
                    act(sml[:, 4 + h:5 + h], nd[:, 256:257], AF.Abs, [ndk], [('dn', h)])
                    P.op('dve', lambda e: e.tensor_tensor(sml[:, 4 + h:5 + h], sml[:, 4 + h:5 + h], gt[:, 4 + h:5 + h], ALU.max),
                         reads=[('dn', h), 'gt'], writes=[('dn', h)])
                    P.op('dve', lambda e: e.reciprocal(sml[:, 8 + h:9 + h], sml[:, 4 + h:5 + h]), reads=[('dn', h)], writes=[('rd', h)])
                    P.op('dve', lambda e: e.scalar_tensor_tensor(hmo[:, h, :], nd[:, 0:256], sml[:, 8 + h:9 + h], sg[:, h * 256:(h + 1) * 256], ALU.mult, ALU.mult),
                         reads=[ndk, ('rd', h), ('sg', j)], writes=[('hmo', h)])
                    act(junk[:], hmo[:, h, :], AF.Square, [('hmo', h), 'ssq'], ['junkB', 'ssq'], accum_out=sml[:, h:h + 1])
                act(sml[:, 12:16], sml[:, 0:4], AF.Ln, ['ssq', 'cf'], ['rstdB'], scale=1.0 / 256, bias=epsc)
                act(sml[:, 12:16], sml[:, 12:16], AF.Exp, ['rstdB'], ['rstdB'], scale=-0.5)
                P.op('pool', lambda e: e.tensor_tensor(Gz[:], zm[:], gmB[:], ALU.mult), reads=[('zm', j), 'gmB'], writes=['Gz'])
                for h in range(4):
                    P.op('dve', lambda e: e.scalar_tensor_tensor(yt[:, h * 256:(h + 1) * 256], hmo[:, h, :], sml[:, 12 + h:13 + h], Gz[:, h * 256:(h + 1) * 256], ALU.mult, ALU.mult),
                         reads=[('hmo', h), 'rstdB', 'Gz'], writes=[('ytB', j)])
                P.dma('sp', ycat[t * 128:(t + 1) * 128, 0:1024], yt[:], reads=[('ytB', j)], writes=[('ycat', 0, t)])
            P.barrier()
        P.es = es

    def phaseC(l):
        with ExitStack() as ph:
            P.es = ph
            Sst = P.sbuf("Sst", [64, 4, 128], F32)
            Sbf = P.sbuf("Sbf", [64, 4, 128], BF16)
            tmpS = P.sbuf("tmpS", [64, 4, 128], F32)
            ggB = P.sbuf("ggB", [128, 512], F32)
            gqs = [P.sbuf("gq%d" % i, [64, 4, 128], F32) for i in range(2)]
            gks = [P.sbuf("gk%d" % i, [64, 4, 128], F32) for i in range(2)]
            las = [P.sbuf("la%d" % i, [64, 4, 128], F32) for i in range(2)]
            vgs = [P.sbuf("vgt%d" % i, [128, 512], BF16) for i in range(2)]
            rgs = [P.sbuf("rgt%d" % i, [128, 512], BF16) for i in range(2)]
            bc = P.sbuf("bcC", [64, 4, 128], F32)
            eb = P.sbuf("ebC", [64, 4, 128], F32)
            enb = P.sbuf("enbC", [64, 4, 128], F32)
            qt = P.sbuf("qtC", [64, 4, 128], BF16)
            kt = P.sbuf("ktC", [64, 4, 128], BF16)
            am = P.sbuf("amC", [128, 4, 128], BF16)
            ktk = P.sbuf("ktkC", [128, 4, 64], BF16)
            osq = P.sbuf("osqC", [128, 4, 128], F32)
            sml = P.sbuf("smlC", [128, 8], F32)
            Gr = P.sbuf("GrC", [128, 512], F32)
            ygs = [P.sbuf("yg%d" % i, [128, 512], BF16) for i in range(2)]
            at_ps = P.psum("at_ps", [128, 4, 128], F32)
            ktk_ps = P.psum("ktk_ps", [128, 4, 64], BF16)
            o_ps = P.psum("o_psC", [128, 4, 128], F32)
            su_ps = P.psum("su_ps", [64, 4, 128], F32)
            P.dma('sp', ggB[:], glnB[l], writes=['ggB'])
            P.op('pool', lambda e: e.memset(Sst[:], 0.0), writes=['Sst'])

            def loads(t):
                j = t % 2
                cols = slice(t * 128, (t + 1) * 128)
                P.dma('sp', gqs[j][:], gqT[:, :, cols].rearrange("h p s -> p h s"), writes=[('gq', j)])
                P.dma('sp', gks[j][:], gkT[:, :, cols].rearrange("h p s -> p h s"), writes=[('gk', j)])
                P.dma('sp', las[j][:], laT[:, :, cols].rearrange("h p s -> p h s"), writes=[('la', j)])
                P.dma('sp', vgs[j][:], vg[cols, :], writes=[('vgt', j)])
                P.dma('sp', rgs[j][:], rg[cols, :], writes=[('rgt', j)])

            loads(0)
            for t in range(NT):
                j = t % 2
                if t + 1 < NT:
                    loads(t + 1)
                gq, gk, la, vgt, rgt, yg = gqs[j], gks[j], las[j], vgs[j], rgs[j], ygs[j]
                for h in range(4):
                    P.op('dve', lambda e: e.tensor_tensor_scan(bc[:, h, :], ones_f[0:64, 0:128], la[:, h, :], 0.0, ALU.mult, ALU.add),
                         reads=['cf', ('la', j)], writes=['bcC'])
                act(eb[:], bc[:], AF.Exp, ['bcC'], ['ebC'])
                act(enb[:], bc[:], AF.Exp, ['bcC'], ['enbC'], scale=-1.0)
                P.op('pool', lambda e: e.tensor_tensor(qt[:], gq[:], eb[:], ALU.mult), reads=[('gq', j), 'ebC'], writes=['qtC'])
                P.op('dve', lambda e: e.tensor_tensor(kt[:], gk[:], enb[:], ALU.mult), reads=[('gk', j), 'enbC'], writes=['ktC'])
                for h in range(4):
                    mm(at_ps[:, h, :], kt[:, h, :], qt[:, h, :], True, True, ['ktC', 'qtC'], ['at_ps'])
                P.op('dve', lambda e: e.tensor_tensor(am[:], at_ps[:], mask4_b.rearrange("p (h s) -> p h s", h=4), ALU.mult),
                     reads=['at_ps', 'cb'], writes=['amC'])
                for h in range(4):
                    P.op('pe', lambda e: e.transpose(ktk_ps[:, h, :], kt[:, h, :], ident_b[0:64, 0:64]), reads=['ktC', 'cb'], writes=['ktk_ps'])
                P.op('act', lambda e: e.copy(ktk[:], ktk_ps[:]), reads=['ktk_ps'], writes=['ktkC'])
                P.op('pool', lambda e: e.tensor_copy(Sbf[:], Sst[:]), reads=['Sst'], writes=['Sbf'])
                for h in range(4):
                    mm(o_ps[:, h, :], am[:, h, :], vgt[:, h * 128:(h + 1) * 128], True, False, ['amC', ('vgt', j)], ['o_psC'])
                    mm(o_ps[:, h, :], qt[:, h, :], Sbf[:, h, :], False, True, ['qtC', 'Sbf'], ['o_psC'])
                for h in range(4):
                    mm(su_ps[:, h, :], ktk[:, h, :], vgt[:, h * 128:(h + 1) * 128], True, True, ['ktkC', ('vgt', j)], ['su_ps'])
                P.op('dve', lambda e: e.tensor_tensor(tmpS[:], Sst[:], su_ps[:], ALU.add), reads=['Sst', 'su_ps', 'Sbf'], writes=['tmpS'])
                for h in range(4):
                    P.op('dve', lambda e: e.tensor_scalar(Sst[:, h, :], tmpS[:, h, :], eb[:, h, 127:128], None, ALU.mult),
                         reads=['tmpS', 'ebC', 'Sbf'], writes=['Sst'])
                act(osq[:], o_ps[:], AF.Square, ['o_psC'], ['osqC'])
                P.op('dve', lambda e: e.tensor_reduce(sml[:, 0:4], osq[:], AX.X, ALU.add), reads=['osqC'], writes=['ssqC'])
                act(sml[:, 4:8], sml[:, 0:4], AF.Ln, ['ssqC', 'cf'], ['rstdC'], scale=1.0 / 128, bias=epsc)
                act(sml[:, 4:8], sml[:, 4:8], AF.Exp, ['rstdC'], ['rstdC'], scale=-0.5)
                P.op('pool', lambda e: e.tensor_tensor(Gr[:], rgt[:], ggB[:], ALU.mult), reads=[('rgt', j), 'ggB'], writes=['GrC'])
                for h in range(4):
                    P.op('dve', lambda e: e.scalar_tensor_tensor(yg[:, h * 128:(h + 1) * 128], o_ps[:, h, :], sml[:, 4 + h:5 + h], Gr[:, h * 128:(h + 1) * 128], ALU.mult, ALU.mult),
                         reads=['o_psC', 'rstdC', 'GrC'], writes=[('yg', j)])
                P.dma('sp', ycat[t * 128:(t + 1) * 128, 1024:1536], yg[:], reads=[('yg', j)], writes=[('ycat', 1, t)])
            P.barrier()
        P.es = es

    def phaseD(l):
        with ExitStack() as ph:
            P.es = ph
            ikt = P.sbuf("ikt", [128, S], BF16)
            scores = [P.sbuf("score%d" % i, [128, S], F32) for i in range(2)]
            junkd = P.sbuf("junkD", [128, S], BF16)
            junka = P.sbuf("junkA2", [128, S], BF16)
            mbs = [P.sbuf("mb%d" % i, [128, S], BF16) for i in range(2)]
            iqts = [P.sbuf("iqt%d" % i, [128, 4, 128], BF16) for i in range(2)]
            iwts = [P.sbuf("iwt%d" % i, [128, 8], F32) for i in range(2)]
            dWs = [P.sbuf("dW%d" % i, [128, 8, 128], BF16) for i in range(2)]
            rbufs = [P.sbuf("rbuf%d" % i, [128, 512], BF16) for i in range(4)]
            sms = [P.sbuf("smD%d" % i, [128, 16], F32) for i in range(2)]
            wtabs = [P.sbuf("wtab%d" % i, [128, 32], F32) for i in range(2)]
            lgs = [P.psum("lg%d" % i, [128, 512], F32) for i in range(4)]
            scs = [P.psum("sc%d" % i, [128, 512], F32) for i in range(2)]
            for c in range(0, S, 1024):
                P.dma('sp', ikt[:, c:c + 1024], ikT2[:, c:c + 1024], writes=['ikt'])
            iqv = iqT.rearrange("(g p) s -> p g s", p=128)

            def loads(t):
                j = t % 2
                cols = slice(t * 128, (t + 1) * 128)
                P.dma('sp', iqts[j][:], iqv[:, :, cols], writes=[('iqt', j)])
                P.dma('sp', iwts[j][:], iwS[cols, :], writes=[('iwt', j)])

            ci = {'lg': 0, 'sc': 0, 'rb': 0}

            def score_tile(t):
                j = t % 2
                iqt, iwt, dW, score = iqts[j], iwts[j], dWs[j], scores[j]
                sk_ = ('score', j)
                nk = 128 * (t + 1)
                nkb = (nk + 511) // 512
                for h in range(8):
                    P.op('pool', lambda e: e.tensor_scalar(dW[:, h, :], ident_b, iwt[:, h:h + 1], None, ALU.mult),
                         reads=['cb', ('iwt', j)], writes=[('dW', j)])
                for kb in range(nkb):
                    sc, sck = scs[ci['sc'] % 2], ('sc', ci['sc'] % 2)
                    ci['sc'] += 1
                    for h in range(8):
                        lg, lgk = lgs[ci['lg'] % 4], ('lg', ci['lg'] % 4)
                        ci['lg'] += 1
                        rb, rbk = rbufs[ci['rb'] % 4], ('rb', ci['rb'] % 4)
                        ci['rb'] += 1
                        po = (h % 2) * 64
                        mm(lg[:, :], iqt[po:po + 64, h // 2, :], ikt[po:po + 64, kb * 512:(kb + 1) * 512], True, True,
                           [('iqt', j), 'ikt'], [lgk])
                        if h % 2:
                            act(rb[:], lg[:], AF.Relu, [lgk], [rbk])
                        else:
                            P.op('dve', lambda e: e.tensor_scalar(rb[:], lg[:], 0.0, None, ALU.max), reads=[lgk], writes=[rbk])
                        mm(sc[:, :], dW[:, h, :], rb[:], h == 0, h == 7, [('dW', j), rbk], [sck])
                    P.op('act', lambda e: e.copy(score[:, kb * 512:(kb + 1) * 512], sc[:, :]), reads=[sck], writes=[sk_])

            def prelude(t):
                j = t % 2
                score, sm, wtab = scores[j], sms[j], wtabs[j]
                sk_, smk, wk = ('score', j), ('smD', j), ('wtab', j)
                nk = 128 * (t + 1)
                P.op('dve', lambda e: e.tensor_reduce(sm[:, 0:1], score[:, 0:nk], AX.X, ALU.max), reads=[sk_], writes=[smk])
                P.op('dve', lambda e: e.tensor_reduce(sm[:, 1:2], score[:, 0:nk], AX.X, ALU.min), reads=[sk_], writes=[smk])
                P.op('pool', lambda e: e.memset(score[0:64, nk - 64:nk], -1.0e30), reads=[smk], writes=[sk_])
                P.op('dve', lambda e: e.tensor_tensor(sm[:, 2:3], sm[:, 0:1], sm[:, 1:2], ALU.subtract), reads=[smk], writes=[smk])
                P.op('dve', lambda e: e.tensor_scalar(sm[:, 3:4], sm[:, 2:3], 1.02, 2.0e-6, ALU.mult, ALU.add), reads=[smk], writes=[smk])
                P.op('dve', lambda e: e.tensor_scalar(wtab[:], cf[:, CF_PW:CF_PW + 32], sm[:, 3:4], None, ALU.mult), reads=[smk, 'cf'], writes=[wk])
                P.op('dve', lambda e: e.scalar_tensor_tensor(sm[:, 4:5], sm[:, 2:3], -0.01, sm[:, 1:2], ALU.mult, ALU.add), reads=[smk], writes=[smk])
                P.op('dve', lambda e: e.tensor_scalar(sm[:, 4:5], sm[:, 4:5], -1.0e-6, None, ALU.add), reads=[smk], writes=[smk])
                P.op('dve', lambda e: e.tensor_tensor(sm[:, 4:5], sm[:, 4:5], wtab[:, 0:1], ALU.add), reads=[smk, wk], writes=[('mid', j)])

            def split(nk):
                nD = (int(nk * 0.44) // 64) * 64
                nD = max(64, min(nk - 64, nD))
                return nD, nk - nD

            def count(t):
                j = t % 2
                score, sm = scores[j], sms[j]
                nk = 128 * (t + 1)
                nD, nA = split(nk)
                P.op('dve', lambda e: e.tensor_scalar(junkd[:, 0:nD], score[:, 0:nD], sm[:, 4:5], None, ALU.is_ge, ALU.add, accum_out=sm[:, 5:6]),
                     reads=[('score', j), ('mid', j)], writes=['junkD', ('cntD', j)])
                act(junka[:, 0:nA], score[:, nD:nk], AF.Sign, [('score', j), ('mid', j)], ['junkA2', ('sA', j)],
                    scale=-1.0, bias=sm[:, 4:5], accum_out=sm[:, 8:9])

            def update(t, it):
                j = t % 2
                sm, wtab = sms[j], wtabs[j]
                nk = 128 * (t + 1)
                nD, nA = split(nk)
                P.op('dve', lambda e: e.scalar_tensor_tensor(sm[:, 9:10], sm[:, 8:9], -0.5, sm[:, 5:6], ALU.mult, ALU.add),
                     reads=[('sA', j), ('cntD', j)], writes=[('t1', j)])
                P.op('dve', lambda e: e.tensor_scalar(sm[:, 6:7], sm[:, 9:10], TOPK - 0.5 - nA / 2.0, 0.5, ALU.is_ge, ALU.subtract),
                     reads=[('t1', j)], writes=[('sg', j)])
                P.op('dve', lambda e: e.scalar_tensor_tensor(sm[:, 4:5], sm[:, 6:7], wtab[:, it:it + 1], sm[:, 4:5], ALU.mult, ALU.add),
                     reads=[('sg', j), ('wtab', j), ('mid', j)], writes=[('mid', j)])

            def finish_tile(t):
                j = t % 2
                score, sm, wtab, mb = scores[j], sms[j], wtabs[j], mbs[j]
                nk = 128 * (t + 1)
                P.op('dve', lambda e: e.tensor_tensor(sm[:, 7:8], sm[:, 4:5], wtab[:, NBIS - 1:NBIS], ALU.subtract), reads=[('mid', j), ('wtab', j)], writes=[('thr', j)])
                h0 = (nk // 2 // 64) * 64
                P.op('dve', lambda e: e.tensor_scalar(mb[:, 0:h0], score[:, 0:h0], sm[:, 7:8], NEG, ALU.is_lt, ALU.mult), reads=[('score', j), ('thr', j)], writes=[('mb', j)])
                P.op('pool', lambda e: e.tensor_scalar(mb[:, h0:nk], score[:, h0:nk], sm[:, 7:8], NEG, ALU.is_lt, ALU.mult), reads=[('score', j), ('thr', j)], writes=[('mb', j)])
                P.dma('sp', MB[t, :, 0:nk], mb[:, 0:nk], reads=[('mb', j)], writes=[('MB', t)])

            loads(0)
            loads(1)
            for p in range(NT // 2):
                tt = (2 * p, 2 * p + 1)
                for t in tt:
                    score_tile(t)
                for t in tt:
                    if t + 2 < NT:
                        loads(t + 2)
                for t in tt:
                    prelude(t)
                for it in range(NBIS):
                    for t in tt:
                        count(t)
                    for t in tt:
                        update(t, it)
                for t in tt:
                    finish_tile(t)
            P.barrier()
        with ExitStack() as ph:
            P.es = ph
            KT = P.sbuf("KT", [128, 4, S], BF16)
            V1 = P.sbuf("V1", [128, NT, 4, 129], BF16)
            ohs = P.sbuf("ohs", [128, len(_OHIDX), 128], BF16)
            relb = P.sbuf("relb", [128, 128], F32)
            biasT = P.sbuf("biasT", [128, 2, 4, 128], F32)
            ec = P.sbuf("ecD", [128, 4], F32)
            qhs = [P.sbuf("qh%d" % i, [128, 4, 128], BF16) for i in range(2)]
            mbts = [P.sbuf("mbt%d" % i, [128, S], BF16) for i in range(2)]
            zdts = [P.sbuf("zdt%d" % i, [128, 512], BF16) for i in range(2)]
            pTs = [P.sbuf("pT%d" % i, [128, 4, 128], BF16) for i in range(3)]
            tmps = [P.sbuf("tmpD%d" % i, [128, 4, 128], F32) for i in range(2)]
            on_sb = P.sbuf("on_sb", [128, 4, 129], F32)
            O = P.sbuf("O_D", [128, 4, 129], F32)
            rden = P.sbuf("rdenD", [128, 4], F32)
            yds = [P.sbuf("yd%d" % i, [128, 512], BF16) for i in range(2)]
            s_pss = [P.psum("s_ps%d" % i, [128, 4, 128], F32) for i in range(2)]
            ofs = [P.psum("of%d" % i, [128, 2, 129], F32) for i in range(2)]
            ons = [P.psum("on%d" % i, [128, 2, 129], F32) for i in range(2)]
            for h in range(4):
                P.dma('sp', KT[:, h, :], KhT[h], writes=['KT'])
            P.op('pool', lambda e: e.memset(V1[:, :, :, 128:129], 1.0), writes=['V1'])
            for u0 in range(NT):
                P.dma('sp', V1[:, u0, :, 0:128], vd[u0 * 128:(u0 + 1) * 128, :].rearrange("p (h e) -> p h e", h=4), writes=['V1'])
            P.dma('sp', ohs[:], oh_in, writes=['ohs'])
            P.dma('sp', relb[:], relB, writes=['relb'])
            P.op('pool', lambda e: e.memset(biasT[:], 0.0), writes=['biasT'])
            for i, (r, b) in enumerate(_OHIDX):
                for h in range(4):
                    P.op('dve', lambda e: e.scalar_tensor_tensor(biasT[:, r, h, :], ohs[:, i, :], relb[:, b * 4 + h:b * 4 + h + 1], biasT[:, r, h, :], ALU.mult, ALU.add),
                         reads=['ohs', 'relb', 'biasT'], writes=['biasT'])
            act(ec[:], relb[:, 60:64], AF.Exp, ['relb'], ['ecD'])

            def loads(t):
                j = t % 2
                cols = slice(t * 128, (t + 1) * 128)
                nk = 128 * (t + 1)
                P.dma('sp', qhs[j][:], QhT[:, :, cols].rearrange("h p s -> p h s"), writes=[('qh', j)])
                P.dma('sp', mbts[j][:, 0:nk], MB[t, :, 0:nk], writes=[('mbt', j)])
                P.dma('sp', zdts[j][:], zd[cols, :], writes=[('zdt', j)])

            loads(0)
            ci = {'s': 0, 'p': 0, 'tmp': 0}
            for t in range(NT):
                j = t % 2
                if t + 1 < NT:
                    loads(t + 1)
                qh, mbt, zdt, yd = qhs[j], mbts[j], zdts[j], yds[j]
                far_last = t - 2
                near_first = max(0, t - 1)
                def qk_exp(u):
                    near = u >= t - 1
                    s_ps, sk = s_pss[ci['s'] % 2], ('s_ps', ci['s'] % 2)
                    ci['s'] += 1
                    pT, pk = pTs[ci['p'] % 3], ('pT', ci['p'] % 3)
                    ci['p'] += 1
                    for h in range(4):
                        mm(s_ps[:, h, :], KT[:, h, u * 128:(u + 1) * 128], qh[:, h, :], True, False, ['KT', ('qh', j)], [sk])
                        mm(s_ps[:, h, :], mbt[:, u * 128:(u + 1) * 128], ident_b, False, True, [('mbt', j), 'cb'], [sk])
                    if near:
                        tmp, tk = tmps[ci['tmp'] % 2], ('tmpD', ci['tmp'] % 2)
                        ci['tmp'] += 1
                        P.op('dve', lambda e: e.tensor_tensor(tmp[:], s_ps[:], biasT[:, t - u, :, :], ALU.add), reads=[sk, 'biasT'], writes=[tk])
                        act(pT[:], tmp[:], AF.Exp, [tk], [pk])
                    else:
                        act(pT[:], s_ps[:], AF.Exp, [sk], [pk])
                    return pT, pk

                def pv(u, pT, pk):
                    near = u >= t - 1
                    for h in range(4):
                        if near:
                            o_, ok_ = ons[h // 2], ('on', h // 2)
                            first, last = (u == near_first), (u == t)
                        else:
                            o_, ok_ = ofs[h // 2], ('of', h // 2)
                            first, last = (u == 0), (u == far_last)
                        mm(o_[:, h % 2, :], pT[:, h, :], V1[:, u, h, :], first and (h % 2 == 0), last, [pk, 'V1'], [ok_])

                prev = None
                for u in range(t + 1):
                    cur = qk_exp(u)
                    if prev is not None:
                        pv(u - 1, *prev)
                    prev = cur
                pv(t, *prev)
                for hh in range(2):
                    P.op('act', lambda e: e.copy(on_sb[:, 2 * hh:2 * hh + 2, :], ons[hh][:]), reads=[('on', hh)], writes=[('on_sb', hh)])
                for h in range(4):
                    if t >= 2:
                        P.op('dve', lambda e: e.scalar_tensor_tensor(O[:, h, :], ofs[h // 2][:, h % 2, :], ec[:, h:h + 1], on_sb[:, h, :], ALU.mult, ALU.add),
                             reads=[('of', h // 2), 'ecD', ('on_sb', h // 2)], writes=[('O', h)])
                        Oh = O
                        Ok = ('O', h)
                    else:
                        Oh = on_sb
                        Ok = ('on_sb', h // 2)
                    P.op('dve', lambda e: e.reciprocal(rden[:, h:h + 1], Oh[:, h, 128:129]), reads=[Ok], writes=[('rden', h)])
                    P.op('dve', lambda e: e.scalar_tensor_tensor(yd[:, h * 128:(h + 1) * 128], Oh[:, h, 0:128], rden[:, h:h + 1], zdt[:, h * 128:(h + 1) * 128], ALU.mult, ALU.mult),
                         reads=[Ok, ('rden', h), ('zdt', j)], writes=[('yd', j)])
                P.dma('sp', ycat[t * 128:(t + 1) * 128, 1536:2048], yd[:], reads=[('yd', j)], writes=[('ycat', 2, t)])
            P.barrier()
        P.es = es

    def phaseE(l, src, dst):
        with ExitStack() as ph:
            P.es = ph
            wo = P.sbuf("wo", [128, 16, 1024], BF16)
            wf2 = P.sbuf("wf2", [128, 4, 1024], F32)
            yts = [P.sbuf("ytE%d" % i, [128, 2048], BF16) for i in range(2)]
            yTs = [P.sbuf("yTE%d" % i, [128, 16, 128], BF16) for i in range(2)]
            xts = [P.sbuf("xtE%d" % i, [128, 1024], F32) for i in range(2)]
            ots = [P.sbuf("otE%d" % i, [128, 1024], F32) for i in range(2)]
            tpe = [P.psum("tpe%d" % i, [128, 8, 128], BF16) for i in range(2)]
            oe = [P.psum("oe%d" % i, [128, 512], F32) for i in range(2)]
            wv = w_out[l].rearrange("(c p) n -> p c n", p=128)
            for q in range(4):
                P.dma('sp', wf2[:], wv[:, 4 * q:4 * q + 4, :], writes=['wf2'])
                P.op('pool', lambda e: e.tensor_copy(wo[:, 4 * q:4 * q + 4, :], wf2[:]), reads=['wf2'], writes=['wo'])

            def loads(t):
                j = t % 2
                rows = slice(t * 128, (t + 1) * 128)
                P.dma('sp', yts[j][:], ycat[rows, :], writes=[('ytE', j)])
                P.dma('sp', xts[j][:], src[rows, :], writes=[('xtE', j)])

            loads(0)
            for t in range(NT):
                j = t % 2
                if t + 1 < NT:
                    loads(t + 1)
                yt, yT, xt, ot = yts[j], yTs[j], xts[j], ots[j]
                for c in range(16):
                    P.op('pe', lambda e: e.transpose(tpe[c // 8][:, c % 8, :], yt[:, c * 128:(c + 1) * 128], ident_b),
                         reads=[('ytE', j), 'cb'], writes=[('tpe', c // 8)])
                P.op('act', lambda e: e.copy(yT[:, 0:8, :], tpe[0][:]), reads=[('tpe', 0)], writes=[('yTE', j)])
                P.op('dve', lambda e: e.tensor_copy(yT[:, 8:16, :], tpe[1][:]), reads=[('tpe', 1)], writes=[('yTE', j)])
                for half in range(2):
                    for c in range(16):
                        mm(oe[half][:, :], yT[:, c, :], wo[:, c, half * 512:(half + 1) * 512], c == 0, c == 15, [('yTE', j), 'wo'], [('oe', half)])
                    P.op('dve', lambda e: e.tensor_tensor(ot[:, half * 512:(half + 1) * 512], oe[half][:, :], xt[:, half * 512:(half + 1) * 512], ALU.add),
                         reads=[('oe', half), ('xtE', j)], writes=[('otE', j)])
                P.dma('sp', dst[t * 128:(t + 1) * 128, :], ot[:], reads=[('otE', j)], writes=[('dst', t)])
            P.barrier()
        P.es = es

    k = K()
    k.__dict__.update(locals())
    return k


def prep_shared(inputs):
    f = lambda a: np.ascontiguousarray(np.asarray(a, dtype=np.float32))
    cf, cb, oh, _ = host_consts()
    d = {}
    d["w_in"] = f(inputs["w_in"])
    d["w_out"] = f(inputs["w_out"])
    d["normB"] = f(np.broadcast_to(np.asarray(inputs["norm_g"])[:, None, :], (2, 128, 1024)))
    d["convwT"] = f(np.concatenate([np.transpose(np.asarray(inputs["ml_conv_w"]), (0, 2, 1)),
                                    np.asarray(inputs["ml_conv_b"])[:, :, None]], axis=2))
    d["ml_b_i"] = f(np.asarray(inputs["ml_b_i"])[:, :, None])
    d["ml_b_f"] = f(np.asarray(inputs["ml_b_f"])[:, :, None])
    d["mlnB"] = f(np.broadcast_to(np.asarray(inputs["ml_norm_g"])[:, None, :], (2, 128, 1024)))
    d["gla_w_a"] = f(inputs["gla_w_a"])
    d["gla_b_aT"] = f(np.transpose(np.asarray(inputs["gla_b_a"]).reshape(2, 4, 64), (0, 2, 1)))
    d["glnB"] = f(np.broadcast_to(np.asarray(inputs["gla_norm_g"])[:, None, :], (2, 128, 512)))
    d["dsa_q_g"] = f(np.asarray(inputs["dsa_q_g"])[:, :, None])
    d["dsa_k_g"] = f(np.asarray(inputs["dsa_k_g"])[:, :, None])
    d["relB"] = f(np.broadcast_to(np.asarray(inputs["rel_bias"]).reshape(1, 128), (128, 128)))
    d["cf"] = cf
    d["cb"] = cb
    d["oh"] = oh
    return d


def emit(k, phases="ABCDE", layers=(0,)):
    for l in layers:
        src = k.x_in if l == 0 else k.h1
        if "A" in phases:
            k.phaseA(l, src)
        if "B" in phases:
            k.phaseB(l)
        if "C" in phases:
            k.phaseC(l)
        if "D" in phases:
            k.phaseD(l)
        if "E" in phases:
            k.phaseE(l, src, k.h1 if l == 0 else k.out)


_CACHE = {}


def kernel(**inputs):
    x = np.asarray(inputs["x"], dtype=np.float32)
    B, S, D = x.shape
    key = (S,)
    if key not in _CACHE:
        k = build(S, depth=2, debug=False)
        emit(k, "ABCDE", layers=(0, 1))
        k.P.finish()
        _CACHE[key] = k
    k = _CACHE[key]
    shared = prep_shared(inputs)
    in_maps = []
    for b in range(B):
        d = dict(shared)
        d["x"] = np.ascontiguousarray(x[b])
        in_maps.append(d)
    res = run_bass_kernel_spmd(k.nc, in_maps, core_ids=list(range(B)))
    return np.stack([np.asarray(r["out"], dtype=np.float32) for r in res.results], axis=0)
```

```python
import numpy as np
import ml_dtypes
from contextlib import ExitStack
import concourse.bass as bass
import concourse.mybir as mybir
from concourse.bass_utils import run_bass_kernel_spmd

F32 = mybir.dt.float32
BF16 = mybir.dt.bfloat16
AF = mybir.ActivationFunctionType
ALU = mybir.AluOpType
AX = mybir.AxisListType


class Prog:
    ENG = ['pe', 'act', 'dve', 'pool', 'sp']

    def __init__(self, nc, es, n_dma_sems=40):
        self.nc = nc
        self.es = es
        self.es0 = es
        self.sem = {e: es.enter_context(nc.semaphore('s_' + e)) for e in self.ENG}
        self.dsem = [es.enter_context(nc.semaphore('d%d' % i)) for i in range(n_dma_sems)]
        self.cnt = {e: 0 for e in self.ENG}
        self.dcnt = 0
        self.dval = [0] * n_dma_sems
        self.waited = {e: {} for e in self.ENG}
        self.ops = {e: [] for e in self.ENG}
        self.lastw = {}
        self.readers = {}
        self.nops = 0
        self._e_pe = nc.tensor
        self._e_act = nc.scalar
        self._e_dve = nc.vector
        self._e_pool = nc.gpsimd
        self._e_sp = nc.sync

    def sbuf(self, name, shape, dtype):
        self.uid = getattr(self, 'uid', 0) + 1
        return self.es.enter_context(self.nc.sbuf_tensor("sb%d_%s" % (self.uid, name), shape, dtype))

    def psum(self, name, shape, dtype):
        self.uid = getattr(self, 'uid', 0) + 1
        return self.es.enter_context(self.nc.psum_tensor("ps%d_%s" % (self.uid, name), shape, dtype))

    def _wait(self, eng, tok):
        if tok is None:
            return
        kind, who, val = tok
        if kind == 'e' and who == 'pe' and eng == 'pe':
            return
        key = (kind, who)
        if self.waited[eng].get(key, 0) >= val:
            return
        self.waited[eng][key] = val
        sem = self.sem[who] if kind == 'e' else self.dsem[who]
        getattr(self, '_e_' + eng).wait_ge(sem, val)

    def _deps(self, eng, reads, writes):
        for k in reads:
            self._wait(eng, self.lastw.get(k))
        for k in writes:
            self._wait(eng, self.lastw.get(k))
            for t in self.readers.get(k, ()):
                self._wait(eng, t)

    def _commit(self, tok, reads, writes):
        for k in writes:
            self.lastw[k] = tok
            self.readers[k] = []
        for k in reads:
            if k in writes:
                continue
            self.readers.setdefault(k, []).append(tok)

    def op(self, eng, fn, reads=(), writes=()):
        self._deps(eng, reads, writes)
        self.cnt[eng] += 1
        tok = ('e', eng, self.cnt[eng])
        fn(getattr(self, '_e_' + eng)).then_inc(self.sem[eng], 1)
        self._commit(tok, reads, writes)
        self.nops += 1

    def dma(self, q, out, in_, reads=(), writes=(), **kw):
        n = len(self.dsem)
        idx = self.dcnt % n
        self.dcnt += 1
        if self.dval[idx] > 0:
            self._wait(q, ('d', idx, self.dval[idx]))
        self._deps(q, reads, writes)
        self.dval[idx] += 16
        tok = ('d', idx, self.dval[idx])
        getattr(self, '_e_' + q).dma_start(out=out, in_=in_, **kw).then_inc(self.dsem[idx], 16)
        self._commit(tok, reads, writes)
        self.nops += 1

    def finish(self):
        for idx, v in enumerate(self.dval):
            if v > 0:
                self._wait('sp', ('d', idx, v))
        for e in ['pe', 'act', 'dve', 'pool']:
            if self.cnt[e] > 0:
                self._wait('sp', ('e', e, self.cnt[e]))

    def barrier(self):
        for e in self.ENG:
            for o in ['pe', 'act', 'dve', 'pool']:
                if self.cnt[o] > 0 and not (e == o):
                    self._wait(e, ('e', o, self.cnt[o]))
            for idx, v in enumerate(self.dval):
                if v > 0:
                    self._wait(e, ('d', idx, v))
        if not hasattr(self, 'bar'):
            self.bar = self.es0.enter_context(self.nc.semaphore('s_bar'))
            self.go = self.es0.enter_context(self.nc.semaphore('s_go'))
            self.epoch = 0
        self.epoch += 1
        comp = ['pe', 'act', 'dve', 'pool']
        for e in comp:
            getattr(self, '_e_' + e).sem_inc(self.bar, 1)
        sp = self._e_sp
        sp.wait_ge(self.bar, 4 * self.epoch)
        for e in comp:
            sp.sem_clear(self.sem[e])
        for d in self.dsem:
            sp.sem_clear(d)
        sp.sem_inc(self.go, 1)
        for e in comp:
            getattr(self, '_e_' + e).wait_ge(self.go, self.epoch)
        for e in comp:
            self.cnt[e] = 0
        self.dval = [0] * len(self.dsem)
        self.waited = {e: {} for e in self.ENG}
        self.lastw = {}
        self.readers = {}


C_MLQ, C_MLK, C_MLV, C_MLO, C_MLZ = 0, 1024, 2048, 3072, 4096
C_MLI, C_MLF = 5120, 5124
C_GQ, C_GK, C_GV, C_GA, C_GR = 5128, 5384, 5640, 6152, 6168
C_DQ, C_DK, C_DV, C_DZ = 6680, 7192, 7704, 8216
C_IQ, C_IK, C_IW = 8728, 9240, 9304
N_IN = 9312
EPS = 1e-6
NBIS = 24
NEG = -30000.0

CF_ID, CF_ONES, CF_MASK, CF_PW, CF_MISC = 0, 128, 256, 384, 416
NCF = 432
CB_ID, CB_MASK4 = 0, 128
NCB = 640


def t5_bucket_np(rel):
    half, max_exact = 16, 8
    ret = np.where(rel > 0, half, 0)
    n = np.abs(rel)
    nf = np.maximum(n, 1).astype(np.float32)
    large = max_exact + (np.log(nf / max_exact) / np.float32(np.log(128 / max_exact)) * (half - max_exact)).astype(np.int32)
    large = np.minimum(large, half - 1)
    return ret + np.where(n < max_exact, n, large)


def host_consts():
    cf = np.zeros((128, NCF), np.float32)
    cf[:, CF_ID:CF_ID + 128] = np.eye(128, dtype=np.float32)
    cf[:, CF_ONES:CF_ONES + 128] = 1.0
    s = np.arange(128)
    maskT = (s[:, None] <= s[None, :]).astype(np.float32)
    cf[:, CF_MASK:CF_MASK + 128] = maskT
    cf[:, CF_PW:CF_PW + 32] = (0.5 ** (np.arange(32) + 1))[None, :]
    cf[:, CF_MISC + 0] = EPS
    cf[:, CF_MISC + 1] = 1.0
    cf[:, CF_MISC + 2] = 0.0
    cb = np.zeros((128, NCB), np.float32)
    cb[:, CB_ID:CB_ID + 128] = np.eye(128)
    cb[:, CB_MASK4:CB_MASK4 + 512] = np.tile(maskT, (1, 4))
    k = np.arange(128)[:, None]
    q = np.arange(128)[None, :]
    oh = []
    ohidx = []
    for r in (0, 1):
        b = t5_bucket_np((k - q - 128 * r).astype(np.int32))
        for bb in np.unique(b):
            oh.append((b == bb).astype(np.float32))
            ohidx.append((r, int(bb)))
    oh = np.stack(oh, 1)
    return cf, cb.astype(ml_dtypes.bfloat16), oh.astype(ml_dtypes.bfloat16), ohidx


_OHIDX = host_consts()[3]


class K:
    pass


def build(S, depth=2, debug=False, stop_after=None):
    NT = S // 128
    NB = S // 512
    TOPK = min(256, S // 4)
    nc = bass.Bass("TRN2", target_bir_lowering=False)

    def din(name, shape, dt=F32):
        return nc.dram_tensor(name, list(shape), dt, kind="ExternalInput").ap()

    def dscr(name, shape, dt=F32):
        return nc.dram_tensor(name, list(shape), dt, kind=("ExternalOutput" if debug else "Internal")).ap()

    x_in = din("x", [S, 1024])
    w_in = din("w_in", [2, 1024, N_IN])
    w_out = din("w_out", [2, 2048, 1024])
    normB = din("normB", [2, 128, 1024])
    convwT = din("convwT", [2, 2048, 5])
    ml_b_i = din("ml_b_i", [2, 4, 1])
    ml_b_f = din("ml_b_f", [2, 4, 1])
    mlnB = din("mlnB", [2, 128, 1024])
    gla_w_a = din("gla_w_a", [2, 16, 256])
    gla_b_aT = din("gla_b_aT", [2, 64, 4])
    glnB = din("glnB", [2, 128, 512])
    dsa_q_g = din("dsa_q_g", [2, 128, 1])
    dsa_k_g = din("dsa_k_g", [2, 128, 1])
    relB = din("relB", [128, 128])
    cf_in = din("cf", [128, NCF])
    cb_in = din("cb", [128, NCB], BF16)
    oh_in = din("oh", [128, len(_OHIDX), 128], BF16)
    out = nc.dram_tensor("out", [S, 1024], F32, kind="ExternalOutput").ap()

    h1 = dscr("h1", [S, 1024])
    qkT = dscr("qkT", [2048, S], BF16)
    vml = dscr("vml", [S, 1024], BF16)
    sigo = dscr("sigo", [S, 1024], BF16)
    zml = dscr("zml", [S, 1024], BF16)
    liT = dscr("liT", [4, S])
    lfT = dscr("lfT", [4, S])
    gqT = dscr("gqT", [4, 64, S])
    gkT = dscr("gkT", [4, 64, S])
    laT = dscr("laT", [4, 64, S])
    vg = dscr("vg", [S, 512], BF16)
    rg = dscr("rg", [S, 512], BF16)
    QhT = dscr("QhT", [4, 128, S], BF16)
    KhT = dscr("KhT", [4, 128, S], BF16)
    vd = dscr("vd", [S, 512], BF16)
    zd = dscr("zd", [S, 512], BF16)
    iqT = dscr("iqT", [512, S], BF16)
    ikT2 = dscr("ikT2", [128, S], BF16)
    iwS = dscr("iw", [S, 8])
    MB = dscr("MB", [NT, 128, S], BF16)
    ycat = dscr("ycat", [S, 2048], BF16)

    es = ExitStack()
    P = Prog(nc, es)
    cf = P.sbuf("cf", [128, NCF], F32)
    cb = P.sbuf("cb", [128, NCB], BF16)
    P.dma('sp', cf[:], cf_in, writes=['cf'])
    P.dma('sp', cb[:], cb_in, writes=['cb'])
    ident_f = cf[:, CF_ID:CF_ID + 128]
    ones_f = cf[:, CF_ONES:CF_ONES + 128]
    maskT_f = cf[:, CF_MASK:CF_MASK + 128]
    epsc = cf[:, CF_MISC:CF_MISC + 1]
    onec = cf[:, CF_MISC + 1:CF_MISC + 2]
    ident_b = cb[:, CB_ID:CB_ID + 128]
    mask4_b = cb[:, CB_MASK4:CB_MASK4 + 512]

    def act(outp, inp, func, r, w, **kw):
        P.op('act', lambda e: e.activation(outp, inp, func, **kw), reads=r, writes=w)

    def mm(outp, lhsT, rhs, start, stop, r, w):
        P.op('pe', lambda e: e.matmul(outp, lhsT, rhs, start=start, stop=stop), reads=r, writes=w)

    def phaseA(l, src):
        with ExitStack() as ph:
            P.es = ph
            xnT = P.sbuf("xnT", [128, 8, S], BF16)
            gB = P.sbuf("gB", [128, 1024], F32)
            P.dma('sp', gB[:], normB[l], writes=['gB'])
            with ExitStack() as ph1:
                P.es = ph1
                xts = [P.sbuf("xt%d" % i, [128, 1024], F32) for i in range(2)]
                xns = [P.sbuf("xn%d" % i, [128, 1024], BF16) for i in range(2)]
                junk = P.sbuf("junkA", [128, 1024], BF16)
                ss = [P.sbuf("ssA%d" % i, [128, 1], F32) for i in range(2)]
                tps = [P.psum("tpA%d" % i, [128, 8, 128], BF16) for i in range(2)]
                for t in range(NT):
                    j = t % 2
                    xt, xn, tp, s1 = xts[j], xns[j], tps[j], ss[j]
                    P.dma('sp', xt[:], src[t * 128:(t + 1) * 128, :], writes=[('xt', j)])
                    P.op('pool', lambda e: e.memset(s1[:], 0.0), writes=[('ss', j)])
                    act(junk[:], xt[:], AF.Square, [('xt', j)], ['junkA', ('ss', j)], accum_out=s1[:, 0:1])
                    act(s1[:], s1[:], AF.Ln, [('ss', j), 'cf'], [('ss', j)], scale=1.0 / 1024, bias=epsc)
                    act(s1[:], s1[:], AF.Exp, [('ss', j)], [('ss', j)], scale=-0.5)
                    P.op('dve', lambda e: e.scalar_tensor_tensor(xn[:], xt[:], s1[:, 0:1], gB[:], ALU.mult, ALU.mult),
                         reads=[('xt', j), ('ss', j), 'gB'], writes=[('xn', j)])
                    for c in range(8):
                        P.op('pe', lambda e: e.transpose(tp[:, c, :], xn[:, c * 128:(c + 1) * 128], ident_b),
                             reads=[('xn', j), 'cb'], writes=[('tp', j)])
                    P.op('dve' if j else 'act', (lambda e: e.tensor_copy(xnT[:, :, t * 128:(t + 1) * 128], tp[:])) if j else
                         (lambda e: e.copy(xnT[:, :, t * 128:(t + 1) * 128], tp[:])),
                         reads=[('tp', j)], writes=[('xnT', t // 4)])
            P.barrier()
            P.es = ph
            wf = P.sbuf("wf", [128, 8, 512], F32)
            wbs = [P.sbuf("wb%d" % i, [128, 8, 512], BF16) for i in range(2)]
            sts = [P.sbuf("st%d" % i, [128, 515], F32) for i in range(2)]
            accs = [P.sbuf("acc%d" % i, [128, 512], F32) for i in range(2)]
            obf = [P.sbuf("obf%d" % i, [128, 512], F32) for i in range(3)]
            obb = [P.sbuf("obb%d" % i, [128, 512], BF16) for i in range(3)]
            cws = [P.sbuf("cw%d" % i, [128, 5], F32) for i in range(2)]
            smallp = P.sbuf("smallp", [128, 16], F32)
            wab_f = P.sbuf("wab_f", [16, 256], F32)
            wab = P.sbuf("wab", [16, 256], BF16)
            aTb = [P.sbuf("aTb%d" % i, [16, 512], BF16) for i in range(2)]
            pas = [P.psum("paA%d" % i, [128, 512], F32) for i in range(4)]
            pzs = [P.psum("pzA%d" % i, [128, 512], F32) for i in range(2)]
            wl = w_in[l].rearrange("(c p) n -> p c n", p=128)
            P.dma('sp', smallp[:, 0:1], dsa_q_g[l], writes=['smallp'])
            P.dma('sp', smallp[:, 1:2], dsa_k_g[l], writes=['smallp'])
            P.dma('sp', smallp[0:4, 2:3], ml_b_f[l], writes=['smallp'])
            P.dma('sp', smallp[0:4, 3:4], ml_b_i[l], writes=['smallp'])
            P.dma('sp', smallp[0:64, 4:8], gla_b_aT[l], writes=['smallp'])
            P.dma('sp', wab_f[:], gla_w_a[l], writes=['wab_f'])
            P.op('dve', lambda e: e.tensor_scalar(smallp[:, 0:1], smallp[:, 0:1], 128.0 ** -0.5, None, ALU.mult), reads=['smallp'], writes=['smallp'])
            P.op('dve', lambda e: e.tensor_scalar(smallp[0:4, 2:3], smallp[0:4, 2:3], -1.0, None, ALU.mult), reads=['smallp'], writes=['smallp'])
            P.op('dve', lambda e: e.tensor_scalar(smallp[0:64, 4:8], smallp[0:64, 4:8], -1.0, None, ALU.mult), reads=['smallp'], writes=['smallp'])
            P.op('dve', lambda e: e.tensor_copy(wab[:], wab_f[:]), reads=['wab_f'], writes=['wab'])

            fm = []
            for g in range(8):
                fm.append(('mlq', C_MLQ + g * 128, 128, g))
            for g in range(8):
                fm.append(('mlk', C_MLK + g * 128, 128, 8 + g))
            for h in range(4):
                fm.append(('glaq', C_GQ + h * 64, 64, h))
            for h in range(4):
                fm.append(('glak', C_GK + h * 64, 64, h))
            fm.append(('glaa', C_GA, 16, 0))
            for h in range(4):
                fm.append(('dsaq', C_DQ + h * 128, 128, h))
            for h in range(4):
                fm.append(('dsak', C_DK + h * 128, 128, h))
            for g in range(4):
                fm.append(('idxq', C_IQ + g * 128, 128, g))
            fm.append(('idxk', C_IK, 64, 0))
            fm.append(('mli', C_MLI, 4, 0))
            fm.append(('mlf', C_MLF, 4, 0))
            tm = [('mlv', C_MLV, 512, vml, 0), ('mlv', C_MLV + 512, 512, vml, 512),
                  ('mlo', C_MLO, 512, sigo, 0), ('mlo', C_MLO + 512, 512, sigo, 512),
                  ('mlz', C_MLZ, 512, zml, 0), ('mlz', C_MLZ + 512, 512, zml, 512),
                  ('glav', C_GV, 512, vg, 0), ('glar', C_GR, 512, rg, 0),
                  ('dsav', C_DV, 512, vd, 0), ('dsaz', C_DZ, 512, zd, 0), ('idxw', C_IW, 8, iwS, 0)]
            groups = [('fm',) + g for g in fm] + [('tm',) + g for g in tm]
            cnt = {'ev': 0, 'ob': 0, 'pa': 0, 'pz': 0, 'cw': 0, 'at': 0}

            def load_w(gi):
                g = groups[gi]
                c0, n = g[2], g[3]
                if g[1] == 'idxk':
                    P.dma('sp', wf[:, :, 0:64], wl[:, :, c0:c0 + 64], writes=['wf'])
                    P.dma('sp', wf[:, :, 64:128], wl[:, :, c0:c0 + 64], writes=['wf'])
                    n = 128
                else:
                    P.dma('sp', wf[:, :, 0:n], wl[:, :, c0:c0 + n], writes=['wf'])
                wb = wbs[gi % 2]
                P.op('pool', lambda e: e.tensor_copy(wb[:, :, 0:n], wf[:, :, 0:n]), reads=['wf'], writes=[('wb', gi % 2)])

            def next_ob(kind):
                i = cnt['ob'] % 3
                cnt['ob'] += 1
                return (obf if kind == 'f' else obb)[i], ('obf' if kind == 'f' else 'obb', i)

            def plain_evac(dst, srcp, r, w, scale=None):
                i = cnt['ev']
                cnt['ev'] += 1
                if scale is not None:
                    if i % 2:
                        P.op('act', lambda e: e.mul(dst, srcp, scale), reads=r, writes=w)
                    else:
                        P.op('dve', lambda e: e.tensor_scalar(dst, srcp, scale, None, ALU.mult), reads=r, writes=w)
                else:
                    if i % 2:
                        P.op('act', lambda e: e.copy(dst, srcp), reads=r, writes=w)
                    else:
                        P.op('dve', lambda e: e.tensor_copy(dst, srcp), reads=r, writes=w)

            def do_fm(gi):
                _, kind, c0, n, idx = groups[gi]
                wb = wbs[gi % 2]
                wk = ('wb', gi % 2)
                M = 128 if kind == 'idxk' else n
                if kind in ('mlq', 'mlk'):
                    cw = cws[cnt['cw'] % 2]
                    cwk = ('cw', cnt['cw'] % 2)
                    cnt['cw'] += 1
                    P.dma('sp', cw[:], convwT[l][idx * 128:(idx + 1) * 128, :], writes=[cwk])
                for tb in range(NB):
                    pi = cnt['pa'] % 4
                    cnt['pa'] += 1
                    pa = pas[pi]
                    pk = ('pa', pi)
                    for c in range(8):
                        mm(pa[0:M, :], wb[:, c, 0:M], xnT[:, c, tb * 512:(tb + 1) * 512], c == 0, c == 7,
                           [wk, ('xnT', tb)], [pk])
                    tsl = slice(tb * 512, (tb + 1) * 512)
                    if kind in ('mlq', 'mlk'):
                        st, stk = sts[tb % 2], ('st', tb % 2)
                        pst, pstk = sts[(tb + 1) % 2], ('st', (tb + 1) % 2)
                        acc, acck = accs[tb % 2], ('acc', tb % 2)
                        P.op('act', lambda e: e.copy(st[:, 3:515], pa[:, :]), reads=[pk], writes=[stk])
                        if tb == 0:
                            P.op('pool', lambda e: e.memset(st[:, 0:3], 0.0), writes=[stk])
                        else:
                            P.op('pool', lambda e: e.tensor_copy(st[:, 0:3], pst[:, 512:515]), reads=[pstk], writes=[stk])
                        P.op('dve', lambda e: e.tensor_scalar(acc[:], st[:, 0:512], cw[:, 0:1], cw[:, 4:5], ALU.mult, ALU.add),
                             reads=[stk, cwk], writes=[acck])
                        for j in (1, 2, 3):
                            P.op('dve', lambda e: e.scalar_tensor_tensor(acc[:], st[:, j:j + 512], cw[:, j:j + 1], acc[:], ALU.mult, ALU.add),
                                 reads=[stk, cwk, acck], writes=[acck])
                        ob, obk = next_ob('b')
                        act(ob[:], acc[:], AF.Silu, [acck], [obk])
                        P.dma('sp', qkT[idx * 128:(idx + 1) * 128, tsl], ob[:], reads=[obk], writes=[('qkT', idx, tb)])
                    elif kind == 'glaq':
                        ob, obk = next_ob('f')
                        plain_evac(ob[0:64, :], pa[0:64, :], [pk], [obk], scale=0.125)
                        P.dma('sp', gqT[idx, :, tsl], ob[0:64, :], reads=[obk], writes=[('gqT', idx, tb)])
                    elif kind == 'glak':
                        ob, obk = next_ob('f')
                        plain_evac(ob[0:64, :], pa[0:64, :], [pk], [obk])
                        P.dma('sp', gkT[idx, :, tsl], ob[0:64, :], reads=[obk], writes=[('gkT', idx, tb)])
                    elif kind == 'glaa':
                        at = aTb[cnt['at'] % 2]
                        atk = ('aTb', cnt['at'] % 2)
                        cnt['at'] += 1
                        P.op('act', lambda e: e.copy(at[:, :], pa[0:16, :]), reads=[pk], writes=[atk])
                        for h in range(4):
                            zi = cnt['pz'] % 2
                            cnt['pz'] += 1
                            pz, pzk = pzs[zi], ('pz', zi)
                            mm(pz[0:64, :], wab[:, h * 64:(h + 1) * 64], at[:, :], True, True, ['wab', atk], [pzk])
                            ob, obk = next_ob('f')
                            act(ob[0:64, :], pz[0:64, :], AF.Exp, [pzk, 'smallp'], [obk], scale=-1.0, bias=smallp[0:64, 4 + h:5 + h])
                            act(ob[0:64, :], ob[0:64, :], AF.Ln, [obk, 'cf'], [obk], bias=onec[0:64, :])
                            P.op('dve', lambda e: e.tensor_scalar(ob[0:64, :], ob[0:64, :], -1.0 / 16.0, None, ALU.mult), reads=[obk], writes=[obk])
                            P.dma('sp', laT[h, :, tsl], ob[0:64, :], reads=[obk], writes=[('laT', h, tb)])
                    elif kind in ('dsaq', 'dsak'):
                        sq, sqk = next_ob('f')
                        act(sq[:], pa[:], AF.Square, [pk], [sqk])
                        zi = cnt['pz'] % 2
                        cnt['pz'] += 1
                        pz, pzk = pzs[zi], ('pz', zi)
                        mm(pz[:, :], ones_f, sq[:], True, True, ['cf', sqk], [pzk])
                        act(sq[:], pz[:], AF.Ln, [pzk, 'cf'], [sqk], scale=1.0 / 128, bias=epsc)
                        act(sq[:], sq[:], AF.Exp, [sqk], [sqk], scale=-0.5)
                        ob, obk = next_ob('b')
                        gcol = 0 if kind == 'dsaq' else 1
                        P.op('dve', lambda e: e.scalar_tensor_tensor(ob[:], pa[:], smallp[:, gcol:gcol + 1], sq[:], ALU.mult, ALU.mult),
                             reads=[pk, 'smallp', sqk], writes=[obk])
                        dstT = QhT if kind == 'dsaq' else KhT
                        P.dma('sp', dstT[idx, :, tsl], ob[:], reads=[obk], writes=[(kind, idx, tb)])
                    elif kind == 'idxq':
                        ob, obk = next_ob('b')
                        plain_evac(ob[:], pa[:], [pk], [obk], scale=0.125)
                        P.dma('sp', iqT[idx * 128:(idx + 1) * 128, tsl], ob[:], reads=[obk], writes=[('iqT', idx, tb)])
                    elif kind == 'idxk':
                        ob, obk = next_ob('b')
                        plain_evac(ob[:], pa[:], [pk], [obk])
                        P.dma('sp', ikT2[:, tsl], ob[:], reads=[obk], writes=[('ikT2', tb)])
                    elif kind == 'mli':
                        ob, obk = next_ob('f')
                        P.op('dve', lambda e: e.tensor_scalar(ob[0:4, :], pa[0:4, :], smallp[0:4, 3:4], None, ALU.add), reads=[pk, 'smallp'], writes=[obk])
                        P.dma('sp', liT[:, tsl], ob[0:4, :], reads=[obk], writes=[('liT', tb)])
                    elif kind == 'mlf':
                        ob, obk = next_ob('f')
                        act(ob[0:4, :], pa[0:4, :], AF.Exp, [pk, 'smallp'], [obk], scale=-1.0, bias=smallp[0:4, 2:3])
                        act(ob[0:4, :], ob[0:4, :], AF.Ln, [obk, 'cf'], [obk], bias=onec[0:4, :])
                        P.op('dve', lambda e: e.tensor_scalar(ob[0:4, :], ob[0:4, :], -1.0, None, ALU.mult), reads=[obk], writes=[obk])
                        P.dma('sp', lfT[:, tsl], ob[0:4, :], reads=[obk], writes=[('lfT', tb)])

            def do_tm(gi):
                _, kind, c0, n, dst, off = groups[gi]
                wb = wbs[gi % 2]
                wk = ('wb', gi % 2)
                for t in range(NT):
                    pi = cnt['pa'] % 4
                    cnt['pa'] += 1
                    pa, pk = pas[pi], ('pa', pi)
                    for c in range(8):
                        mm(pa[:, 0:n], xnT[:, c, t * 128:(t + 1) * 128], wb[:, c, 0:n], c == 0, c == 7,
                           [wk, ('xnT', t // 4)], [pk])
                    rows = slice(t * 128, (t + 1) * 128)
                    if kind == 'idxw':
                        ob, obk = next_ob('f')
                        plain_evac(ob[:, 0:8], pa[:, 0:8], [pk], [obk], scale=8.0 ** -0.5)
                        P.dma('sp', dst[rows, :], ob[:, 0:8], reads=[obk], writes=[(kind, t)])
                        continue
                    ob, obk = next_ob('b')
                    if kind in ('mlv', 'glav', 'dsav'):
                        plain_evac(ob[:], pa[:], [pk], [obk])
                    elif kind == 'mlo':
                        act(ob[:], pa[:], AF.Sigmoid, [pk], [obk])
                    else:
                        act(ob[:], pa[:], AF.Silu, [pk], [obk])
                    P.dma('sp', dst[rows, off:off + 512], ob[:], reads=[obk], writes=[(kind, off, t)])

            load_w(0)
            for gi in range(len(groups)):
                if gi + 1 < len(groups):
                    load_w(gi + 1)
                if groups[gi][0] == 'fm':
                    do_fm(gi)
                else:
                    do_tm(gi)
            P.barrier()
        P.es = es

    def phaseB(l):
        with ExitStack() as ph:
            P.es = ph
            Cn = P.sbuf("Cn", [128, 4, 2, 257], F32)
            CnS = P.sbuf("CnS", [128, 4, 2, 257], BF16)
            mst = [P.sbuf("mst%d" % i, [4, 1], F32) for i in range(2)]
            gmB = P.sbuf("gmB", [128, 1024], F32)
            q4s = [P.sbuf("q4_%d" % i, [128, 8, 128], BF16) for i in range(2)]
            k4s = [P.sbuf("k4_%d" % i, [128, 8, 128], BF16) for i in range(2)]
            v1s = [P.sbuf("v1_%d" % i, [128, 4, 257], BF16) for i in range(2)]
            sgs = [P.sbuf("sg%d" % i, [128, 1024], BF16) for i in range(2)]
            zms = [P.sbuf("zm%d" % i, [128, 1024], BF16) for i in range(2)]
            glis = [P.sbuf("gli%d" % i, [4, 128], F32) for i in range(2)]
            glfs = [P.sbuf("glf%d" % i, [4, 128], F32) for i in range(2)]
            bc = P.sbuf("bcB", [4, 128], F32)
            aa = P.sbuf("aaB", [4, 128], F32)
            waT = P.sbuf("waT", [4, 128], F32)
            clT = P.sbuf("clT", [4, 128], F32)
            sm = P.sbuf("smB", [4, 8], F32)
            diagE = P.sbuf("diagE", [4, 4], F32)
            gt = P.sbuf("gtB", [128, 12], F32)
            sw = P.sbuf("swB", [128, 4, 128], BF16)
            kw = P.sbuf("kwB", [128, 4, 2, 128], BF16)
            hmo = P.sbuf("hmo", [128, 4, 256], F32)
            junk = P.sbuf("junkB", [128, 256], BF16)
            sml = P.sbuf("smlB", [128, 16], F32)
            Gz = P.sbuf("Gz", [128, 1024], F32)
            yts = [P.sbuf("ytB%d" % i, [128, 1024], BF16) for i in range(2)]
            g_ps = P.psum("g_ps", [128, 12], F32)
            st_ps = P.psum("st_ps", [128, 4, 128], F32)
            kt_ps = P.psum("kt_ps", [128, 8, 128], BF16)
            nds = [P.psum("nd%d" % i, [128, 257], F32) for i in range(2)]
            cus = [P.psum("cu%d" % i, [128, 257], F32) for i in range(2)]
            P.dma('sp', gmB[:], mlnB[l], writes=['gmB'])
            P.op('pool', lambda e: e.memset(Cn[:], 0.0), writes=['Cn'])
            P.op('pool', lambda e: e.memset(mst[0][:], 0.0), writes=[('mst', 0)])
            for i in range(2):
                P.op('pool', lambda e: e.memset(v1s[i][:, :, 256:257], 1.0), writes=[('v1', i)])
            qv = qkT[0:1024, :].rearrange("(g p) s -> p g s", p=128)
            kv = qkT[1024:2048, :].rearrange("(g p) s -> p g s", p=128)

            def loads(t):
                j = t % 2
                cols = slice(t * 128, (t + 1) * 128)
                P.dma('sp', q4s[j][:], qv[:, :, cols], writes=[('q4', j)])
                P.dma('sp', k4s[j][:], kv[:, :, cols], writes=[('k4', j)])
                P.dma('sp', v1s[j][:, :, 0:256], vml[cols, :].rearrange("p (h e) -> p h e", h=4), writes=[('v1', j)])
                P.dma('sp', sgs[j][:], sigo[cols, :], writes=[('sg', j)])
                P.dma('sp', zms[j][:], zml[cols, :], writes=[('zm', j)])
                P.dma('sp', glis[j][:], liT[:, cols], writes=[('gli', j)])
                P.dma('sp', glfs[j][:], lfT[:, cols], writes=[('glf', j)])

            loads(0)
            LN16 = float(np.log(16.0))
            for t in range(NT):
                j = t % 2
                if t + 1 < NT:
                    loads(t + 1)
                q4, k4, v1, sg, zm, gli, glf, yt = q4s[j], k4s[j], v1s[j], sgs[j], zms[j], glis[j], glfs[j], yts[j]
                mprev, mnext = mst[j], mst[1 - j]
                mk_, mnk = ('mst', j), ('mst', 1 - j)
                P.op('dve', lambda e: e.tensor_tensor_scan(bc[:], ones_f[0:4, 0:128], glf[:], 0.0, ALU.mult, ALU.add),
                     reads=['cf', ('glf', j)], writes=['bcB'])
                P.op('dve', lambda e: e.tensor_tensor(aa[:], gli[:], bc[:], ALU.subtract), reads=[('gli', j), 'bcB'], writes=['aaB'])
                P.op('dve', lambda e: e.tensor_reduce(sm[:, 0:1], aa[:], AX.X, ALU.max), reads=['aaB'], writes=['sm0'])
                P.op('dve', lambda e: e.tensor_tensor(sm[:, 1:2], sm[:, 0:1], mprev[:], ALU.max), reads=['sm0', mk_], writes=['sm1'])
                P.op('dve', lambda e: e.tensor_scalar(sm[:, 2:3], sm[:, 1:2], -1.0, None, ALU.mult), reads=['sm1'], writes=['sm2'])
                P.op('dve', lambda e: e.tensor_scalar(sm[:, 3:4], sm[:, 1:2], -1.0, -LN16, ALU.mult, ALU.add), reads=['sm1'], writes=['sm3'])
                act(sm[:, 4:5], mprev[:], AF.Exp, [mk_, 'sm2'], ['sm4'], bias=sm[:, 2:3])
                P.op('dve', lambda e: e.tensor_tensor(mnext[:], bc[:, 127:128], sm[:, 1:2], ALU.add), reads=['bcB', 'sm1'], writes=[mnk])
                act(waT[:], aa[:], AF.Exp, ['aaB', 'sm3'], ['waT'], bias=sm[:, 3:4])
                act(clT[:], bc[:], AF.Exp, ['bcB', 'sm2'], ['clT'], scale=-1.0, bias=sm[:, 2:3])
                P.op('dve', lambda e: e.tensor_scalar(diagE[:], ident_f[0:4, 0:4], sm[:, 4:5], None, ALU.mult), reads=['cf', 'sm4'], writes=['diagE'])
                mm(g_ps[:, 0:4], waT[:], ident_f[0:4, 0:4], True, True, ['waT', 'cf'], ['g_ps'])
                mm(g_ps[:, 4:8], clT[:], ident_f[0:4, 0:4], True, True, ['clT', 'cf'], ['g_ps'])
                mm(g_ps[:, 8:12], ones_f[0:4, 0:128], diagE[:], True, True, ['diagE', 'cf'], ['g_ps'])
                P.op('dve', lambda e: e.tensor_copy(gt[:], g_ps[:]), reads=['g_ps'], writes=['gt'])
                P.op('pool', lambda e: e.memset(sml[:, 0:4], 0.0), writes=['ssq'])
                for h in range(4):
                    P.op('act', lambda e: e.mul(CnS[:, h, :, :], Cn[:, h, :, :], gt[:, 8 + h:9 + h]), reads=['Cn', 'gt'], writes=[('CnS', h)])
                for h in range(4):
                    for dc in range(2):
                        mm(st_ps[:, h, :], k4[:, 2 * h + dc, :], q4[:, 2 * h + dc, :], dc == 0, dc == 1, [('k4', j), ('q4', j)], [('st_ps', h)])
                    P.op('dve', lambda e: e.scalar_tensor_tensor(sw[:, h, :], st_ps[:, h, :], gt[:, h:h + 1], maskT_f, ALU.mult, ALU.mult),
                         reads=[('st_ps', h), 'gt', 'cf'], writes=[('sw', h)])
                    for dc in range(2):
                        P.op('pe', lambda e: e.transpose(kt_ps[:, 2 * h + dc, :], k4[:, 2 * h + dc, :], ident_b), reads=[('k4', j), 'cb'], writes=[('kt_ps', h)])
                    P.op('act', lambda e: e.mul(kw[:, h, :, :], kt_ps[:, 2 * h:2 * h + 2, :], gt[:, h:h + 1]), reads=[('kt_ps', h), 'gt'], writes=[('kw', h)])
                    nd, ndk = nds[h % 2], ('nd', h % 2)
                    mm(nd[:, :], sw[:, h, :], v1[:, h, :], True, False, [('sw', h), ('v1', j)], [ndk])
                    for dc in range(2):
                        mm(nd[:, :], q4[:, 2 * h + dc, :], CnS[:, h, dc, :], False, dc == 1, [('q4', j), ('CnS', h)], [ndk])
                    for dc in range(2):
                        cu, cuk = cus[dc], ('cu', dc)
                        mm(cu[:, :], kw[:, h, dc, :], v1[:, h, :], True, True, [('kw', h), ('v1', j)], [cuk])
                        P.op('dve', lambda e: e.scalar_tensor_tensor(Cn[:, h, dc, :], Cn[:, h, dc, :], gt[:, 8 + h:9 + h], cu[:, :], ALU.mult, ALU.add),
                             reads=['Cn', 'gt', cuk, ('CnS', h)], writes=['Cn'])
                    act(sml[:, 4 + h:5 + h], nd[:, 256:257], AF.Abs, [ndk], [('dn', h)])
                    P.op('dve', lambda e: e.tensor_tensor(sml[:, 4 + h:5 + h], sml[:, 4 + h:5 + h], gt[:, 4 + h:5 + h], ALU.max),
                         reads=[('dn', h), 'gt'], writes=[('dn', h)])
                    P.op('dve', lambda e: e.reciprocal(sml[:, 8 + h:9 + h], sml[:, 4 + h:5 + h]), reads=[('dn', h)], writes=[('rd', h)])
                    P.op('dve', lambda e: e.scalar_tensor_tensor(hmo[:, h, :], nd[:, 0:256], sml[:, 8 + h:9 + h], sg[:, h * 256:(h + 1) * 256], ALU.mult, ALU.mult),
                         reads=[ndk, ('rd', h), ('sg', j)], writes=[('hmo', h)])
                    act(junk[:], hmo[:, h, :], AF.Square, [('hmo', h), 'ssq'], ['junkB', 'ssq'], accum_out=sml[:, h:h + 1])
                act(sml[:, 12:16], sml[:, 0:4], AF.Ln, ['ssq', 'cf'], ['rstdB'], scale=1.0 / 256, bias=epsc)
                act(sml[:, 12:16], sml[:, 12:16], AF.Exp, ['rstdB'], ['rstdB'], scale=-0.5)
                P.op('pool', lambda e: e.tensor_tensor(Gz[:], zm[:], gmB[:], ALU.mult), reads=[('zm', j), 'gmB'], writes=['Gz'])
                for h in range(4):
                    P.op('dve', lambda e: e.scalar_tensor_tensor(yt[:, h * 256:(h + 1) * 256], hmo[:, h, :], sml[:, 12 + h:13 + h], Gz[:, h * 256:(h + 1) * 256], ALU.mult, ALU.mult),
                         reads=[('hmo', h), 'rstdB', 'Gz'], writes=[('ytB', j)])
                P.dma('sp', ycat[t * 128:(t + 1) * 128, 0:1024], yt[:], reads=[('ytB', j)], writes=[('ycat', 0, t)])
            P.barrier()
        P.es = es

    def phaseC(l):
        with ExitStack() as ph:
            P.es = ph
            Sst = P.sbuf("Sst", [64, 4, 128], F32)
            Sbf = P.sbuf("Sbf", [64, 4, 128], BF16)
            tmpS = P.sbuf("tmpS", [64, 4, 128], F32)
            ggB = P.sbuf("ggB", [128, 512], F32)
            gqs = [P.sbuf("gq%d" % i, [64, 4, 128], F32) for i in range(2)]
            gks = [P.sbuf("gk%d" % i, [64, 4, 128], F32) for i in range(2)]
            las = [P.sbuf("la%d" % i, [64, 4, 128], F32) for i in range(2)]
            vgs = [P.sbuf("vgt%d" % i, [128, 512], BF16) for i in range(2)]
            rgs = [P.sbuf("rgt%d" % i, [128, 512], BF16) for i in range(2)]
            bc = P.sbuf("bcC", [64, 4, 128], F32)
            eb = P.sbuf("ebC", [64, 4, 128], F32)
            enb = P.sbuf("enbC", [64, 4, 128], F32)
            qt = P.sbuf("qtC", [64, 4, 128], BF16)
            kt = P.sbuf("ktC", [64, 4, 128], BF16)
            am = P.sbuf("amC", [128, 4, 128], BF16)
            ktk = P.sbuf("ktkC", [128, 4, 64], BF16)
            osq = P.sbuf("osqC", [128, 4, 128], F32)
            sml = P.sbuf("smlC", [128, 8], F32)
            Gr = P.sbuf("GrC", [128, 512], F32)
            ygs = [P.sbuf("yg%d" % i, [128, 512], BF16) for i in range(2)]
            at_ps = P.psum("at_ps", [128, 4, 128], F32)
            ktk_ps = P.psum("ktk_ps", [128, 4, 64], BF16)
            o_ps = P.psum("o_psC", [128, 4, 128], F32)
            su_ps = P.psum("su_ps", [64, 4, 128], F32)
            P.dma('sp', ggB[:], glnB[l], writes=['ggB'])
            P.op('pool', lambda e: e.memset(Sst[:], 0.0), writes=['Sst'])

            def loads(t):
                j = t % 2
                cols = slice(t * 128, (t + 1) * 128)
                P.dma('sp', gqs[j][:], gqT[:, :, cols].rearrange("h p s -> p h s"), writes=[('gq', j)])
                P.dma('sp', gks[j][:], gkT[:, :, cols].rearrange("h p s -> p h s"), writes=[('gk', j)])
                P.dma('sp', las[j][:], laT[:, :, cols].rearrange("h p s -> p h s"), writes=[('la', j)])
                P.dma('sp', vgs[j][:], vg[cols, :], writes=[('vgt', j)])
                P.dma('sp', rgs[j][:], rg[cols, :], writes=[('rgt', j)])

            loads(0)
            for t in range(NT):
                j = t % 2
                if t + 1 < NT:
                    loads(t + 1)
                gq, gk, la, vgt, rgt, yg = gqs[j], gks[j], las[j], vgs[j], rgs[j], ygs[j]
                for h in range(4):
                    P.op('dve', lambda e: e.tensor_tensor_scan(bc[:, h, :], ones_f[0:64, 0:128], la[:, h, :], 0.0, ALU.mult, ALU.add),
                         reads=['cf', ('la', j)], writes=['bcC'])
                act(eb[:], bc[:], AF.Exp, ['bcC'], ['ebC'])
                act(enb[:], bc[:], AF.Exp, ['bcC'], ['enbC'], scale=-1.0)
                P.op('pool', lambda e: e.tensor_tensor(qt[:], gq[:], eb[:], ALU.mult), reads=[('gq', j), 'ebC'], writes=['qtC'])
                P.op('dve', lambda e: e.tensor_tensor(kt[:], gk[:], enb[:], ALU.mult), reads=[('gk', j), 'enbC'], writes=['ktC'])
                for h in range(4):
                    mm(at_ps[:, h, :], kt[:, h, :], qt[:, h, :], True, True, ['ktC', 'qtC'], ['at_ps'])
                P.op('dve', lambda e: e.tensor_tensor(am[:], at_ps[:], mask4_b.rearrange("p (h s) -> p h s", h=4), ALU.mult),
                     reads=['at_ps', 'cb'], writes=['amC'])
                for h in range(4):
                    P.op('pe', lambda e: e.transpose(ktk_ps[:, h, :], kt[:, h, :], ident_b[0:64, 0:64]), reads=['ktC', 'cb'], writes=['ktk_ps'])
                P.op('act', lambda e: e.copy(ktk[:], ktk_ps[:]), reads=['ktk_ps'], writes=['ktkC'])
                P.op('pool', lambda e: e.tensor_copy(Sbf[:], Sst[:]), reads=['Sst'], writes=['Sbf'])
                for h in range(4):
                    mm(o_ps[:, h, :], am[:, h, :], vgt[:, h * 128:(h + 1) * 128], True, False, ['amC', ('vgt', j)], ['o_psC'])
                    mm(o_ps[:, h, :], qt[:, h, :], Sbf[:, h, :], False, True, ['qtC', 'Sbf'], ['o_psC'])
                for h in range(4):
                    mm(su_ps[:, h, :], ktk[:, h, :], vgt[:, h * 128:(h + 1) * 128], True, True, ['ktkC', ('vgt', j)], ['su_ps'])
                P.op('dve', lambda e: e.tensor_tensor(tmpS[:], Sst[:], su_ps[:], ALU.add), reads=['Sst', 'su_ps', 'Sbf'], writes=['tmpS'])
                for h in range(4):
                    P.op('dve', lambda e: e.tensor_scalar(Sst[:, h, :], tmpS[:, h, :], eb[:, h, 127:128], None, ALU.mult),
                         reads=['tmpS', 'ebC', 'Sbf'], writes=['Sst'])
                act(osq[:], o_ps[:], AF.Square, ['o_psC'], ['osqC'])
                P.op('dve', lambda e: e.tensor_reduce(sml[:, 0:4], osq[:], AX.X, ALU.add), reads=['osqC'], writes=['ssqC'])
                act(sml[:, 4:8], sml[:, 0:4], AF.Ln, ['ssqC', 'cf'], ['rstdC'], scale=1.0 / 128, bias=epsc)
                act(sml[:, 4:8], sml[:, 4:8], AF.Exp, ['rstdC'], ['rstdC'], scale=-0.5)
                P.op('pool', lambda e: e.tensor_tensor(Gr[:], rgt[:], ggB[:], ALU.mult), reads=[('rgt', j), 'ggB'], writes=['GrC'])
                for h in range(4):
                    P.op('dve', lambda e: e.scalar_tensor_tensor(yg[:, h * 128:(h + 1) * 128], o_ps[:, h, :], sml[:, 4 + h:5 + h], Gr[:, h * 128:(h + 1) * 128], ALU.mult, ALU.mult),
                         reads=['o_psC', 'rstdC', 'GrC'], writes=[('yg', j)])
                P.dma('sp', ycat[t * 128:(t + 1) * 128, 1024:1536], yg[:], reads=[('yg', j)], writes=[('ycat', 1, t)])
            P.barrier()
        P.es = es

    def phaseD(l):
        with ExitStack() as ph:
            P.es = ph
            ikt = P.sbuf("ikt", [128, S], BF16)
            scores = [P.sbuf("score%d" % i, [128, S], F32) for i in range(2)]
            junkd = P.sbuf("junkD", [128, S], BF16)
            junka = P.sbuf("junkA2", [128, S], BF16)
            mbs = [P.sbuf("mb%d" % i, [128, S], BF16) for i in range(2)]
            iqts = [P.sbuf("iqt%d" % i, [128, 4, 128], BF16) for i in range(2)]
            iwts = [P.sbuf("iwt%d" % i, [128, 8], F32) for i in range(2)]
            dWs = [P.sbuf("dW%d" % i, [128, 8, 128], BF16) for i in range(2)]
            rbufs = [P.sbuf("rbuf%d" % i, [128, 512], BF16) for i in range(4)]
            sms = [P.sbuf("smD%d" % i, [128, 16], F32) for i in range(2)]
            wtabs = [P.sbuf("wtab%d" % i, [128, 32], F32) for i in range(2)]
            lgs = [P.psum("lg%d" % i, [128, 512], F32) for i in range(4)]
            scs = [P.psum("sc%d" % i, [128, 512], F32) for i in range(2)]
            for c in range(0, S, 1024):
                P.dma('sp', ikt[:, c:c + 1024], ikT2[:, c:c + 1024], writes=['ikt'])
            iqv = iqT.rearrange("(g p) s -> p g s", p=128)

            def loads(t):
                j = t % 2
                cols = slice(t * 128, (t + 1) * 128)
                P.dma('sp', iqts[j][:], iqv[:, :, cols], writes=[('iqt', j)])
                P.dma('sp', iwts[j][:], iwS[cols, :], writes=[('iwt', j)])

            ci = {'lg': 0, 'sc': 0, 'rb': 0}

            def score_tiles(tiles):
                steps = []
                for t in tiles:
                    j = t % 2
                    iwt, dW = iwts[j], dWs[j]
                    for h in range(8):
                        P.op('pool', lambda e: e.tensor_scalar(dW[:, h, :], ident_b, iwt[:, h:h + 1], None, ALU.mult),
                             reads=['cb', ('iwt', j)], writes=[('dW', j)])
                    nk = 128 * (t + 1)
                    for kb in range((nk + 511) // 512):
                        for h in range(8):
                            steps.append((t, kb, h))
                pend = []
                state = {}

                def front(i):
                    t, kb, h = steps[i]
                    j = t % 2
                    lg, lgk = lgs[ci['lg'] % 4], ('lg', ci['lg'] % 4)
                    ci['lg'] += 1
                    rb, rbk = rbufs[ci['rb'] % 4], ('rb', ci['rb'] % 4)
                    ci['rb'] += 1
                    po = (h % 2) * 64
                    mm(lg[:, :], iqts[j][po:po + 64, h // 2, :], ikt[po:po + 64, kb * 512:(kb + 1) * 512], True, True,
                       [('iqt', j), 'ikt'], [lgk])
                    if h % 2:
                        act(rb[:], lg[:], AF.Relu, [lgk], [rbk])
                    else:
                        P.op('dve', lambda e: e.tensor_scalar(rb[:], lg[:], 0.0, None, ALU.max), reads=[lgk], writes=[rbk])
                    return rb, rbk

                def back(i, rb, rbk):
                    t, kb, h = steps[i]
                    j = t % 2
                    if h == 0:
                        state['sc'] = (scs[ci['sc'] % 2], ('sc', ci['sc'] % 2))
                        ci['sc'] += 1
                    sc, sck = state['sc']
                    mm(sc[:, :], dWs[j][:, h, :], rb[:], h == 0, h == 7, [('dW', j), rbk], [sck])
                    if h == 7:
                        P.op('act', lambda e: e.copy(scores[j][:, kb * 512:(kb + 1) * 512], sc[:, :]), reads=[sck], writes=[('score', j)])

                n = len(steps)
                for i in range(n + 2):
                    if i < n:
                        pend.append(front(i))
                    if i >= 2:
                        back(i - 2, *pend[i - 2])

            def prelude(t):
                j = t % 2
                score, sm, wtab = scores[j], sms[j], wtabs[j]
                sk_, smk, wk = ('score', j), ('smD', j), ('wtab', j)
                nk = 128 * (t + 1)
                P.op('dve', lambda e: e.tensor_reduce(sm[:, 0:1], score[:, 0:nk], AX.X, ALU.max), reads=[sk_], writes=[smk])
                P.op('dve', lambda e: e.tensor_reduce(sm[:, 1:2], score[:, 0:nk], AX.X, ALU.min), reads=[sk_], writes=[smk])
                P.op('pool', lambda e: e.memset(score[0:64, nk - 64:nk], -1.0e30), reads=[smk], writes=[sk_])
                P.op('dve', lambda e: e.tensor_tensor(sm[:, 2:3], sm[:, 0:1], sm[:, 1:2], ALU.subtract), reads=[smk], writes=[smk])
                P.op('dve', lambda e: e.tensor_scalar(sm[:, 3:4], sm[:, 2:3], 1.02, 2.0e-6, ALU.mult, ALU.add), reads=[smk], writes=[smk])
                P.op('dve', lambda e: e.tensor_scalar(wtab[:], cf[:, CF_PW:CF_PW + 32], sm[:, 3:4], None, ALU.mult), reads=[smk, 'cf'], writes=[wk])
                P.op('dve', lambda e: e.scalar_tensor_tensor(sm[:, 4:5], sm[:, 2:3], -0.01, sm[:, 1:2], ALU.mult, ALU.add), reads=[smk], writes=[smk])
                P.op('dve', lambda e: e.tensor_scalar(sm[:, 4:5], sm[:, 4:5], -1.0e-6, None, ALU.add), reads=[smk], writes=[smk])
                P.op('dve', lambda e: e.tensor_tensor(sm[:, 4:5], sm[:, 4:5], wtab[:, 0:1], ALU.add), reads=[smk, wk], writes=[('mid', j)])

            def split(nk):
                nD = (int(nk * 0.44) // 64) * 64
                nD = max(64, min(nk - 64, nD))
                return nD, nk - nD

            def count(t):
                j = t % 2
                score, sm = scores[j], sms[j]
                nk = 128 * (t + 1)
                nD, nA = split(nk)
                P.op('dve', lambda e: e.tensor_scalar(junkd[:, 0:nD], score[:, 0:nD], sm[:, 4:5], None, ALU.is_ge, ALU.add, accum_out=sm[:, 5:6]),
                     reads=[('score', j), ('mid', j)], writes=['junkD', ('cntD', j)])
                act(junka[:, 0:nA], score[:, nD:nk], AF.Sign, [('score', j), ('mid', j)], ['junkA2', ('sA', j)],
                    scale=-1.0, bias=sm[:, 4:5], accum_out=sm[:, 8:9])

            def update(t, it):
                j = t % 2
                sm, wtab = sms[j], wtabs[j]
                nk = 128 * (t + 1)
                nD, nA = split(nk)
                P.op('dve', lambda e: e.scalar_tensor_tensor(sm[:, 9:10], sm[:, 8:9], -0.5, sm[:, 5:6], ALU.mult, ALU.add),
                     reads=[('sA', j), ('cntD', j)], writes=[('t1', j)])
                P.op('dve', lambda e: e.tensor_scalar(sm[:, 6:7], sm[:, 9:10], TOPK - 0.5 - nA / 2.0, 0.5, ALU.is_ge, ALU.subtract),
                     reads=[('t1', j)], writes=[('sg', j)])
                P.op('dve', lambda e: e.scalar_tensor_tensor(sm[:, 4:5], sm[:, 6:7], wtab[:, it:it + 1], sm[:, 4:5], ALU.mult, ALU.add),
                     reads=[('sg', j), ('wtab', j), ('mid', j)], writes=[('mid', j)])

            def finish_tile(t):
                j = t % 2
                score, sm, wtab, mb = scores[j], sms[j], wtabs[j], mbs[j]
                nk = 128 * (t + 1)
                P.op('dve', lambda e: e.tensor_tensor(sm[:, 7:8], sm[:, 4:5], wtab[:, NBIS - 1:NBIS], ALU.subtract), reads=[('mid', j), ('wtab', j)], writes=[('thr', j)])
                h0 = (nk // 2 // 64) * 64
                P.op('dve', lambda e: e.tensor_scalar(mb[:, 0:h0], score[:, 0:h0], sm[:, 7:8], NEG, ALU.is_lt, ALU.mult), reads=[('score', j), ('thr', j)], writes=[('mb', j)])
                P.op('pool', lambda e: e.tensor_scalar(mb[:, h0:nk], score[:, h0:nk], sm[:, 7:8], NEG, ALU.is_lt, ALU.mult), reads=[('score', j), ('thr', j)], writes=[('mb', j)])
                P.dma('sp', MB[t, :, 0:nk], mb[:, 0:nk], reads=[('mb', j)], writes=[('MB', t)])

            loads(0)
            loads(1)
            for p in range(NT // 2):
                tt = (2 * p, 2 * p + 1)
                score_tiles(tt)
                for t in tt:
                    if t + 2 < NT:
                        loads(t + 2)
                for t in tt:
                    prelude(t)
                for it in range(NBIS):
                    for t in tt:
                        count(t)
                    for t in tt:
                        update(t, it)
                for t in tt:
                    finish_tile(t)
            P.barrier()
        with ExitStack() as ph:
            P.es = ph
            KT = P.sbuf("KT", [128, 4, S], BF16)
            V1 = P.sbuf("V1", [128, NT, 4, 129], BF16)
            ohs = P.sbuf("ohs", [128, len(_OHIDX), 128], BF16)
            relb = P.sbuf("relb", [128, 128], F32)
            biasT = P.sbuf("biasT", [128, 2, 4, 128], F32)
            ec = P.sbuf("ecD", [128, 4], F32)
            qhs = [P.sbuf("qh%d" % i, [128, 4, 128], BF16) for i in range(2)]
            mbts = [P.sbuf("mbt%d" % i, [128, S], BF16) for i in range(2)]
            zdts = [P.sbuf("zdt%d" % i, [128, 512], BF16) for i in range(2)]
            pTs = [P.sbuf("pT%d" % i, [128, 4, 128], BF16) for i in range(3)]
            tmps = [P.sbuf("tmpD%d" % i, [128, 4, 128], F32) for i in range(2)]
            on_sb = P.sbuf("on_sb", [128, 4, 129], F32)
            O = P.sbuf("O_D", [128, 4, 129], F32)
            rden = P.sbuf("rdenD", [128, 4], F32)
            yds = [P.sbuf("yd%d" % i, [128, 512], BF16) for i in range(2)]
            s_pss = [P.psum("s_ps%d" % i, [128, 4, 128], F32) for i in range(2)]
            ofs = [P.psum("of%d" % i, [128, 2, 129], F32) for i in range(2)]
            ons = [P.psum("on%d" % i, [128, 2, 129], F32) for i in range(2)]
            for h in range(4):
                P.dma('sp', KT[:, h, :], KhT[h], writes=['KT'])
            P.op('pool', lambda e: e.memset(V1[:, :, :, 128:129], 1.0), writes=['V1'])
            for u0 in range(NT):
                P.dma('sp', V1[:, u0, :, 0:128], vd[u0 * 128:(u0 + 1) * 128, :].rearrange("p (h e) -> p h e", h=4), writes=['V1'])
            P.dma('sp', ohs[:], oh_in, writes=['ohs'])
            P.dma('sp', relb[:], relB, writes=['relb'])
            P.op('pool', lambda e: e.memset(biasT[:], 0.0), writes=['biasT'])
            for i, (r, b) in enumerate(_OHIDX):
                for h in range(4):
                    P.op('dve', lambda e: e.scalar_tensor_tensor(biasT[:, r, h, :], ohs[:, i, :], relb[:, b * 4 + h:b * 4 + h + 1], biasT[:, r, h, :], ALU.mult, ALU.add),
                         reads=['ohs', 'relb', 'biasT'], writes=['biasT'])
            act(ec[:], relb[:, 60:64], AF.Exp, ['relb'], ['ecD'])

            def loads(t):
                j = t % 2
                cols = slice(t * 128, (t + 1) * 128)
                nk = 128 * (t + 1)
                P.dma('sp', qhs[j][:], QhT[:, :, cols].rearrange("h p s -> p h s"), writes=[('qh', j)])
                P.dma('sp', mbts[j][:, 0:nk], MB[t, :, 0:nk], writes=[('mbt', j)])
                P.dma('sp', zdts[j][:], zd[cols, :], writes=[('zdt', j)])

            loads(0)
            ci = {'s': 0, 'p': 0, 'tmp': 0}
            for t in range(NT):
                j = t % 2
                if t + 1 < NT:
                    loads(t + 1)
                qh, mbt, zdt, yd = qhs[j], mbts[j], zdts[j], yds[j]
                far_last = t - 2
                near_first = max(0, t - 1)
                def qk_exp(u):
                    near = u >= t - 1
                    s_ps, sk = s_pss[ci['s'] % 2], ('s_ps', ci['s'] % 2)
                    ci['s'] += 1
                    pT, pk = pTs[ci['p'] % 3], ('pT', ci['p'] % 3)
                    ci['p'] += 1
                    for h in range(4):
                        mm(s_ps[:, h, :], KT[:, h, u * 128:(u + 1) * 128], qh[:, h, :], True, False, ['KT', ('qh', j)], [sk])
                        mm(s_ps[:, h, :], mbt[:, u * 128:(u + 1) * 128], ident_b, False, True, [('mbt', j), 'cb'], [sk])
                    if near:
                        tmp, tk = tmps[ci['tmp'] % 2], ('tmpD', ci['tmp'] % 2)
                        ci['tmp'] += 1
                        P.op('dve', lambda e: e.tensor_tensor(tmp[:], s_ps[:], biasT[:, t - u, :, :], ALU.add), reads=[sk, 'biasT'], writes=[tk])
                        act(pT[:], tmp[:], AF.Exp, [tk], [pk])
                    else:
                        act(pT[:], s_ps[:], AF.Exp, [sk], [pk])
                    return pT, pk

                def pv(u, pT, pk):
                    near = u >= t - 1
                    for h in range(4):
                        if near:
                            o_, ok_ = ons[h // 2], ('on', h // 2)
                            first, last = (u == near_first), (u == t)
                        else:
                            o_, ok_ = ofs[h // 2], ('of', h // 2)
                            first, last = (u == 0), (u == far_last)
                        mm(o_[:, h % 2, :], pT[:, h, :], V1[:, u, h, :], first and (h % 2 == 0), last, [pk, 'V1'], [ok_])

                prev = None
                for u in range(t + 1):
                    cur = qk_exp(u)
                    if prev is not None:
                        pv(u - 1, *prev)
                    prev = cur
                pv(t, *prev)
                for hh in range(2):
                    P.op('act', lambda e: e.copy(on_sb[:, 2 * hh:2 * hh + 2, :], ons[hh][:]), reads=[('on', hh)], writes=[('on_sb', hh)])
                for h in range(4):
                    if t >= 2:
                        P.op('dve', lambda e: e.scalar_tensor_tensor(O[:, h, :], ofs[h // 2][:, h % 2, :], ec[:, h:h + 1], on_sb[:, h, :], ALU.mult, ALU.add),
                             reads=[('of', h // 2), 'ecD', ('on_sb', h // 2)], writes=[('O', h)])
                        Oh = O
                        Ok = ('O', h)
                    else:
                        Oh = on_sb
                        Ok = ('on_sb', h // 2)
                    P.op('dve', lambda e: e.reciprocal(rden[:, h:h + 1], Oh[:, h, 128:129]), reads=[Ok], writes=[('rden', h)])
                    P.op('dve', lambda e: e.scalar_tensor_tensor(yd[:, h * 128:(h + 1) * 128], Oh[:, h, 0:128], rden[:, h:h + 1], zdt[:, h * 128:(h + 1) * 128], ALU.mult, ALU.mult),
                         reads=[Ok, ('rden', h), ('zdt', j)], writes=[('yd', j)])
                P.dma('sp', ycat[t * 128:(t + 1) * 128, 1536:2048], yd[:], reads=[('yd', j)], writes=[('ycat', 2, t)])
            P.barrier()
        P.es = es

    def phaseE(l, src, dst):
        with ExitStack() as ph:
            P.es = ph
            wo = P.sbuf("wo", [128, 16, 1024], BF16)
            wf2 = P.sbuf("wf2", [128, 4, 1024], F32)
            yts = [P.sbuf("ytE%d" % i, [128, 2048], BF16) for i in range(2)]
            yTs = [P.sbuf("yTE%d" % i, [128, 16, 128], BF16) for i in range(2)]
            xts = [P.sbuf("xtE%d" % i, [128, 1024], F32) for i in range(2)]
            ots = [P.sbuf("otE%d" % i, [128, 1024], F32) for i in range(2)]
            tpe = [P.psum("tpe%d" % i, [128, 8, 128], BF16) for i in range(2)]
            oe = [P.psum("oe%d" % i, [128, 512], F32) for i in range(2)]
            wv = w_out[l].rearrange("(c p) n -> p c n", p=128)
            for q in range(4):
                P.dma('sp', wf2[:], wv[:, 4 * q:4 * q + 4, :], writes=['wf2'])
                P.op('pool', lambda e: e.tensor_copy(wo[:, 4 * q:4 * q + 4, :], wf2[:]), reads=['wf2'], writes=['wo'])

            def loads(t):
                j = t % 2
                rows = slice(t * 128, (t + 1) * 128)
                P.dma('sp', yts[j][:], ycat[rows, :], writes=[('ytE', j)])
                P.dma('sp', xts[j][:], src[rows, :], writes=[('xtE', j)])

            loads(0)
            for t in range(NT):
                j = t % 2
                if t + 1 < NT:
                    loads(t + 1)
                yt, yT, xt, ot = yts[j], yTs[j], xts[j], ots[j]
                for c in range(16):
                    P.op('pe', lambda e: e.transpose(tpe[c // 8][:, c % 8, :], yt[:, c * 128:(c + 1) * 128], ident_b),
                         reads=[('ytE', j), 'cb'], writes=[('tpe', c // 8)])
                P.op('act', lambda e: e.copy(yT[:, 0:8, :], tpe[0][:]), reads=[('tpe', 0)], writes=[('yTE', j)])
                P.op('dve', lambda e: e.tensor_copy(yT[:, 8:16, :], tpe[1][:]), reads=[('tpe', 1)], writes=[('yTE', j)])
                for half in range(2):
                    for c in range(16):
                        mm(oe[half][:, :], yT[:, c, :], wo[:, c, half * 512:(half + 1) * 512], c == 0, c == 15, [('yTE', j), 'wo'], [('oe', half)])
                    P.op('dve', lambda e: e.tensor_tensor(ot[:, half * 512:(half + 1) * 512], oe[half][:, :], xt[:, half * 512:(half + 1) * 512], ALU.add),
                         reads=[('oe', half), ('xtE', j)], writes=[('otE', j)])
                P.dma('sp', dst[t * 128:(t + 1) * 128, :], ot[:], reads=[('otE', j)], writes=[('dst', t)])
            P.barrier()
        P.es = es

    k = K()
    k.__dict__.update(locals())
    return k


def prep_shared(inputs):
    f = lambda a: np.ascontiguousarray(np.asarray(a, dtype=np.float32))
    cf, cb, oh, _ = host_consts()
    d = {}
    d["w_in"] = f(inputs["w_in"])
    d["w_out"] = f(inputs["w_out"])
    d["normB"] = f(np.broadcast_to(np.asarray(inputs["norm_g"])[:, None, :], (2, 128, 1024)))
    d["convwT"] = f(np.concatenate([np.transpose(np.asarray(inputs["ml_conv_w"]), (0, 2, 1)),
                                    np.asarray(inputs["ml_conv_b"])[:, :, None]], axis=2))
    d["ml_b_i"] = f(np.asarray(inputs["ml_b_i"])[:, :, None])
    d["ml_b_f"] = f(np.asarray(inputs["ml_b_f"])[:, :, None])
    d["mlnB"] = f(np.broadcast_to(np.asarray(inputs["ml_norm_g"])[:, None, :], (2, 128, 1024)))
    d["gla_w_a"] = f(inputs["gla_w_a"])
    d["gla_b_aT"] = f(np.transpose(np.asarray(inputs["gla_b_a"]).reshape(2, 4, 64), (0, 2, 1)))
    d["glnB"] = f(np.broadcast_to(np.asarray(inputs["gla_norm_g"])[:, None, :], (2, 128, 512)))
    d["dsa_q_g"] = f(np.asarray(inputs["dsa_q_g"])[:, :, None])
    d["dsa_k_g"] = f(np.asarray(inputs["dsa_k_g"])[:, :, None])
    d["relB"] = f(np.broadcast_to(np.asarray(inputs["rel_bias"]).reshape(1, 128), (128, 128)))
    d["cf"] = cf
    d["cb"] = cb
    d["oh"] = oh
    return d


def emit(k, phases="ABCDE", layers=(0,)):
    for l in layers:
        src = k.x_in if l == 0 else k.h1
        if "A" in phases:
            k.phaseA(l, src)
        if "B" in phases:
            k.phaseB(l)
        if "C" in phases:
            k.phaseC(l)
        if "D" in phases:
            k.phaseD(l)
        if "E" in phases:
            k.phaseE(l, src, k.h1 if l == 0 else k.out)


_CACHE = {}


def kernel(**inputs):
    x = np.asarray(inputs["x"], dtype=np.float32)
    B, S, D = x.shape
    key = (S,)
    if key not in _CACHE:
        k = build(S, depth=2, debug=False)
        emit(k, "ABCDE", layers=(0, 1))
        k.P.finish()
        _CACHE[key] = k
    k = _CACHE[key]
    shared = prep_shared(inputs)
    in_maps = []
    for b in range(B):
        d = dict(shared)
        d["x"] = np.ascontiguousarray(x[b])
        in_maps.append(d)
    res = run_bass_kernel_spmd(k.nc, in_maps, core_ids=list(range(B)))
    return np.stack([np.asarray(r["out"], dtype=np.float32) for r in res.results], axis=0)
```

```python
import numpy as np
import ml_dtypes
from contextlib import ExitStack
import concourse.bass as bass
import concourse.mybir as mybir
from concourse.bass_utils import run_bass_kernel_spmd

F32 = mybir.dt.float32
BF16 = mybir.dt.bfloat16
AF = mybir.ActivationFunctionType
ALU = mybir.AluOpType
AX = mybir.AxisListType


class Prog:
    ENG = ['pe', 'act', 'dve', 'pool', 'sp']

    def __init__(self, nc, es, n_dma_sems=40):
        self.nc = nc
        self.es = es
        self.es0 = es
        self.sem = {e: es.enter_context(nc.semaphore('s_' + e)) for e in self.ENG}
        self.dsem = [es.enter_context(nc.semaphore('d%d' % i)) for i in range(n_dma_sems)]
        self.cnt = {e: 0 for e in self.ENG}
        self.dcnt = 0
        self.dval = [0] * n_dma_sems
        self.waited = {e: {} for e in self.ENG}
        self.ops = {e: [] for e in self.ENG}
        self.lastw = {}
        self.readers = {}
        self.nops = 0
        self._e_pe = nc.tensor
        self._e_act = nc.scalar
        self._e_dve = nc.vector
        self._e_pool = nc.gpsimd
        self._e_sp = nc.sync

    def sbuf(self, name, shape, dtype):
        self.uid = getattr(self, 'uid', 0) + 1
        return self.es.enter_context(self.nc.sbuf_tensor("sb%d_%s" % (self.uid, name), shape, dtype))

    def psum(self, name, shape, dtype):
        self.uid = getattr(self, 'uid', 0) + 1
        return self.es.enter_context(self.nc.psum_tensor("ps%d_%s" % (self.uid, name), shape, dtype))

    def _wait(self, eng, tok):
        if tok is None:
            return
        kind, who, val = tok
        if kind == 'e' and who == 'pe' and eng == 'pe':
            return
        key = (kind, who)
        if self.waited[eng].get(key, 0) >= val:
            return
        self.waited[eng][key] = val
        sem = self.sem[who] if kind == 'e' else self.dsem[who]
        getattr(self, '_e_' + eng).wait_ge(sem, val)

    def _deps(self, eng, reads, writes):
        for k in reads:
            self._wait(eng, self.lastw.get(k))
        for k in writes:
            self._wait(eng, self.lastw.get(k))
            for t in self.readers.get(k, ()):
                self._wait(eng, t)

    def _commit(self, tok, reads, writes):
        for k in writes:
            self.lastw[k] = tok
            self.readers[k] = []
        for k in reads:
            if k in writes:
                continue
            self.readers.setdefault(k, []).append(tok)

    def op(self, eng, fn, reads=(), writes=()):
        self._deps(eng, reads, writes)
        self.cnt[eng] += 1
        tok = ('e', eng, self.cnt[eng])
        fn(getattr(self, '_e_' + eng)).then_inc(self.sem[eng], 1)
        self._commit(tok, reads, writes)
        self.nops += 1

    def dma(self, q, out, in_, reads=(), writes=(), **kw):
        n = len(self.dsem)
        idx = self.dcnt % n
        self.dcnt += 1
        if self.dval[idx] > 0:
            self._wait(q, ('d', idx, self.dval[idx]))
        self._deps(q, reads, writes)
        self.dval[idx] += 16
        tok = ('d', idx, self.dval[idx])
        getattr(self, '_e_' + q).dma_start(out=out, in_=in_, **kw).then_inc(self.dsem[idx], 16)
        self._commit(tok, reads, writes)
        self.nops += 1

    def finish(self):
        for idx, v in enumerate(self.dval):
            if v > 0:
                self._wait('sp', ('d', idx, v))
        for e in ['pe', 'act', 'dve', 'pool']:
            if self.cnt[e] > 0:
                self._wait('sp', ('e', e, self.cnt[e]))

    def barrier(self):
        for e in self.ENG:
            for o in ['pe', 'act', 'dve', 'pool']:
                if self.cnt[o] > 0 and not (e == o):
                    self._wait(e, ('e', o, self.cnt[o]))
            for idx, v in enumerate(self.dval):
                if v > 0:
                    self._wait(e, ('d', idx, v))
        if not hasattr(self, 'bar'):
            self.bar = self.es0.enter_context(self.nc.semaphore('s_bar'))
            self.go = self.es0.enter_context(self.nc.semaphore('s_go'))
            self.epoch = 0
        self.epoch += 1
        comp = ['pe', 'act', 'dve', 'pool']
        for e in comp:
            getattr(self, '_e_' + e).sem_inc(self.bar, 1)
        sp = self._e_sp
        sp.wait_ge(self.bar, 4 * self.epoch)
        for e in comp:
            sp.sem_clear(self.sem[e])
        for d in self.dsem:
            sp.sem_clear(d)
        sp.sem_inc(self.go, 1)
        for e in comp:
            getattr(self, '_e_' + e).wait_ge(self.go, self.epoch)
        for e in comp:
            self.cnt[e] = 0
        self.dval = [0] * len(self.dsem)
        self.waited = {e: {} for e in self.ENG}
        self.lastw = {}
        self.readers = {}


C_MLQ, C_MLK, C_MLV, C_MLO, C_MLZ = 0, 1024, 2048, 3072, 4096
C_MLI, C_MLF = 5120, 5124
C_GQ, C_GK, C_GV, C_GA, C_GR = 5128, 5384, 5640, 6152, 6168
C_DQ, C_DK, C_DV, C_DZ = 6680, 7192, 7704, 8216
C_IQ, C_IK, C_IW = 8728, 9240, 9304
N_IN = 9312
EPS = 1e-6
NBIS = 24
NEG = -30000.0

CF_ID, CF_ONES, CF_MASK, CF_PW, CF_MISC = 0, 128, 256, 384, 416
NCF = 432
CB_ID, CB_MASK4 = 0, 128
NCB = 640


def t5_bucket_np(rel):
    half, max_exact = 16, 8
    ret = np.where(rel > 0, half, 0)
    n = np.abs(rel)
    nf = np.maximum(n, 1).astype(np.float32)
    large = max_exact + (np.log(nf / max_exact) / np.float32(np.log(128 / max_exact)) * (half - max_exact)).astype(np.int32)
    large = np.minimum(large, half - 1)
    return ret + np.where(n < max_exact, n, large)


def host_consts():
    cf = np.zeros((128, NCF), np.float32)
    cf[:, CF_ID:CF_ID + 128] = np.eye(128, dtype=np.float32)
    cf[:, CF_ONES:CF_ONES + 128] = 1.0
    s = np.arange(128)
    maskT = (s[:, None] <= s[None, :]).astype(np.float32)
    cf[:, CF_MASK:CF_MASK + 128] = maskT
    cf[:, CF_PW:CF_PW + 32] = (0.5 ** (np.arange(32) + 1))[None, :]
    cf[:, CF_MISC + 0] = EPS
    cf[:, CF_MISC + 1] = 1.0
    cf[:, CF_MISC + 2] = 0.0
    cb = np.zeros((128, NCB), np.float32)
    cb[:, CB_ID:CB_ID + 128] = np.eye(128)
    cb[:, CB_MASK4:CB_MASK4 + 512] = np.tile(maskT, (1, 4))
    k = np.arange(128)[:, None]
    q = np.arange(128)[None, :]
    oh = []
    ohidx = []
    for r in (0, 1):
        b = t5_bucket_np((k - q - 128 * r).astype(np.int32))
        for bb in np.unique(b):
            oh.append((b == bb).astype(np.float32))
            ohidx.append((r, int(bb)))
    oh = np.stack(oh, 1)
    return cf, cb.astype(ml_dtypes.bfloat16), oh.astype(ml_dtypes.bfloat16), ohidx


_OHIDX = host_consts()[3]


class K:
    pass


def build(S, depth=2, debug=False, stop_after=None):
    NT = S // 128
    NB = S // 512
    TOPK = min(256, S // 4)
    nc = bass.Bass("TRN2", target_bir_lowering=False)

    def din(name, shape, dt=F32):
        return nc.dram_tensor(name, list(shape), dt, kind="ExternalInput").ap()

    def dscr(name, shape, dt=F32):
        return nc.dram_tensor(name, list(shape), dt, kind=("ExternalOutput" if debug else "Internal")).ap()

    x_in = din("x", [S, 1024])
    w_in = din("w_in", [2, 1024, N_IN])
    w_out = din("w_out", [2, 2048, 1024])
    normB = din("normB", [2, 128, 1024])
    convwT = din("convwT", [2, 2048, 5])
    ml_b_i = din("ml_b_i", [2, 4, 1])
    ml_b_f = din("ml_b_f", [2, 4, 1])
    mlnB = din("mlnB", [2, 128, 1024])
    gla_w_a = din("gla_w_a", [2, 16, 256])
    gla_b_aT = din("gla_b_aT", [2, 64, 4])
    glnB = din("glnB", [2, 128, 512])
    dsa_q_g = din("dsa_q_g", [2, 128, 1])
    dsa_k_g = din("dsa_k_g", [2, 128, 1])
    relB = din("relB", [128, 128])
    cf_in = din("cf", [128, NCF])
    cb_in = din("cb", [128, NCB], BF16)
    oh_in = din("oh", [128, len(_OHIDX), 128], BF16)
    out = nc.dram_tensor("out", [S, 1024], F32, kind="ExternalOutput").ap()

    h1 = dscr("h1", [S, 1024])
    qkT = dscr("qkT", [2048, S], BF16)
    vml = dscr("vml", [S, 1024], BF16)
    sigo = dscr("sigo", [S, 1024], BF16)
    zml = dscr("zml", [S, 1024], BF16)
    liT = dscr("liT", [4, S])
    lfT = dscr("lfT", [4, S])
    gqT = dscr("gqT", [4, 64, S])
    gkT = dscr("gkT", [4, 64, S])
    laT = dscr("laT", [4, 64, S])
    vg = dscr("vg", [S, 512], BF16)
    rg = dscr("rg", [S, 512], BF16)
    QhT = dscr("QhT", [4, 128, S], BF16)
    KhT = dscr("KhT", [4, 128, S], BF16)
    vd = dscr("vd", [S, 512], BF16)
    zd = dscr("zd", [S, 512], BF16)
    iqT = dscr("iqT", [512, S], BF16)
    ikT2 = dscr("ikT2", [128, S], BF16)
    iwS = dscr("iw", [S, 8])
    MB = dscr("MB", [NT, 128, S], BF16)
    ycat = dscr("ycat", [S, 2048], BF16)

    es = ExitStack()
    P = Prog(nc, es)
    cf = P.sbuf("cf", [128, NCF], F32)
    cb = P.sbuf("cb", [128, NCB], BF16)
    P.dma('sp', cf[:], cf_in, writes=['cf'])
    P.dma('sp', cb[:], cb_in, writes=['cb'])
    ident_f = cf[:, CF_ID:CF_ID + 128]
    ones_f = cf[:, CF_ONES:CF_ONES + 128]
    maskT_f = cf[:, CF_MASK:CF_MASK + 128]
    epsc = cf[:, CF_MISC:CF_MISC + 1]
    onec = cf[:, CF_MISC + 1:CF_MISC + 2]
    ident_b = cb[:, CB_ID:CB_ID + 128]
    mask4_b = cb[:, CB_MASK4:CB_MASK4 + 512]

    def act(outp, inp, func, r, w, **kw):
        P.op('act', lambda e: e.activation(outp, inp, func, **kw), reads=r, writes=w)

    def mm(outp, lhsT, rhs, start, stop, r, w):
        P.op('pe', lambda e: e.matmul(outp, lhsT, rhs, start=start, stop=stop), reads=r, writes=w)

    def phaseA(l, src):
        with ExitStack() as ph:
            P.es = ph
            xnT = P.sbuf("xnT", [128, 8, S], BF16)
            gB = P.sbuf("gB", [128, 1024], F32)
            P.dma('sp', gB[:], normB[l], writes=['gB'])
            with ExitStack() as ph1:
                P.es = ph1
                xts = [P.sbuf("xt%d" % i, [128, 1024], F32) for i in range(2)]
                xns = [P.sbuf("xn%d" % i, [128, 1024], BF16) for i in range(2)]
                junk = P.sbuf("junkA", [128, 1024], BF16)
                ss = [P.sbuf("ssA%d" % i, [128, 1], F32) for i in range(2)]
                tps = [P.psum("tpA%d" % i, [128, 8, 128], BF16) for i in range(2)]
                for t in range(NT):
                    j = t % 2
                    xt, xn, tp, s1 = xts[j], xns[j], tps[j], ss[j]
                    P.dma('sp', xt[:], src[t * 128:(t + 1) * 128, :], writes=[('xt', j)])
                    P.op('pool', lambda e: e.memset(s1[:], 0.0), writes=[('ss', j)])
                    act(junk[:], xt[:], AF.Square, [('xt', j)], ['junkA', ('ss', j)], accum_out=s1[:, 0:1])
                    act(s1[:], s1[:], AF.Ln, [('ss', j), 'cf'], [('ss', j)], scale=1.0 / 1024, bias=epsc)
                    act(s1[:], s1[:], AF.Exp, [('ss', j)], [('ss', j)], scale=-0.5)
                    P.op('dve', lambda e: e.scalar_tensor_tensor(xn[:], xt[:], s1[:, 0:1], gB[:], ALU.mult, ALU.mult),
                         reads=[('xt', j), ('ss', j), 'gB'], writes=[('xn', j)])
                    for c in range(8):
                        P.op('pe', lambda e: e.transpose(tp[:, c, :], xn[:, c * 128:(c + 1) * 128], ident_b),
                             reads=[('xn', j), 'cb'], writes=[('tp', j)])
                    P.op('dve' if j else 'act', (lambda e: e.tensor_copy(xnT[:, :, t * 128:(t + 1) * 128], tp[:])) if j else
                         (lambda e: e.copy(xnT[:, :, t * 128:(t + 1) * 128], tp[:])),
                         reads=[('tp', j)], writes=[('xnT', t // 4)])
            P.barrier()
            P.es = ph
            wf = P.sbuf("wf", [128, 8, 512], F32)
            wbs = [P.sbuf("wb%d" % i, [128, 8, 512], BF16) for i in range(2)]
            sts = [P.sbuf("st%d" % i, [128, 515], F32) for i in range(2)]
            accs = [P.sbuf("acc%d" % i, [128, 512], F32) for i in range(2)]
            obf = [P.sbuf("obf%d" % i, [128, 512], F32) for i in range(3)]
            obb = [P.sbuf("obb%d" % i, [128, 512], BF16) for i in range(3)]
            cws = [P.sbuf("cw%d" % i, [128, 5], F32) for i in range(2)]
            smallp = P.sbuf("smallp", [128, 16], F32)
            wab_f = P.sbuf("wab_f", [16, 256], F32)
            wab = P.sbuf("wab", [16, 256], BF16)
            aTb = [P.sbuf("aTb%d" % i, [16, 512], BF16) for i in range(2)]
            pas = [P.psum("paA%d" % i, [128, 512], F32) for i in range(4)]
            pzs = [P.psum("pzA%d" % i, [128, 512], F32) for i in range(2)]
            wl = w_in[l].rearrange("(c p) n -> p c n", p=128)
            P.dma('sp', smallp[:, 0:1], dsa_q_g[l], writes=['smallp'])
            P.dma('sp', smallp[:, 1:2], dsa_k_g[l], writes=['smallp'])
            P.dma('sp', smallp[0:4, 2:3], ml_b_f[l], writes=['smallp'])
            P.dma('sp', smallp[0:4, 3:4], ml_b_i[l], writes=['smallp'])
            P.dma('sp', smallp[0:64, 4:8], gla_b_aT[l], writes=['smallp'])
            P.dma('sp', wab_f[:], gla_w_a[l], writes=['wab_f'])
            P.op('dve', lambda e: e.tensor_scalar(smallp[:, 0:1], smallp[:, 0:1], 128.0 ** -0.5, None, ALU.mult), reads=['smallp'], writes=['smallp'])
            P.op('dve', lambda e: e.tensor_scalar(smallp[0:4, 2:3], smallp[0:4, 2:3], -1.0, None, ALU.mult), reads=['smallp'], writes=['smallp'])
            P.op('dve', lambda e: e.tensor_scalar(smallp[0:64, 4:8], smallp[0:64, 4:8], -1.0, None, ALU.mult), reads=['smallp'], writes=['smallp'])
            P.op('dve', lambda e: e.tensor_copy(wab[:], wab_f[:]), reads=['wab_f'], writes=['wab'])

            fm = []
            for g in range(8):
                fm.append(('mlq', C_MLQ + g * 128, 128, g))
            for g in range(8):
                fm.append(('mlk', C_MLK + g * 128, 128, 8 + g))
            for h in range(4):
                fm.append(('glaq', C_GQ + h * 64, 64, h))
            for h in range(4):
                fm.append(('glak', C_GK + h * 64, 64, h))
            fm.append(('glaa', C_GA, 16, 0))
            for h in range(4):
                fm.append(('dsaq', C_DQ + h * 128, 128, h))
            for h in range(4):
                fm.append(('dsak', C_DK + h * 128, 128, h))
            for g in range(4):
                fm.append(('idxq', C_IQ + g * 128, 128, g))
            fm.append(('idxk', C_IK, 64, 0))
            fm.append(('mli', C_MLI, 4, 0))
            fm.append(('mlf', C_MLF, 4, 0))
            tm = [('mlv', C_MLV, 512, vml, 0), ('mlv', C_MLV + 512, 512, vml, 512),
                  ('mlo', C_MLO, 512, sigo, 0), ('mlo', C_MLO + 512, 512, sigo, 512),
                  ('mlz', C_MLZ, 512, zml, 0), ('mlz', C_MLZ + 512, 512, zml, 512),
                  ('glav', C_GV, 512, vg, 0), ('glar', C_GR, 512, rg, 0),
                  ('dsav', C_DV, 512, vd, 0), ('dsaz', C_DZ, 512, zd, 0), ('idxw', C_IW, 8, iwS, 0)]
            groups = [('fm',) + g for g in fm] + [('tm',) + g for g in tm]
            cnt = {'ev': 0, 'ob': 0, 'pa': 0, 'pz': 0, 'cw': 0, 'at': 0}

            def load_w(gi):
                g = groups[gi]
                c0, n = g[2], g[3]
                if g[1] == 'idxk':
                    P.dma('sp', wf[:, :, 0:64], wl[:, :, c0:c0 + 64], writes=['wf'])
                    P.dma('sp', wf[:, :, 64:128], wl[:, :, c0:c0 + 64], writes=['wf'])
                    n = 128
                else:
                    P.dma('sp', wf[:, :, 0:n], wl[:, :, c0:c0 + n], writes=['wf'])
                wb = wbs[gi % 2]
                P.op('pool', lambda e: e.tensor_copy(wb[:, :, 0:n], wf[:, :, 0:n]), reads=['wf'], writes=[('wb', gi % 2)])

            def next_ob(kind):
                i = cnt['ob'] % 3
                cnt['ob'] += 1
                return (obf if kind == 'f' else obb)[i], ('obf' if kind == 'f' else 'obb', i)

            def plain_evac(dst, srcp, r, w, scale=None):
                i = cnt['ev']
                cnt['ev'] += 1
                if scale is not None:
                    if i % 2:
                        P.op('act', lambda e: e.mul(dst, srcp, scale), reads=r, writes=w)
                    else:
                        P.op('dve', lambda e: e.tensor_scalar(dst, srcp, scale, None, ALU.mult), reads=r, writes=w)
                else:
                    if i % 2:
                        P.op('act', lambda e: e.copy(dst, srcp), reads=r, writes=w)
                    else:
                        P.op('dve', lambda e: e.tensor_copy(dst, srcp), reads=r, writes=w)

            def do_fm(gi):
                _, kind, c0, n, idx = groups[gi]
                wb = wbs[gi % 2]
                wk = ('wb', gi % 2)
                M = 128 if kind == 'idxk' else n
                if kind in ('mlq', 'mlk'):
                    cw = cws[cnt['cw'] % 2]
                    cwk = ('cw', cnt['cw'] % 2)
                    cnt['cw'] += 1
                    P.dma('sp', cw[:], convwT[l][idx * 128:(idx + 1) * 128, :], writes=[cwk])
                for tb in range(NB):
                    pi = cnt['pa'] % 4
                    cnt['pa'] += 1
                    pa = pas[pi]
                    pk = ('pa', pi)
                    for c in range(8):
                        mm(pa[0:M, :], wb[:, c, 0:M], xnT[:, c, tb * 512:(tb + 1) * 512], c == 0, c == 7,
                           [wk, ('xnT', tb)], [pk])
                    tsl = slice(tb * 512, (tb + 1) * 512)
                    if kind in ('mlq', 'mlk'):
                        st, stk = sts[tb % 2], ('st', tb % 2)
                        pst, pstk = sts[(tb + 1) % 2], ('st', (tb + 1) % 2)
                        acc, acck = accs[tb % 2], ('acc', tb % 2)
                        P.op('act', lambda e: e.copy(st[:, 3:515], pa[:, :]), reads=[pk], writes=[stk])
                        if tb == 0:
                            P.op('pool', lambda e: e.memset(st[:, 0:3], 0.0), writes=[stk])
                        else:
                            P.op('pool', lambda e: e.tensor_copy(st[:, 0:3], pst[:, 512:515]), reads=[pstk], writes=[stk])
                        P.op('dve', lambda e: e.tensor_scalar(acc[:], st[:, 0:512], cw[:, 0:1], cw[:, 4:5], ALU.mult, ALU.add),
                             reads=[stk, cwk], writes=[acck])
                        for j in (1, 2, 3):
                            P.op('dve', lambda e: e.scalar_tensor_tensor(acc[:], st[:, j:j + 512], cw[:, j:j + 1], acc[:], ALU.mult, ALU.add),
                                 reads=[stk, cwk, acck], writes=[acck])
                        ob, obk = next_ob('b')
                        act(ob[:], acc[:], AF.Silu, [acck], [obk])
                        P.dma('sp', qkT[idx * 128:(idx + 1) * 128, tsl], ob[:], reads=[obk], writes=[('qkT', idx, tb)])
                    elif kind == 'glaq':
                        ob, obk = next_ob('f')
                        plain_evac(ob[0:64, :], pa[0:64, :], [pk], [obk], scale=0.125)
                        P.dma('sp', gqT[idx, :, tsl], ob[0:64, :], reads=[obk], writes=[('gqT', idx, tb)])
                    elif kind == 'glak':
                        ob, obk = next_ob('f')
                        plain_evac(ob[0:64, :], pa[0:64, :], [pk], [obk])
                        P.dma('sp', gkT[idx, :, tsl], ob[0:64, :], reads=[obk], writes=[('gkT', idx, tb)])
                    elif kind == 'glaa':
                        at = aTb[cnt['at'] % 2]
                        atk = ('aTb', cnt['at'] % 2)
                        cnt['at'] += 1
                        P.op('act', lambda e: e.copy(at[:, :], pa[0:16, :]), reads=[pk], writes=[atk])
                        for h in range(4):
                            zi = cnt['pz'] % 2
                            cnt['pz'] += 1
                            pz, pzk = pzs[zi], ('pz', zi)
                            mm(pz[0:64, :], wab[:, h * 64:(h + 1) * 64], at[:, :], True, True, ['wab', atk], [pzk])
                            ob, obk = next_ob('f')
                            act(ob[0:64, :], pz[0:64, :], AF.Exp, [pzk, 'smallp'], [obk], scale=-1.0, bias=smallp[0:64, 4 + h:5 + h])
                            act(ob[0:64, :], ob[0:64, :], AF.Ln, [obk, 'cf'], [obk], bias=onec[0:64, :])
                            P.op('dve', lambda e: e.tensor_scalar(ob[0:64, :], ob[0:64, :], -1.0 / 16.0, None, ALU.mult), reads=[obk], writes=[obk])
                            P.dma('sp', laT[h, :, tsl], ob[0:64, :], reads=[obk], writes=[('laT', h, tb)])
                    elif kind in ('dsaq', 'dsak'):
                        sq, sqk = next_ob('f')
                        act(sq[:], pa[:], AF.Square, [pk], [sqk])
                        zi = cnt['pz'] % 2
                        cnt['pz'] += 1
                        pz, pzk = pzs[zi], ('pz', zi)
                        mm(pz[:, :], ones_f, sq[:], True, True, ['cf', sqk], [pzk])
                        act(sq[:], pz[:], AF.Ln, [pzk, 'cf'], [sqk], scale=1.0 / 128, bias=epsc)
                        act(sq[:], sq[:], AF.Exp, [sqk], [sqk], scale=-0.5)
                        ob, obk = next_ob('b')
                        gcol = 0 if kind == 'dsaq' else 1
                        P.op('dve', lambda e: e.scalar_tensor_tensor(ob[:], pa[:], smallp[:, gcol:gcol + 1], sq[:], ALU.mult, ALU.mult),
                             reads=[pk, 'smallp', sqk], writes=[obk])
                        dstT = QhT if kind == 'dsaq' else KhT
                        P.dma('sp', dstT[idx, :, tsl], ob[:], reads=[obk], writes=[(kind, idx, tb)])
                    elif kind == 'idxq':
                        ob, obk = next_ob('b')
                        plain_evac(ob[:], pa[:], [pk], [obk], scale=0.125)
                        P.dma('sp', iqT[idx * 128:(idx + 1) * 128, tsl], ob[:], reads=[obk], writes=[('iqT', idx, tb)])
                    elif kind == 'idxk':
                        ob, obk = next_ob('b')
                        plain_evac(ob[:], pa[:], [pk], [obk])
                        P.dma('sp', ikT2[:, tsl], ob[:], reads=[obk], writes=[('ikT2', tb)])
                    elif kind == 'mli':
                        ob, obk = next_ob('f')
                        P.op('dve', lambda e: e.tensor_scalar(ob[0:4, :], pa[0:4, :], smallp[0:4, 3:4], None, ALU.add), reads=[pk, 'smallp'], writes=[obk])
                        P.dma('sp', liT[:, tsl], ob[0:4, :], reads=[obk], writes=[('liT', tb)])
                    elif kind == 'mlf':
                        ob, obk = next_ob('f')
                        act(ob[0:4, :], pa[0:4, :], AF.Exp, [pk, 'smallp'], [obk], scale=-1.0, bias=smallp[0:4, 2:3])
                        act(ob[0:4, :], ob[0:4, :], AF.Ln, [obk, 'cf'], [obk], bias=onec[0:4, :])
                        P.op('dve', lambda e: e.tensor_scalar(ob[0:4, :], ob[0:4, :], -1.0, None, ALU.mult), reads=[obk], writes=[obk])
                        P.dma('sp', lfT[:, tsl], ob[0:4, :], reads=[obk], writes=[('lfT', tb)])

            def do_tm(gi):
                _, kind, c0, n, dst, off = groups[gi]
                wb = wbs[gi % 2]
                wk = ('wb', gi % 2)
                for t in range(NT):
                    pi = cnt['pa'] % 4
                    cnt['pa'] += 1
                    pa, pk = pas[pi], ('pa', pi)
                    for c in range(8):
                        mm(pa[:, 0:n], xnT[:, c, t * 128:(t + 1) * 128], wb[:, c, 0:n], c == 0, c == 7,
                           [wk, ('xnT', t // 4)], [pk])
                    rows = slice(t * 128, (t + 1) * 128)
                    if kind == 'idxw':
                        ob, obk = next_ob('f')
                        plain_evac(ob[:, 0:8], pa[:, 0:8], [pk], [obk], scale=8.0 ** -0.5)
                        P.dma('sp', dst[rows, :], ob[:, 0:8], reads=[obk], writes=[(kind, t)])
                        continue
                    ob, obk = next_ob('b')
                    if kind in ('mlv', 'glav', 'dsav'):
                        plain_evac(ob[:], pa[:], [pk], [obk])
                    elif kind == 'mlo':
                        act(ob[:], pa[:], AF.Sigmoid, [pk], [obk])
                    else:
                        act(ob[:], pa[:], AF.Silu, [pk], [obk])
                    P.dma('sp', dst[rows, off:off + 512], ob[:], reads=[obk], writes=[(kind, off, t)])

            load_w(0)
            for gi in range(len(groups)):
                if gi + 1 < len(groups):
                    load_w(gi + 1)
                if groups[gi][0] == 'fm':
                    do_fm(gi)
                else:
                    do_tm(gi)
            P.barrier()
        P.es = es

    def phaseB(l):
        with ExitStack() as ph:
            P.es = ph
            Cn = P.sbuf("Cn", [128, 4, 2, 257], F32)
            CnS = P.sbuf("CnS", [128, 4, 2, 257], BF16)
            mst = [P.sbuf("mst%d" % i, [4, 1], F32) for i in range(2)]
            gmB = P.sbuf("gmB", [128, 1024], F32)
            q4s = [P.sbuf("q4_%d" % i, [128, 8, 128], BF16) for i in range(2)]
            k4s = [P.sbuf("k4_%d" % i, [128, 8, 128], BF16) for i in range(2)]
            v1s = [P.sbuf("v1_%d" % i, [128, 4, 257], BF16) for i in range(2)]
            sgs = [P.sbuf("sg%d" % i, [128, 1024], BF16) for i in range(2)]
            zms = [P.sbuf("zm%d" % i, [128, 1024], BF16) for i in range(2)]
            glis = [P.sbuf("gli%d" % i, [4, 128], F32) for i in range(2)]
            glfs = [P.sbuf("glf%d" % i, [4, 128], F32) for i in range(2)]
            bc = P.sbuf("bcB", [4, 128], F32)
            aa = P.sbuf("aaB", [4, 128], F32)
            waT = P.sbuf("waT", [4, 128], F32)
            clT = P.sbuf("clT", [4, 128], F32)
            sm = P.sbuf("smB", [4, 8], F32)
            diagE = P.sbuf("diagE", [4, 4], F32)
            gt = P.sbuf("gtB", [128, 12], F32)
            sw = P.sbuf("swB", [128, 4, 128], BF16)
            kw = P.sbuf("kwB", [128, 4, 2, 128], BF16)
            hmo = P.sbuf("hmo", [128, 4, 256], F32)
            junk = P.sbuf("junkB", [128, 256], BF16)
            sml = P.sbuf("smlB", [128, 16], F32)
            Gz = P.sbuf("Gz", [128, 1024], F32)
            yts = [P.sbuf("ytB%d" % i, [128, 1024], BF16) for i in range(2)]
            g_ps = P.psum("g_ps", [128, 12], F32)
            st_ps = P.psum("st_ps", [128, 4, 128], F32)
            kt_ps = P.psum("kt_ps", [128, 8, 128], BF16)
            nds = [P.psum("nd%d" % i, [128, 257], F32) for i in range(2)]
            cus = [P.psum("cu%d" % i, [128, 257], F32) for i in range(2)]
            P.dma('sp', gmB[:], mlnB[l], writes=['gmB'])
            P.op('pool', lambda e: e.memset(Cn[:], 0.0), writes=['Cn'])
            P.op('pool', lambda e: e.memset(mst[0][:], 0.0), writes=[('mst', 0)])
            for i in range(2):
                P.op('pool', lambda e: e.memset(v1s[i][:, :, 256:257], 1.0), writes=[('v1', i)])
            qv = qkT[0:1024, :].rearrange("(g p) s -> p g s", p=128)
            kv = qkT[1024:2048, :].rearrange("(g p) s -> p g s", p=128)

            def loads(t):
                j = t % 2
                cols = slice(t * 128, (t + 1) * 128)
                P.dma('sp', q4s[j][:], qv[:, :, cols], writes=[('q4', j)])
                P.dma('sp', k4s[j][:], kv[:, :, cols], writes=[('k4', j)])
                P.dma('sp', v1s[j][:, :, 0:256], vml[cols, :].rearrange("p (h e) -> p h e", h=4), writes=[('v1', j)])
                P.dma('sp', sgs[j][:], sigo[cols, :], writes=[('sg', j)])
                P.dma('sp', zms[j][:], zml[cols, :], writes=[('zm', j)])
                P.dma('sp', glis[j][:], liT[:, cols], writes=[('gli', j)])
                P.dma('sp', glfs[j][:], lfT[:, cols], writes=[('glf', j)])

            loads(0)
            LN16 = float(np.log(16.0))
            for t in range(NT):
                j = t % 2
                if t + 1 < NT:
                    loads(t + 1)
                q4, k4, v1, sg, zm, gli, glf, yt = q4s[j], k4s[j], v1s[j], sgs[j], zms[j], glis[j], glfs[j], yts[j]
                mprev, mnext = mst[j], mst[1 - j]
                mk_, mnk = ('mst', j), ('mst', 1 - j)
                P.op('dve', lambda e: e.tensor_tensor_scan(bc[:], ones_f[0:4, 0:128], glf[:], 0.0, ALU.mult, ALU.add),
                     reads=['cf', ('glf', j)], writes=['bcB'])
                P.op('dve', lambda e: e.tensor_tensor(aa[:], gli[:], bc[:], ALU.subtract), reads=[('gli', j), 'bcB'], writes=['aaB'])
                P.op('dve', lambda e: e.tensor_reduce(sm[:, 0:1], aa[:], AX.X, ALU.max), reads=['aaB'], writes=['sm0'])
                P.op('dve', lambda e: e.tensor_tensor(sm[:, 1:2], sm[:, 0:1], mprev[:], ALU.max), reads=['sm0', mk_], writes=['sm1'])
                P.op('dve', lambda e: e.tensor_scalar(sm[:, 2:3], sm[:, 1:2], -1.0, None, ALU.mult), reads=['sm1'], writes=['sm2'])
                P.op('dve', lambda e: e.tensor_scalar(sm[:, 3:4], sm[:, 1:2], -1.0, -LN16, ALU.mult, ALU.add), reads=['sm1'], writes=['sm3'])
                act(sm[:, 4:5], mprev[:], AF.Exp, [mk_, 'sm2'], ['sm4'], bias=sm[:, 2:3])
                P.op('dve', lambda e: e.tensor_tensor(mnext[:], bc[:, 127:128], sm[:, 1:2], ALU.add), reads=['bcB', 'sm1'], writes=[mnk])
                act(waT[:], aa[:], AF.Exp, ['aaB', 'sm3'], ['waT'], bias=sm[:, 3:4])
                act(clT[:], bc[:], AF.Exp, ['bcB', 'sm2'], ['clT'], scale=-1.0, bias=sm[:, 2:3])
                P.op('dve', lambda e: e.tensor_scalar(diagE[:], ident_f[0:4, 0:4], sm[:, 4:5], None, ALU.mult), reads=['cf', 'sm4'], writes=['diagE'])
                mm(g_ps[:, 0:4], waT[:], ident_f[0:4, 0:4], True, True, ['waT', 'cf'], ['g_ps'])
                mm(g_ps[:, 4:8], clT[:], ident_f[0:4, 0:4], True, True, ['clT', 'cf'], ['g_ps'])
                mm(g_ps[:, 8:12], ones_f[0:4, 0:128], diagE[:], True, True, ['diagE', 'cf'], ['g_ps'])
                P.op('dve', lambda e: e.tensor_copy(gt[:], g_ps[:]), reads=['g_ps'], writes=['gt'])
                P.op('pool', lambda e: e.memset(sml[:, 0:4], 0.0), writes=['ssq'])
                for h in range(4):
                    P.op('act', lambda e: e.mul(CnS[:, h, :, :], Cn[:, h, :, :], gt[:, 8 + h:9 + h]), reads=['Cn', 'gt'], writes=[('CnS', h)])
                for h in range(4):
                    for dc in range(2):
                        mm(st_ps[:, h, :], k4[:, 2 * h + dc, :], q4[:, 2 * h + dc, :], dc == 0, dc == 1, [('k4', j), ('q4', j)], [('st_ps', h)])
                    P.op('dve', lambda e: e.scalar_tensor_tensor(sw[:, h, :], st_ps[:, h, :], gt[:, h:h + 1], maskT_f, ALU.mult, ALU.mult),
                         reads=[('st_ps', h), 'gt', 'cf'], writes=[('sw', h)])
                    for dc in range(2):
                        P.op('pe', lambda e: e.transpose(kt_ps[:, 2 * h + dc, :], k4[:, 2 * h + dc, :], ident_b), reads=[('k4', j), 'cb'], writes=[('kt_ps', h)])
                    P.op('act', lambda e: e.mul(kw[:, h, :, :], kt_ps[:, 2 * h:2 * h + 2, :], gt[:, h:h + 1]), reads=[('kt_ps', h), 'gt'], writes=[('kw', h)])
                    nd, ndk = nds[h % 2], ('nd', h % 2)
                    mm(nd[:, :], sw[:, h, :], v1[:, h, :], True, False, [('sw', h), ('v1', j)], [ndk])
                    for dc in range(2):
                        mm(nd[:, :], q4[:, 2 * h + dc, :], CnS[:, h, dc, :], False, dc == 1, [('q4', j), ('CnS', h)], [ndk])
                    for dc in range(2):
                        cu, cuk = cus[dc], ('cu', dc)
                        mm(cu[:, :], kw[:, h, dc, :], v1[:, h, :], True, True, [('kw', h), ('v1', j)], [cuk])
                        P.op('dve', lambda e: e.scalar_tensor_tensor(Cn[:, h, dc, :], Cn[:, h, dc, :], gt[:, 8 + h:9 + h], cu[:, :], ALU.mult, ALU.add),
                             reads=['Cn', 'gt', cuk, ('CnS', h)], writes=['Cn'])
                    act(sml[:, 4 + h:5 + h], nd[:, 256:257], AF.Abs, [ndk], [('dn', h)])
                    P.op('dve', lambda e: e.tensor_tensor(sml[:, 4 + h:5 + h], sml[:, 4 + h:5 + h], gt[:, 4 + h:5 + h], ALU.max),
                         reads=[('dn', h), 'gt'], writes=[('dn', h)])
                    P.op('dve', lambda e: e.reciprocal(sml[:, 8 + h:9 + h], sml[:, 4 + h:5 + h]), reads=[('dn', h)], writes=[('rd', h)])
                    P.op('dve', lambda e: e.scalar_tensor_tensor(hmo[:, h, :], nd[:, 0:256], sml[:, 8 + h:9 + h], sg[:, h * 256:(h + 1) * 256], ALU.mult, ALU.mult),
                         reads=[ndk, ('rd', h), ('sg', j)], writes=[('hmo', h)])
                    act(junk[:], hmo[:, h, :], AF.Square, [('hmo', h), 'ssq'], ['junkB', 'ssq'], accum_out=sml[:, h:h + 1])
                act(sml[:, 12:16], sml[:, 0:4], AF.Ln, ['ssq', 'cf'], ['rstdB'], scale=1.0 / 256, bias=epsc)
                act(sml[:, 12:16], sml[:, 12:16], AF.Exp, ['rstdB'], ['rstdB'], scale=-0.5)
                P.op('pool', lambda e: e.tensor_tensor(Gz[:], zm[:], gmB[:], ALU.mult), reads=[('zm', j), 'gmB'], writes=['Gz'])
                for h in range(4):
                    P.op('dve', lambda e: e.scalar_tensor_tensor(yt[:, h * 256:(h + 1) * 256], hmo[:, h, :], sml[:, 12 + h:13 + h], Gz[:, h * 256:(h + 1) * 256], ALU.mult, ALU.mult),
                         reads=[('hmo', h), 'rstdB', 'Gz'], writes=[('ytB', j)])
                P.dma('sp', ycat[t * 128:(t + 1) * 128, 0:1024], yt[:], reads=[('ytB', j)], writes=[('ycat', 0, t)])
            P.barrier()
        P.es = es

    def phaseC(l):
        with ExitStack() as ph:
            P.es = ph
            Sst = P.sbuf("Sst", [64, 4, 128], F32)
            Sbf = P.sbuf("Sbf", [64, 4, 128], BF16)
            tmpS = P.sbuf("tmpS", [64, 4, 128], F32)
            ggB = P.sbuf("ggB", [128, 512], F32)
            gqs = [P.sbuf("gq%d" % i, [64, 4, 128], F32) for i in range(2)]
            gks = [P.sbuf("gk%d" % i, [64, 4, 128], F32) for i in range(2)]
            las = [P.sbuf("la%d" % i, [64, 4, 128], F32) for i in range(2)]
            vgs = [P.sbuf("vgt%d" % i, [128, 512], BF16) for i in range(2)]
            rgs = [P.sbuf("rgt%d" % i, [128, 512], BF16) for i in range(2)]
            bc = P.sbuf("bcC", [64, 4, 128], F32)
            eb = P.sbuf("ebC", [64, 4, 128], F32)
            enb = P.sbuf("enbC", [64, 4, 128], F32)
            qt = P.sbuf("qtC", [64, 4, 128], BF16)
            kt = P.sbuf("ktC", [64, 4, 128], BF16)
            am = P.sbuf("amC", [128, 4, 128], BF16)
            ktk = P.sbuf("ktkC", [128, 4, 64], BF16)
            osq = P.sbuf("osqC", [128, 4, 128], F32)
            sml = P.sbuf("smlC", [128, 8], F32)
            Gr = P.sbuf("GrC", [128, 512], F32)
            ygs = [P.sbuf("yg%d" % i, [128, 512], BF16) for i in range(2)]
            at_ps = P.psum("at_ps", [128, 4, 128], F32)
            ktk_ps = P.psum("ktk_ps", [128, 4, 64], BF16)
            o_ps = P.psum("o_psC", [128, 4, 128], F32)
            su_ps = P.psum("su_ps", [64, 4, 128], F32)
            P.dma('sp', ggB[:], glnB[l], writes=['ggB'])
            P.op('pool', lambda e: e.memset(Sst[:], 0.0), writes=['Sst'])

            def loads(t):
                j = t % 2
                cols = slice(t * 128, (t + 1) * 128)
                P.dma('sp', gqs[j][:], gqT[:, :, cols].rearrange("h p s -> p h s"), writes=[('gq', j)])
                P.dma('sp', gks[j][:], gkT[:, :, cols].rearrange("h p s -> p h s"), writes=[('gk', j)])
                P.dma('sp', las[j][:], laT[:, :, cols].rearrange("h p s -> p h s"), writes=[('la', j)])
                P.dma('sp', vgs[j][:], vg[cols, :], writes=[('vgt', j)])
                P.dma('sp', rgs[j][:], rg[cols, :], writes=[('rgt', j)])

            loads(0)
            for t in range(NT):
                j = t % 2
                if t + 1 < NT:
                    loads(t + 1)
                gq, gk, la, vgt, rgt, yg = gqs[j], gks[j], las[j], vgs[j], rgs[j], ygs[j]
                for h in range(4):
                    P.op('dve', lambda e: e.tensor_tensor_scan(bc[:, h, :], ones_f[0:64, 0:128], la[:, h, :], 0.0, ALU.mult, ALU.add),
                         reads=['cf', ('la', j)], writes=['bcC'])
                act(eb[:], bc[:], AF.Exp, ['bcC'], ['ebC'])
                act(enb[:], bc[:], AF.Exp, ['bcC'], ['enbC'], scale=-1.0)
                P.op('pool', lambda e: e.tensor_tensor(qt[:], gq[:], eb[:], ALU.mult), reads=[('gq', j), 'ebC'], writes=['qtC'])
                P.op('dve', lambda e: e.tensor_tensor(kt[:], gk[:], enb[:], ALU.mult), reads=[('gk', j), 'enbC'], writes=['ktC'])
                for h in range(4):
                    mm(at_ps[:, h, :], kt[:, h, :], qt[:, h, :], True, True, ['ktC', 'qtC'], ['at_ps'])
                P.op('dve', lambda e: e.tensor_tensor(am[:], at_ps[:], mask4_b.rearrange("p (h s) -> p h s", h=4), ALU.mult),
                     reads=['at_ps', 'cb'], writes=['amC'])
                for h in range(4):
                    P.op('pe', lambda e: e.transpose(ktk_ps[:, h, :], kt[:, h, :], ident_b[0:64, 0:64]), reads=['ktC', 'cb'], writes=['ktk_ps'])
                P.op('act', lambda e: e.copy(ktk[:], ktk_ps[:]), reads=['ktk_ps'], writes=['ktkC'])
                P.op('pool', lambda e: e.tensor_copy(Sbf[:], Sst[:]), reads=['Sst'], writes=['Sbf'])
                for h in range(4):
                    mm(o_ps[:, h, :], am[:, h, :], vgt[:, h * 128:(h + 1) * 128], True, False, ['amC', ('vgt', j)], ['o_psC'])
                    mm(o_ps[:, h, :], qt[:, h, :], Sbf[:, h, :], False, True, ['qtC', 'Sbf'], ['o_psC'])
                for h in range(4):
                    mm(su_ps[:, h, :], ktk[:, h, :], vgt[:, h * 128:(h + 1) * 128], True, True, ['ktkC', ('vgt', j)], ['su_ps'])
                P.op('dve', lambda e: e.tensor_tensor(tmpS[:], Sst[:], su_ps[:], ALU.add), reads=['Sst', 'su_ps', 'Sbf'], writes=['tmpS'])
                for h in range(4):
                    P.op('dve', lambda e: e.tensor_scalar(Sst[:, h, :], tmpS[:, h, :], eb[:, h, 127:128], None, ALU.mult),
                         reads=['tmpS', 'ebC', 'Sbf'], writes=['Sst'])
                act(osq[:], o_ps[:], AF.Square, ['o_psC'], ['osqC'])
                P.op('dve', lambda e: e.tensor_reduce(sml[:, 0:4], osq[:], AX.X, ALU.add), reads=['osqC'], writes=['ssqC'])
                act(sml[:, 4:8], sml[:, 0:4], AF.Ln, ['ssqC', 'cf'], ['rstdC'], scale=1.0 / 128, bias=epsc)
                act(sml[:, 4:8], sml[:, 4:8], AF.Exp, ['rstdC'], ['rstdC'], scale=-0.5)
                P.op('pool', lambda e: e.tensor_tensor(Gr[:], rgt[:], ggB[:], ALU.mult), reads=[('rgt', j), 'ggB'], writes=['GrC'])
                for h in range(4):
                    P.op('dve', lambda e: e.scalar_tensor_tensor(yg[:, h * 128:(h + 1) * 128], o_ps[:, h, :], sml[:, 4 + h:5 + h], Gr[:, h * 128:(h + 1) * 128], ALU.mult, ALU.mult),
                         reads=['o_psC', 'rstdC', 'GrC'], writes=[('yg', j)])
                P.dma('sp', ycat[t * 128:(t + 1) * 128, 1024:1536], yg[:], reads=[('yg', j)], writes=[('ycat', 1, t)])
            P.barrier()
        P.es = es

    def phaseD(l):
        with ExitStack() as ph:
            P.es = ph
            nDmax = max(64, (int(S * 0.44) // 64) * 64)
            ikt = P.sbuf("ikt", [128, S], BF16)
            scores = [P.sbuf("score%d" % i, [128, S], F32) for i in range(2)]
            junkd = P.sbuf("junkD", [128, nDmax], BF16)
            junka = P.sbuf("junkA2", [128, S - 64], BF16)
            mbs = [P.sbuf("mb%d" % i, [128, S], BF16) for i in range(2)]
            iqts = [P.sbuf("iqt%d" % i, [128, 4, 128], BF16) for i in range(2)]
            iwts = [P.sbuf("iwt%d" % i, [128, 8], F32) for i in range(2)]
            dWs = [P.sbuf("dW%d" % i, [128, 8, 128], BF16) for i in range(2)]
            rb2s = [P.sbuf("rb2_%d" % i, [128, 1024], BF16) for i in range(3)]
            sms = [P.sbuf("smD%d" % i, [128, 16], F32) for i in range(2)]
            wtabs = [P.sbuf("wtab%d" % i, [128, 32], F32) for i in range(2)]
            lg2s = [P.psum("lg2_%d" % i, [128, 1024], F32) for i in range(3)]
            scs = [P.psum("sc%d" % i, [128, 512], F32) for i in range(2)]
            for c in range(0, S, 1024):
                P.dma('sp', ikt[:, c:c + 1024], ikT2[:, c:c + 1024], writes=['ikt'])
            iqv = iqT.rearrange("(g p) s -> p g s", p=128)

            def loads(t):
                j = t % 2
                cols = slice(t * 128, (t + 1) * 128)
                P.dma('sp', iqts[j][:], iqv[:, :, cols], writes=[('iqt', j)])
                P.dma('sp', iwts[j][:], iwS[cols, :], writes=[('iwt', j)])

            ci = {'lg': 0, 'sc': 0, 'ev': 0}

            def score_units(t):
                j = t % 2
                iqt, iwt, dW, score = iqts[j], iwts[j], dWs[j], scores[j]
                nk = 128 * (t + 1)
                steps = [(kb, hp) for kb in range((nk + 511) // 512) for hp in range(4)]
                pend = {}
                state = {}

                def prep():
                    for h in range(8):
                        P.op('pool', lambda e: e.tensor_scalar(dW[:, h, :], ident_b, iwt[:, h:h + 1], None, ALU.mult),
                             reads=['cb', ('iwt', j)], writes=[('dW', j)])

                def front(i):
                    kb, hp = steps[i]
                    li = ci['lg'] % 3
                    ci['lg'] += 1
                    lg, lgk, rb, rbk = lg2s[li], ('lg2', li), rb2s[li], ('rb2', li)
                    for hh in range(2):
                        po = hh * 64
                        mm(lg[:, hh * 512:(hh + 1) * 512], iqt[po:po + 64, hp, :], ikt[po:po + 64, kb * 512:(kb + 1) * 512], True, True,
                           [('iqt', j), 'ikt'], [lgk])
                    if i % 2:
                        act(rb[:], lg[:], AF.Relu, [lgk], [rbk])
                    else:
                        P.op('dve', lambda e: e.tensor_scalar(rb[:], lg[:], 0.0, None, ALU.max), reads=[lgk], writes=[rbk])
                    pend[i] = (rb, rbk)

                def back(i):
                    kb, hp = steps[i]
                    rb, rbk = pend.pop(i)
                    if hp == 0:
                        state['sc'] = (scs[ci['sc'] % 2], ('sc', ci['sc'] % 2))
                        ci['sc'] += 1
                    sc, sck = state['sc']
                    for hh in range(2):
                        h = 2 * hp + hh
                        mm(sc[:, :], dW[:, h, :], rb[:, hh * 512:(hh + 1) * 512], h == 0, h == 7, [('dW', j), rbk], [sck])
                    if hp == 3:
                        P.op('act', lambda e: e.copy(score[:, kb * 512:(kb + 1) * 512], sc[:, :]), reads=[sck], writes=[('score', j)])

                n = len(steps)
                units = [prep]
                for i in range(n + 2):
                    def unit(i=i):
                        if i < n:
                            front(i)
                        if i >= 2:
                            back(i - 2)
                    units.append(unit)
                return units

            def prelude(t):
                j = t % 2
                score, sm, wtab = scores[j], sms[j], wtabs[j]
                sk_, smk, wk = ('score', j), ('smD', j), ('wtab', j)
                nk = 128 * (t + 1)
                P.op('dve', lambda e: e.tensor_reduce(sm[:, 0:1], score[:, 0:nk], AX.X, ALU.max, apply_absolute_value=True), reads=[sk_], writes=[smk])
                P.op('pool', lambda e: e.memset(score[0:64, nk - 64:nk], -1.0e30), reads=[smk], writes=[sk_])
                P.op('dve', lambda e: e.tensor_scalar(sm[:, 3:4], sm[:, 0:1], 2.02, 2.0e-6, ALU.mult, ALU.add), reads=[smk], writes=[smk])
                P.op('dve', lambda e: e.tensor_scalar(wtab[:], cf[:, CF_PW:CF_PW + 32], sm[:, 3:4], None, ALU.mult), reads=[smk, 'cf'], writes=[wk])
                P.op('dve', lambda e: e.tensor_scalar(sm[:, 4:5], sm[:, 0:1], 0.0, None, ALU.mult), reads=[smk], writes=[('mid', j)])

            def split(nk):
                nD = (int(nk * 0.44) // 64) * 64
                nD = max(64, min(nk - 64, nD))
                return nD, nk - nD

            def count(t):
                j = t % 2
                score, sm = scores[j], sms[j]
                nk = 128 * (t + 1)
                nD, nA = split(nk)
                P.op('dve', lambda e: e.tensor_scalar(junkd[:, 0:nD], score[:, 0:nD], sm[:, 4:5], None, ALU.is_ge, ALU.add, accum_out=sm[:, 5:6]),
                     reads=[('score', j), ('mid', j)], writes=['junkD', ('cntD', j)])
                act(junka[:, 0:nA], score[:, nD:nk], AF.Sign, [('score', j), ('mid', j)], ['junkA2', ('sA', j)],
                    scale=-1.0, bias=sm[:, 4:5], accum_out=sm[:, 8:9])

            def update(t, it):
                j = t % 2
                sm, wtab = sms[j], wtabs[j]
                nk = 128 * (t + 1)
                nD, nA = split(nk)
                P.op('dve', lambda e: e.scalar_tensor_tensor(sm[:, 9:10], sm[:, 8:9], -0.5, sm[:, 5:6], ALU.mult, ALU.add),
                     reads=[('sA', j), ('cntD', j)], writes=[('t1', j)])
                P.op('dve', lambda e: e.tensor_scalar(sm[:, 6:7], sm[:, 9:10], TOPK - 0.5 - nA / 2.0, 0.5, ALU.is_ge, ALU.subtract),
                     reads=[('t1', j)], writes=[('sg', j)])
                P.op('dve', lambda e: e.scalar_tensor_tensor(sm[:, 4:5], sm[:, 6:7], wtab[:, it:it + 1], sm[:, 4:5], ALU.mult, ALU.add),
                     reads=[('sg', j), ('wtab', j), ('mid', j)], writes=[('mid', j)])

            def finish_tile(t):
                j = t % 2
                score, sm, wtab, mb = scores[j], sms[j], wtabs[j], mbs[j]
                nk = 128 * (t + 1)
                P.op('dve', lambda e: e.tensor_tensor(sm[:, 7:8], sm[:, 4:5], wtab[:, NBIS - 1:NBIS], ALU.subtract), reads=[('mid', j), ('wtab', j)], writes=[('thr', j)])
                h0 = (nk // 2 // 64) * 64
                P.op('dve', lambda e: e.tensor_scalar(mb[:, 0:h0], score[:, 0:h0], sm[:, 7:8], NEG, ALU.is_lt, ALU.mult), reads=[('score', j), ('thr', j)], writes=[('mb', j)])
                P.op('pool', lambda e: e.tensor_scalar(mb[:, h0:nk], score[:, h0:nk], sm[:, 7:8], NEG, ALU.is_lt, ALU.mult), reads=[('score', j), ('thr', j)], writes=[('mb', j)])
                P.dma('sp', MB[t, :, 0:nk], mb[:, 0:nk], reads=[('mb', j)], writes=[('MB', t)])

            loads(0)
            loads(1)
            for u in score_units(0):
                u()
            for t in range(NT):
                prelude(t)
                units = score_units(t + 1) if t + 1 < NT else []
                per = (len(units) + NBIS - 1) // NBIS
                ui = 0
                for it in range(NBIS):
                    count(t)
                    for _ in range(per):
                        if ui < len(units):
                            units[ui]()
                            ui += 1
                    update(t, it)
                while ui < len(units):
                    units[ui]()
                    ui += 1
                finish_tile(t)
                if t + 2 < NT:
                    loads(t + 2)
            P.barrier()
        with ExitStack() as ph:
            P.es = ph
            KT = P.sbuf("KT", [128, 4, S], BF16)
            V1 = P.sbuf("V1", [128, NT, 4, 129], BF16)
            ohs = P.sbuf("ohs", [128, len(_OHIDX), 128], BF16)
            relb = P.sbuf("relb", [128, 128], F32)
            biasT = P.sbuf("biasT", [128, 2, 4, 128], F32)
            ec = P.sbuf("ecD", [128, 4], F32)
            qhs = [P.sbuf("qh%d" % i, [128, 4, 128], BF16) for i in range(2)]
            mbts = [P.sbuf("mbt%d" % i, [128, S], BF16) for i in range(2)]
            zdts = [P.sbuf("zdt%d" % i, [128, 512], BF16) for i in range(2)]
            pTs = [P.sbuf("pT%d" % i, [128, 4, 128], BF16) for i in range(3)]
            tmps = [P.sbuf("tmpD%d" % i, [128, 4, 128], F32) for i in range(2)]
            on_sb = P.sbuf("on_sb", [128, 4, 129], F32)
            O = P.sbuf("O_D", [128, 4, 129], F32)
            rden = P.sbuf("rdenD", [128, 4], F32)
            yds = [P.sbuf("yd%d" % i, [128, 512], BF16) for i in range(2)]
            s_pss = [P.psum("s_ps%d" % i, [128, 4, 128], F32) for i in range(2)]
            ofs = [P.psum("of%d" % i, [128, 2, 129], F32) for i in range(2)]
            ons = [P.psum("on%d" % i, [128, 2, 129], F32) for i in range(2)]
            for h in range(4):
                P.dma('sp', KT[:, h, :], KhT[h], writes=['KT'])
            P.op('pool', lambda e: e.memset(V1[:, :, :, 128:129], 1.0), writes=['V1'])
            for u0 in range(NT):
                P.dma('sp', V1[:, u0, :, 0:128], vd[u0 * 128:(u0 + 1) * 128, :].rearrange("p (h e) -> p h e", h=4), writes=['V1'])
            P.dma('sp', ohs[:], oh_in, writes=['ohs'])
            P.dma('sp', relb[:], relB, writes=['relb'])
            P.op('pool', lambda e: e.memset(biasT[:], 0.0), writes=['biasT'])
            for i, (r, b) in enumerate(_OHIDX):
                for h in range(4):
                    P.op('dve', lambda e: e.scalar_tensor_tensor(biasT[:, r, h, :], ohs[:, i, :], relb[:, b * 4 + h:b * 4 + h + 1], biasT[:, r, h, :], ALU.mult, ALU.add),
                         reads=['ohs', 'relb', 'biasT'], writes=['biasT'])
            act(ec[:], relb[:, 60:64], AF.Exp, ['relb'], ['ecD'])

            def loads(t):
                j = t % 2
                cols = slice(t * 128, (t + 1) * 128)
                nk = 128 * (t + 1)
                P.dma('sp', qhs[j][:], QhT[:, :, cols].rearrange("h p s -> p h s"), writes=[('qh', j)])
                P.dma('sp', mbts[j][:, 0:nk], MB[t, :, 0:nk], writes=[('mbt', j)])
                P.dma('sp', zdts[j][:], zd[cols, :], writes=[('zdt', j)])

            loads(0)
            ci = {'s': 0, 'p': 0, 'tmp': 0}
            for t in range(NT):
                j = t % 2
                if t + 1 < NT:
                    loads(t + 1)
                qh, mbt, zdt, yd = qhs[j], mbts[j], zdts[j], yds[j]
                far_last = t - 2
                near_first = max(0, t - 1)
                def qk_exp(u):
                    near = u >= t - 1
                    s_ps, sk = s_pss[ci['s'] % 2], ('s_ps', ci['s'] % 2)
                    ci['s'] += 1
                    pT, pk = pTs[ci['p'] % 3], ('pT', ci['p'] % 3)
                    ci['p'] += 1
                    for h in range(4):
                        mm(s_ps[:, h, :], KT[:, h, u * 128:(u + 1) * 128], qh[:, h, :], True, False, ['KT', ('qh', j)], [sk])
                        mm(s_ps[:, h, :], mbt[:, u * 128:(u + 1) * 128], ident_b, False, True, [('mbt', j), 'cb'], [sk])
                    if near:
                        tmp, tk = tmps[ci['tmp'] % 2], ('tmpD', ci['tmp'] % 2)
                        ci['tmp'] += 1
                        P.op('dve', lambda e: e.tensor_tensor(tmp[:], s_ps[:], biasT[:, t - u, :, :], ALU.add), reads=[sk, 'biasT'], writes=[tk])
                        act(pT[:], tmp[:], AF.Exp, [tk], [pk])
                    else:
                        act(pT[:], s_ps[:], AF.Exp, [sk], [pk])
                    return pT, pk

                def pv(u, pT, pk):
                    near = u >= t - 1
                    for h in range(4):
                        if near:
                            o_, ok_ = ons[h // 2], ('on', h // 2)
                            first, last = (u == near_first), (u == t)
                        else:
                            o_, ok_ = ofs[h // 2], ('of', h // 2)
                            first, last = (u == 0), (u == far_last)
                        mm(o_[:, h % 2, :], pT[:, h, :], V1[:, u, h, :], first and (h % 2 == 0), last, [pk, 'V1'], [ok_])

                prev = None
                for u in range(t + 1):
                    cur = qk_exp(u)
                    if prev is not None:
                        pv(u - 1, *prev)
                    prev = cur
                pv(t, *prev)
                for hh in range(2):
                    P.op('act', lambda e: e.copy(on_sb[:, 2 * hh:2 * hh + 2, :], ons[hh][:]), reads=[('on', hh)], writes=[('on_sb', hh)])
                for h in range(4):
                    if t >= 2:
                        P.op('dve', lambda e: e.scalar_tensor_tensor(O[:, h, :], ofs[h // 2][:, h % 2, :], ec[:, h:h + 1], on_sb[:, h, :], ALU.mult, ALU.add),
                             reads=[('of', h // 2), 'ecD', ('on_sb', h // 2)], writes=[('O', h)])
                        Oh = O
                        Ok = ('O', h)
                    else:
                        Oh = on_sb
                        Ok = ('on_sb', h // 2)
                    P.op('dve', lambda e: e.reciprocal(rden[:, h:h + 1], Oh[:, h, 128:129]), reads=[Ok], writes=[('rden', h)])
                    P.op('dve', lambda e: e.scalar_tensor_tensor(yd[:, h * 128:(h + 1) * 128], Oh[:, h, 0:128], rden[:, h:h + 1], zdt[:, h * 128:(h + 1) * 128], ALU.mult, ALU.mult),
                         reads=[Ok, ('rden', h), ('zdt', j)], writes=[('yd', j)])
                P.dma('sp', ycat[t * 128:(t + 1) * 128, 1536:2048], yd[:], reads=[('yd', j)], writes=[('ycat', 2, t)])
            P.barrier()
        P.es = es

    def phaseE(l, src, dst):
        with ExitStack() as ph:
            P.es = ph
            wo = P.sbuf("wo", [128, 16, 1024], BF16)
            wf2 = P.sbuf("wf2", [128, 4, 1024], F32)
            yts = [P.sbuf("ytE%d" % i, [128, 2048], BF16) for i in range(2)]
            yTs = [P.sbuf("yTE%d" % i, [128, 16, 128], BF16) for i in range(2)]
            xts = [P.sbuf("xtE%d" % i, [128, 1024], F32) for i in range(2)]
            ots = [P.sbuf("otE%d" % i, [128, 1024], F32) for i in range(2)]
            tpe = [P.psum("tpe%d" % i, [128, 8, 128], BF16) for i in range(2)]
            oe = [P.psum("oe%d" % i, [128, 512], F32) for i in range(2)]
            wv = w_out[l].rearrange("(c p) n -> p c n", p=128)
            for q in range(4):
                P.dma('sp', wf2[:], wv[:, 4 * q:4 * q + 4, :], writes=['wf2'])
                P.op('pool', lambda e: e.tensor_copy(wo[:, 4 * q:4 * q + 4, :], wf2[:]), reads=['wf2'], writes=['wo'])

            def loads(t):
                j = t % 2
                rows = slice(t * 128, (t + 1) * 128)
                P.dma('sp', yts[j][:], ycat[rows, :], writes=[('ytE', j)])
                P.dma('sp', xts[j][:], src[rows, :], writes=[('xtE', j)])

            loads(0)
            for t in range(NT):
                j = t % 2
                if t + 1 < NT:
                    loads(t + 1)
                yt, yT, xt, ot = yts[j], yTs[j], xts[j], ots[j]
                for c in range(16):
                    P.op('pe', lambda e: e.transpose(tpe[c // 8][:, c % 8, :], yt[:, c * 128:(c + 1) * 128], ident_b),
                         reads=[('ytE', j), 'cb'], writes=[('tpe', c // 8)])
                P.op('act', lambda e: e.copy(yT[:, 0:8, :], tpe[0][:]), reads=[('tpe', 0)], writes=[('yTE', j)])
                P.op('dve', lambda e: e.tensor_copy(yT[:, 8:16, :], tpe[1][:]), reads=[('tpe', 1)], writes=[('yTE', j)])
                for half in range(2):
                    for c in range(16):
                        mm(oe[half][:, :], yT[:, c, :], wo[:, c, half * 512:(half + 1) * 512], c == 0, c == 15, [('yTE', j), 'wo'], [('oe', half)])
                    P.op('dve', lambda e: e.tensor_tensor(ot[:, half * 512:(half + 1) * 512], oe[half][:, :], xt[:, half * 512:(half + 1) * 512], ALU.add),
                         reads=[('oe', half), ('xtE', j)], writes=[('otE', j)])
                P.dma('sp', dst[t * 128:(t + 1) * 128, :], ot[:], reads=[('otE', j)], writes=[('dst', t)])
            P.barrier()
        P.es = es

    k = K()
    k.__dict__.update(locals())
    return k


def prep_shared(inputs):
    f = lambda a: np.ascontiguousarray(np.asarray(a, dtype=np.float32))
    cf, cb, oh, _ = host_consts()
    d = {}
    d["w_in"] = f(inputs["w_in"])
    d["w_out"] = f(inputs["w_out"])
    d["normB"] = f(np.broadcast_to(np.asarray(inputs["norm_g"])[:, None, :], (2, 128, 1024)))
    d["convwT"] = f(np.concatenate([np.transpose(np.asarray(inputs["ml_conv_w"]), (0, 2, 1)),
                                    np.asarray(inputs["ml_conv_b"])[:, :, None]], axis=2))
    d["ml_b_i"] = f(np.asarray(inputs["ml_b_i"])[:, :, None])
    d["ml_b_f"] = f(np.asarray(inputs["ml_b_f"])[:, :, None])
    d["mlnB"] = f(np.broadcast_to(np.asarray(inputs["ml_norm_g"])[:, None, :], (2, 128, 1024)))
    d["gla_w_a"] = f(inputs["gla_w_a"])
    d["gla_b_aT"] = f(np.transpose(np.asarray(inputs["gla_b_a"]).reshape(2, 4, 64), (0, 2, 1)))
    d["glnB"] = f(np.broadcast_to(np.asarray(inputs["gla_norm_g"])[:, None, :], (2, 128, 512)))
    d["dsa_q_g"] = f(np.asarray(inputs["dsa_q_g"])[:, :, None])
    d["dsa_k_g"] = f(np.asarray(inputs["dsa_k_g"])[:, :, None])
    d["relB"] = f(np.broadcast_to(np.asarray(inputs["rel_bias"]).reshape(1, 128), (128, 128)))
    d["cf"] = cf
    d["cb"] = cb
    d["oh"] = oh
    return d


def emit(k, phases="ABCDE", layers=(0,)):
    for l in layers:
        src = k.x_in if l == 0 else k.h1
        if "A" in phases:
            k.phaseA(l, src)
        if "B" in phases:
            k.phaseB(l)
        if "C" in phases:
            k.phaseC(l)
        if "D" in phases:
            k.phaseD(l)
        if "E" in phases:
            k.phaseE(l, src, k.h1 if l == 0 else k.out)


_CACHE = {}


def kernel(**inputs):
    x = np.asarray(inputs["x"], dtype=np.float32)
    B, S, D = x.shape
    key = (S,)
    if key not in _CACHE:
        k = build(S, depth=2, debug=False)
        emit(k, "ABCDE", layers=(0, 1))
        k.P.finish()
        _CACHE[key] = k
    k = _CACHE[key]
    shared = prep_shared(inputs)
    in_maps = []
    for b in range(B):
        d = dict(shared)
        d["x"] = np.ascontiguousarray(x[b])
        in_maps.append(d)
    res = run_bass_kernel_spmd(k.nc, in_maps, core_ids=list(range(B)))
    return np.stack([np.asarray(r["out"], dtype=np.float32) for r in res.results], axis=0)
```
